# Optimizing a Trainium2 kernel written in Bass

```python
import math
import jax
import jax.numpy as jnp
from jax import lax
import numpy as np

D_MODEL = 4096
BATCH = 2
SEQ = 4096
DEPTH = 2

MOBA_HEADS = 8
MOBA_HEAD_DIM = 128
MOBA_BLOCK = 256
MOBA_TOPK = 3
MOBA_Q_CHUNK = 32
SSM_D_INNER = 1024
SSM_HEAD_DIM = 64
SSM_HEADS = SSM_D_INNER // SSM_HEAD_DIM
SSM_GROUPS = 2
SSM_STATE = 128
SSM_CONV = 4
SSM_CHUNK = 128
SSM_CONV_CH = SSM_D_INNER + 2 * SSM_GROUPS * SSM_STATE
MLA_HEADS = 8
MLA_Q_LORA = 768
MLA_KV_LORA = 512
MLA_NOPE = 128
MLA_ROPE = 64
MLA_V = 128
MLA_Q_BLOCK = 128
ROPE_THETA = 10000.0
SWA_HEADS = 16
SWA_KV_HEADS = 2
SWA_HEAD_DIM = 64
SWA_WINDOW = 128
N_BRANCH = 4
BRANCH_W = 1024
N_GROUPS = 4
EXPERTS_PER_GROUP = 8
N_EXPERTS = N_GROUPS * EXPERTS_PER_GROUP
EXPERT_TOPK = 2
EXPERT_HIDDEN = 512
MOE_BLOCK = 128
NORM_EPS = 1e-6
N_ALIBI = MOBA_HEADS + SWA_HEADS

IN_SIZES = (
    3 * MOBA_HEADS * MOBA_HEAD_DIM,
    SSM_D_INNER,
    SSM_CONV_CH,
    SSM_HEADS,
    MLA_Q_LORA,
    MLA_KV_LORA,
    MLA_ROPE,
    SWA_HEADS * SWA_HEAD_DIM,
    SWA_KV_HEADS * SWA_HEAD_DIM,
    SWA_KV_HEADS * SWA_HEAD_DIM,
    N_BRANCH * D_MODEL,
)
N_IN = sum(IN_SIZES)

kernel_name = "hybrid_gated_mixers_hier_moe_block"


def rmsnorm(x, w):
    xf = x.astype(jnp.float32)
    y = xf * lax.rsqrt(jnp.mean(xf * xf, axis=-1, keepdims=True) + NORM_EPS)
    return (y * w.astype(jnp.float32)).astype(x.dtype)


def alibi_slopes():
    i = jnp.arange(1, N_ALIBI + 1, dtype=jnp.float32)
    s = jnp.exp2(-8.0 * i / N_ALIBI)
    return s[:SWA_HEADS], s[SWA_HEADS:]


def moba_attention(q, k, v, positions, slopes):
    B, S, H, Dh = q.shape
    BLK, QC = MOBA_BLOCK, MOBA_Q_CHUNK
    nblk = -(-S // BLK)
    pad = nblk * BLK - S
    kp = jnp.pad(k, ((0, 0), (0, pad), (0, 0), (0, 0)))
    vp = jnp.pad(v, ((0, 0), (0, pad), (0, 0), (0, 0)))
    pp = jnp.pad(positions, ((0, 0), (0, pad)))
    kb = kp.reshape(B, nblk, BLK, H, Dh).transpose(0, 3, 1, 2, 4)
    vb = vp.reshape(B, nblk, BLK, H, Dh).transpose(0, 3, 1, 2, 4)
    pb = pp.reshape(B, nblk, BLK)
    k_mean = jnp.mean(kb.astype(jnp.float32), axis=3)
    gate = jnp.einsum('bshd,bhnd->bhsn', q.astype(jnp.float32), k_mean)
    q_blk = jnp.arange(S) // BLK
    past = jnp.arange(nblk)[None, :] < q_blk[:, None]
    gate = jnp.where(past, gate, -jnp.inf)
    ksel = min(MOBA_TOPK, nblk)
    _, sel = lax.top_k(gate, ksel)
    sel_valid = jnp.arange(ksel)[None, :] < q_blk[:, None]
    scale = Dh ** -0.5
    bi = jnp.arange(B)[:, None, None, None]
    hi = jnp.arange(H)[None, :, None, None]

    def chunk(ci):
        t0 = ci * QC
        qc = lax.dynamic_slice_in_dim(q, t0, QC, axis=1)
        pq = lax.dynamic_slice_in_dim(positions, t0, QC, axis=1)
        sc = lax.dynamic_slice_in_dim(sel, t0, QC, axis=2)
        valid = lax.dynamic_slice_in_dim(sel_valid, t0, QC, axis=0)
        kg = kb[bi, hi, sc]
        vg = vb[bi, hi, sc]
        pk = pb[bi, sc]
        s_past = jnp.einsum('bqhd,bhqrkd->bhqrk', qc, kg).astype(jnp.float32) * scale
        dist = (pq[:, None, :, None, None] - pk).astype(jnp.float32)
        s_past = s_past - slopes[:, None, None, None] * jnp.abs(dist)
        s_past = jnp.where(valid[None, None, :, :, None], s_past, -jnp.inf)
        s_past = s_past.reshape(B, H, QC, ksel * BLK)
        j = t0 // BLK
        k_own = lax.dynamic_index_in_dim(kb, j, axis=2, keepdims=False)
        v_own = lax.dynamic_index_in_dim(vb, j, axis=2, keepdims=False)
        pos_own = lax.dynamic_index_in_dim(pb, j, axis=1, keepdims=False)
        s_own = jnp.einsum('bqhd,bhkd->bhqk', qc, k_own).astype(jnp.float32) * scale
        d_own = (pq[:, None, :, None] - pos_own[:, None, None, :]).astype(jnp.float32)
        s_own = s_own - slopes[:, None, None] * jnp.abs(d_own)
        q_loc = t0 - j * BLK + jnp.arange(QC)
        causal = jnp.arange(BLK)[None, :] <= q_loc[:, None]
        s_own = jnp.where(causal, s_own, -jnp.inf)
        p = jax.nn.softmax(jnp.concatenate([s_past, s_own], axis=-1), axis=-1).astype(v.dtype)
        o = jnp.einsum('bhqk,bhqkd->bqhd', p[..., :ksel * BLK], vg.reshape(B, H, QC, ksel * BLK, Dh))
        return o + jnp.einsum('bhqk,bhkd->bqhd', p[..., ksel * BLK:], v_own)

    out = lax.map(chunk, jnp.arange(S // QC))
    return out.transpose(1, 0, 2, 3, 4).reshape(B, S, H * Dh)


def causal_depthwise_conv(u, w, b):
    K, C = w.shape
    y = lax.conv_general_dilated(u, w[:, None, :], window_strides=(1,), padding=[(K - 1, 0)],
                                 dimension_numbers=('NWC', 'WIO', 'NWC'), feature_group_count=C)
    return y + b


def ssd_chunked(xs, dt, A, Bm, Cm):
    B, S, H, P = xs.shape
    N = Bm.shape[-1]
    L = SSM_CHUNK
    nc = S // L
    X = (xs.astype(jnp.float32) * dt[..., None]).reshape(B, nc, L, H, P)
    a = (dt * A).reshape(B, nc, L, H).transpose(0, 3, 1, 2)
    a_cs = jnp.cumsum(a, axis=-1)
    Bc = Bm.astype(jnp.float32).reshape(B, nc, L, H, N)
    Cc = Cm.astype(jnp.float32).reshape(B, nc, L, H, N)
    tri = jnp.tril(jnp.ones((L, L), dtype=bool))
    seg = jnp.exp(jnp.where(tri, a_cs[..., :, None] - a_cs[..., None, :], -jnp.inf))
    y_diag = jnp.einsum('bclhn,bcshn,bhcls,bcshp->bclhp', Cc, Bc, seg, X)
    decay_states = jnp.exp(a_cs[..., -1:] - a_cs)
    states = jnp.einsum('bclhn,bhcl,bclhp->bchpn', Bc, decay_states, X)
    chunk_decay = jnp.exp(a_cs[..., -1])

    def step(hstate, inp):
        st, dec = inp
        return hstate * dec[..., None, None] + st, hstate

    h0 = jnp.zeros((B, H, P, N), jnp.float32)
    _, prev = lax.scan(step, h0, (states.transpose(1, 0, 2, 3, 4), chunk_decay.transpose(2, 0, 1)))
    prev = prev.transpose(1, 0, 2, 3, 4)
    y_off = jnp.einsum('bclhn,bchpn,bhcl->bclhp', Cc, prev, jnp.exp(a_cs))
    return (y_diag + y_off).reshape(B, S, H, P)


def mamba2_mixer(z, xbc, dt_raw, conv_w, conv_b, dt_bias, a_log, d_skip, norm_w):
    B, S, _ = z.shape
    xbc = jax.nn.silu(causal_depthwise_conv(xbc, conv_w, conv_b))
    xs, Bm, Cm = jnp.split(xbc, [SSM_D_INNER, SSM_D_INNER + SSM_GROUPS * SSM_STATE], axis=-1)
    xs = xs.reshape(B, S, SSM_HEADS, SSM_HEAD_DIM)
    rep = SSM_HEADS // SSM_GROUPS
    Bm = jnp.repeat(Bm.reshape(B, S, SSM_GROUPS, SSM_STATE), rep, axis=2)
    Cm = jnp.repeat(Cm.reshape(B, S, SSM_GROUPS, SSM_STATE), rep, axis=2)
    dt = jax.nn.softplus(dt_raw.astype(jnp.float32) + dt_bias.astype(jnp.float32))
    A = -jnp.exp(a_log.astype(jnp.float32))
    y = ssd_chunked(xs, dt, A, Bm, Cm) + d_skip.astype(jnp.float32)[:, None] * xs.astype(jnp.float32)
    g = (y.reshape(B, S, SSM_D_INNER) * jax.nn.silu(z.astype(jnp.float32)))
    g = g.reshape(B, S, SSM_GROUPS, SSM_D_INNER // SSM_GROUPS)
    g = g * lax.rsqrt(jnp.mean(g * g, axis=-1, keepdims=True) + NORM_EPS)
    return (g.reshape(B, S, SSM_D_INNER) * norm_w.astype(jnp.float32)).astype(z.dtype)


def apply_rope(x, cos, sin):
    half = x.shape[-1] // 2
    x1, x2 = x[..., :half], x[..., half:]
    return jnp.concatenate([x1 * cos - x2 * sin, x2 * cos + x1 * sin], axis=-1)


def mla_attention(q_lat, kv_lat, k_rope_raw, positions, q_norm, wq_b, kv_norm, wkv_b):
    B, S, _ = q_lat.shape
    H, QB = MLA_HEADS, MLA_Q_BLOCK
    q = (rmsnorm(q_lat, q_norm) @ wq_b).reshape(B, S, H, MLA_NOPE + MLA_ROPE)
    kv = (rmsnorm(kv_lat, kv_norm) @ wkv_b).reshape(B, S, H, MLA_NOPE + MLA_V)
    q_nope, q_rope = q[..., :MLA_NOPE], q[..., MLA_NOPE:]
    k_nope, v = kv[..., :MLA_NOPE], kv[..., MLA_NOPE:]
    half = MLA_ROPE // 2
    inv_freq = ROPE_THETA ** (-jnp.arange(half, dtype=jnp.float32) / half)
    ang = positions.astype(jnp.float32)[..., None] * inv_freq
    cos, sin = jnp.cos(ang).astype(q.dtype), jnp.sin(ang).astype(q.dtype)
    q_rope = apply_rope(q_rope, cos[:, :, None, :], sin[:, :, None, :])
    k_rope = apply_rope(k_rope_raw, cos, sin)
    scale = (MLA_NOPE + MLA_ROPE) ** -0.5
    key_idx = jnp.arange(S)

    def qblock(bi):
        t0 = bi * QB
        qn = lax.dynamic_slice_in_dim(q_nope, t0, QB, axis=1)
        qr = lax.dynamic_slice_in_dim(q_rope, t0, QB, axis=1)
        s = jnp.einsum('bqhd,bkhd->bhqk', qn, k_nope) + jnp.einsum('bqhd,bkd->bhqk', qr, k_rope)
        s = s.astype(jnp.float32) * scale
        causal = key_idx[None, :] <= (t0 + jnp.arange(QB))[:, None]
        p = jax.nn.softmax(jnp.where(causal, s, -jnp.inf), axis=-1).astype(v.dtype)
        return jnp.einsum('bhqk,bkhd->bqhd', p, v)

    out = lax.map(qblock, jnp.arange(S // QB))
    return out.transpose(1, 0, 2, 3, 4).reshape(B, S, H * MLA_V)


def swa_attention(q, k, v, positions, sinks, slopes):
    B, S, H, Dh = q.shape
    KVH = k.shape[2]
    R = H // KVH
    W = SWA_WINDOW
    nb = S // W

    def with_prev(t):
        tb = t.reshape((B, nb, W) + t.shape[2:])
        prev = jnp.pad(tb[:, :-1], ((0, 0), (1, 0)) + ((0, 0),) * (tb.ndim - 2))
        return jnp.concatenate([prev, tb], axis=2)

    qb = q.reshape(B, nb, W, KVH, R, Dh)
    kb, vb, pk = with_prev(k), with_prev(v), with_prev(positions)
    pq = positions.reshape(B, nb, W)
    s = jnp.einsum('bnqgrd,bnkgd->bgrnqk', qb, kb).astype(jnp.float32) * Dh ** -0.5
    dist = jnp.abs(pq[..., :, None] - pk[..., None, :]).astype(jnp.float32)
    s = s - slopes.reshape(KVH, R)[None, :, :, None, None, None] * dist[:, None, None]
    rel = jnp.arange(W)[:, None] + W - jnp.arange(2 * W)[None, :]
    key_glob = (jnp.arange(nb)[:, None] - 1) * W + jnp.arange(2 * W)[None, :]
    allowed = ((rel >= 0) & (rel < W))[None] & (key_glob >= 0)[:, None, :]
    s = jnp.where(allowed, s, -jnp.inf)
    sink = jnp.broadcast_to(sinks.astype(jnp.float32).reshape(KVH, R)[None, :, :, None, None, None],
                            s.shape[:-1] + (1,))
    p = jax.nn.softmax(jnp.concatenate([s, sink], axis=-1), axis=-1)[..., :-1].astype(v.dtype)
    o = jnp.einsum('bgrnqk,bnkgd->bnqgrd', p, vb)
    return o.reshape(B, S, H * Dh)


def hybrid_mixer(h, positions, w_in, conv_w, conv_b, dt_bias, a_log, d_skip, ssm_norm,
                 mla_q_norm, mla_wq_b, mla_kv_norm, mla_wkv_b, swa_sinks, w_branch, w_out,
                 moba_slopes, swa_slopes):
    B, S, _ = h.shape
    proj = h @ w_in
    (m_qkv, z, xbc, dt_raw, q_lat, kv_lat, k_rope, sq, sk, sv, gate_logits) = jnp.split(
        proj, np.cumsum(IN_SIZES)[:-1].tolist(), axis=-1)
    m_qkv = m_qkv.reshape(B, S, 3, MOBA_HEADS, MOBA_HEAD_DIM)
    o_moba = moba_attention(m_qkv[:, :, 0], m_qkv[:, :, 1], m_qkv[:, :, 2], positions, moba_slopes)
    o_ssm = mamba2_mixer(z, xbc, dt_raw, conv_w, conv_b, dt_bias, a_log, d_skip, ssm_norm)
    o_mla = mla_attention(q_lat, kv_lat, k_rope, positions, mla_q_norm, mla_wq_b, mla_kv_norm, mla_wkv_b)
    o_swa = swa_attention(sq.reshape(B, S, SWA_HEADS, SWA_HEAD_DIM),
                          sk.reshape(B, S, SWA_KV_HEADS, SWA_HEAD_DIM),
                          sv.reshape(B, S, SWA_KV_HEADS, SWA_HEAD_DIM),
                          positions, swa_sinks, swa_slopes)
    o_cat = jnp.stack([o_moba, o_ssm, o_mla, o_swa], axis=2)
    y = jnp.einsum('bsrk,rkd->bsrd', o_cat, w_branch)
    gates = jax.nn.sigmoid(gate_logits.reshape(B, S, N_BRANCH, D_MODEL))
    merged = jnp.sum(gates * y, axis=2)
    return merged @ w_out


def hier_moe(h, wg, bg, wr, br, w_gate, w_up, w_down):
    B, S, D = h.shape
    T = B * S
    hf = h.reshape(T, D)
    g_prob = jax.nn.softmax((hf @ wg).astype(jnp.float32) + bg.astype(jnp.float32), axis=-1)
    g_w, g_idx = lax.top_k(g_prob, 1)
    e_logits = ((hf @ wr).astype(jnp.float32) + br.astype(jnp.float32)).reshape(T, N_GROUPS, EXPERTS_PER_GROUP)
    e_logits = jnp.take_along_axis(e_logits, g_idx[:, :, None], axis=1)[:, 0]
    e_w, e_loc = lax.top_k(jax.nn.softmax(e_logits, axis=-1), EXPERT_TOPK)
    e_w = e_w / jnp.sum(e_w, axis=-1, keepdims=True) * g_w
    e_idx = g_idx * EXPERTS_PER_GROUP + e_loc
    M = T * EXPERT_TOPK
    flat_e = e_idx.reshape(M)
    flat_tok = jnp.repeat(jnp.arange(T, dtype=jnp.int32), EXPERT_TOPK)
    flat_w = e_w.reshape(M)
    order = jnp.argsort(flat_e)
    sorted_e = flat_e[order]
    counts = jnp.bincount(flat_e, length=N_EXPERTS)
    start = jnp.cumsum(counts) - counts
    padded = (counts + MOE_BLOCK - 1) // MOE_BLOCK * MOE_BLOCK
    pad_end = jnp.cumsum(padded)
    pad_start = pad_end - padded
    dest = pad_start[sorted_e] + jnp.arange(M, dtype=jnp.int32) - start[sorted_e]
    n_blocks = -(-M // MOE_BLOCK) + N_EXPERTS
    cap = n_blocks * MOE_BLOCK
    slot_tok = jnp.full((cap,), T, jnp.int32).at[dest].set(flat_tok[order])
    slot_w = jnp.zeros((cap,), h.dtype).at[dest].set(flat_w[order].astype(h.dtype))
    block_expert = jnp.minimum(
        jnp.searchsorted(pad_end, jnp.arange(n_blocks, dtype=jnp.int32) * MOE_BLOCK, side='right'),
        N_EXPERTS - 1)
    h_pad = jnp.concatenate([hf, jnp.zeros((1, D), h.dtype)], axis=0)

    def expert_block(args):
        tok, e = args
        xb = h_pad[tok]
        return (jax.nn.silu(xb @ w_gate[e]) * (xb @ w_up[e])) @ w_down[e]

    ys = lax.map(expert_block, (slot_tok.reshape(n_blocks, MOE_BLOCK), block_expert))
    out = jnp.zeros((T + 1, D), h.dtype).at[slot_tok].add(ys.reshape(cap, D) * slot_w[:, None])
    return out[:T].reshape(B, S, D)


def setup_inputs(seed: int = 0) -> dict:
    key = jax.random.key(seed)
    ks = jax.random.split(key, 32)
    f32 = jnp.float32
    D, L = D_MODEL, DEPTH

    def nrm(k, shape, scale):
        return jax.random.normal(k, shape, f32) * scale

    x = nrm(ks[0], (BATCH, SEQ, D), 1.0)
    c = nrm(ks[1], (BATCH, D), 1.0)
    offs = jax.random.randint(ks[2], (BATCH, 1), 0, 1024, dtype=jnp.int32)
    positions = offs + jnp.arange(SEQ, dtype=jnp.int32)[None, :]
    dt0 = jnp.exp(jax.random.uniform(ks[10], (L, SSM_HEADS), f32, math.log(1e-3), math.log(1e-1)))
    return {
        "x": x,
        "c": c,
        "positions": positions,
        "ada_w": nrm(ks[3], (L, D, 6 * D), 0.2 * D ** -0.5),
        "ada_b": nrm(ks[4], (L, 6 * D), 0.02),
        "norm_mix": 1.0 + nrm(ks[5], (L, D), 0.02),
        "norm_ffn": 1.0 + nrm(ks[6], (L, D), 0.02),
        "w_in": nrm(ks[7], (L, D, N_IN), D ** -0.5),
        "conv_w": nrm(ks[8], (L, SSM_CONV, SSM_CONV_CH), SSM_CONV ** -0.5),
        "conv_b": nrm(ks[9], (L, SSM_CONV_CH), 0.02),
        "dt_bias": dt0 + jnp.log(-jnp.expm1(-dt0)),
        "a_log": jnp.log(jax.random.uniform(ks[11], (L, SSM_HEADS), f32, 1.0, 16.0)),
        "d_skip": 1.0 + nrm(ks[12], (L, SSM_HEADS), 0.02),
        "ssm_norm": 1.0 + nrm(ks[13], (L, SSM_D_INNER), 0.02),
        "mla_q_norm": 1.0 + nrm(ks[14], (L, MLA_Q_LORA), 0.02),
        "mla_wq_b": nrm(ks[15], (L, MLA_Q_LORA, MLA_HEADS * (MLA_NOPE + MLA_ROPE)), MLA_Q_LORA ** -0.5),
        "mla_kv_norm": 1.0 + nrm(ks[16], (L, MLA_KV_LORA), 0.02),
        "mla_wkv_b": nrm(ks[17], (L, MLA_KV_LORA, MLA_HEADS * (MLA_NOPE + MLA_V)), MLA_KV_LORA ** -0.5),
        "swa_sinks": nrm(ks[18], (L, SWA_HEADS), 0.5),
        "w_branch": nrm(ks[19], (L, N_BRANCH, BRANCH_W, D), BRANCH_W ** -0.5),
        "w_out": nrm(ks[20], (L, D, D), D ** -0.5),
        "router_group_w": nrm(ks[21], (L, D, N_GROUPS), D ** -0.5),
        "router_group_b": nrm(ks[22], (L, N_GROUPS), 0.01),
        "router_w": nrm(ks[23], (L, D, N_EXPERTS), D ** -0.5),
        "router_b": nrm(ks[24], (L, N_EXPERTS), 0.01),
        "exp_w_gate": nrm(ks[25], (L, N_EXPERTS, D, EXPERT_HIDDEN), D ** -0.5),
        "exp_w_up": nrm(ks[26], (L, N_EXPERTS, D, EXPERT_HIDDEN), D ** -0.5),
        "exp_w_down": nrm(ks[27], (L, N_EXPERTS, EXPERT_HIDDEN, D), EXPERT_HIDDEN ** -0.5),
        "final_norm": 1.0 + nrm(ks[28], (D,), 0.02),
    }


def reference(x, c, positions, ada_w, ada_b, norm_mix, norm_ffn, w_in, conv_w, conv_b, dt_bias,
              a_log, d_skip, ssm_norm, mla_q_norm, mla_wq_b, mla_kv_norm, mla_wkv_b, swa_sinks,
              w_branch, w_out, router_group_w, router_group_b, router_w, router_b,
              exp_w_gate, exp_w_up, exp_w_down, final_norm):
    swa_slopes, moba_slopes = alibi_slopes()
    for l in range(DEPTH):
        mod = (c @ ada_w[l] + ada_b[l])[:, None, :]
        sh1, sc1, g1, sh2, sc2, g2 = jnp.split(mod, 6, axis=-1)
        h = rmsnorm(x, norm_mix[l]) * (1.0 + sc1) + sh1
        x = x + g1 * hybrid_mixer(h, positions, w_in[l], conv_w[l], conv_b[l], dt_bias[l], a_log[l],
                                  d_skip[l], ssm_norm[l], mla_q_norm[l], mla_wq_b[l], mla_kv_norm[l],
                                  mla_wkv_b[l], swa_sinks[l], w_branch[l], w_out[l],
                                  moba_slopes, swa_slopes)
        h = rmsnorm(x, norm_ffn[l]) * (1.0 + sc2) + sh2
        x = x + g2 * hier_moe(h, router_group_w[l], router_group_b[l], router_w[l], router_b[l],
                              exp_w_gate[l], exp_w_up[l], exp_w_down[l])
    return rmsnorm(x, final_norm)
```

```python
import math
from contextlib import ExitStack
import numpy as np
import concourse.bass as bass
import concourse.mybir as mybir
from concourse.bass_utils import run_bass_kernel_spmd

F32 = mybir.dt.float32
BF16 = mybir.dt.bfloat16
I32 = mybir.dt.int32
AF = mybir.ActivationFunctionType
ALU = mybir.AluOpType
AX = mybir.AxisListType

FULL = dict(D=4096, S=4096, L=2, MH=8, MBLK=256, MTOPK=3, SDI=1024, SHD=64, SG=2, SN=128, SCONV=4,
            LH=8, QL=768, KVL=512, NOPE=128, ROPE=64, LV=128, WH=16, WKV=2, WD=64, WW=128, BW=1024,
            NG=4, EPG=8, EH=512, B=2)
NEG = -30000.0


def derive(c):
    c = dict(c)
    c['SH'] = c['SDI'] // c['SHD']
    c['CONVC'] = c['SDI'] + 2 * c['SG'] * c['SN']
    c['NE'] = c['NG'] * c['EPG']
    sizes = [3 * c['MH'] * 128, c['SDI'], c['CONVC'], c['SH'], c['QL'], c['KVL'], c['ROPE'],
             c['WH'] * c['WD'], c['WKV'] * c['WD'], c['WKV'] * c['WD'], 4 * c['D']]
    offs = np.concatenate([[0], np.cumsum(sizes)]).tolist()
    c['IN_OFF'] = offs
    c['N_IN'] = offs[-1]
    c['NALIBI'] = c['MH'] + c['WH']
    return c


class Buf:
    __slots__ = ('t', 'w', 'r', 'ds')

    def __init__(self, t):
        self.t = t
        self.w = None
        self.r = {}
        self.ds = None

    def __getitem__(self, k):
        return self.t[k]


class Rot:
    def __init__(self, bufs):
        self.bufs = bufs
        self.i = 0

    def next(self):
        b = self.bufs[self.i % len(self.bufs)]
        self.i += 1
        return b


class KB:
    def __init__(self, nc):
        self.nc = nc
        self.eng = {'pe': nc.tensor, 'act': nc.scalar, 'dve': nc.vector, 'pool': nc.gpsimd, 'sp': nc.sync}
        self.sems = {}
        self.cnt = {}
        for k in self.eng:
            self.sems[k] = nc.alloc_semaphore('e_' + k)
            self.cnt[k] = 0
        self.seen = {k: {} for k in self.eng}
        self.free_ds = []
        self.nds = 0
        self.scopes = []
        self.uid = 0
        self.ndram = 0

    def begin(self):
        self.scopes.append((ExitStack(), []))

    def end(self):
        self.barrier()
        st, bufs = self.scopes.pop()
        for b in bufs:
            if b.ds is not None:
                self.free_ds.append(b.ds)
                b.ds = None
        st.close()

    def dram(self, shape, dt):
        self.ndram += 1
        return self.nc.dram_tensor(f'scr{self.ndram}', list(shape), dt).ap()

    def sb(self, name, shape, dt):
        self.uid += 1
        st, bufs = self.scopes[-1]
        t = st.enter_context(self.nc.sbuf_tensor(f'{name}_{self.uid}', list(shape), dt))
        b = Buf(t)
        bufs.append(b)
        return b

    def ps(self, name, shape, dt=F32):
        self.uid += 1
        st, bufs = self.scopes[-1]
        t = st.enter_context(self.nc.psum_tensor(f'{name}_{self.uid}', list(shape), dt))
        b = Buf(t)
        bufs.append(b)
        return b

    def rot(self, name, shape, dt, n, psum=False):
        return Rot([(self.ps if psum else self.sb)(f'{name}{i}', shape, dt) for i in range(n)])

    def _dsem(self, b):
        if b.ds is None:
            if self.free_ds:
                b.ds = self.free_ds.pop()
            else:
                k = f'd{self.nds}'
                self.nds += 1
                self.sems[k] = self.nc.alloc_semaphore(k)
                self.cnt[k] = 0
                b.ds = k
        return b.ds

    def _need(self, e, dep, raw=True):
        if dep is None:
            return
        k, v = dep
        if k == e and (e == 'pe' or not raw):
            return
        if self.seen[e].get(k, 0) >= v:
            return
        self.eng[e].wait_ge(self.sems[k], v)
        self.seen[e][k] = v

    def _deps(self, e, reads, writes):
        for b in reads:
            self._need(e, b.w, raw=True)
        for b in writes:
            self._need(e, b.w, raw=False)
            for k, v in b.r.items():
                self._need(e, (k, v), raw=False)

    def op(self, e, ins_fn, reads=(), writes=()):
        self._deps(e, reads, writes)
        ins = ins_fn(self.eng[e])
        self.cnt[e] += 1
        ins.then_inc(self.sems[e], 1)
        v = self.cnt[e]
        for b in reads:
            if b.r.get(e, 0) < v:
                b.r[e] = v
        for b in writes:
            b.w = (e, v)
            b.r = {}
        return ins

    def load(self, q, b, pairs, **kw):
        self._deps(q, (), (b,))
        ds = self._dsem(b)
        for o, i in pairs:
            self.eng[q].dma_start(out=o, in_=i, **{'allow_slow_non_contiguous': True, **kw}).then_inc(self.sems[ds], 16)
            self.cnt[ds] += 16
        b.w = (ds, self.cnt[ds])
        b.r = {}

    def store(self, q, b, pairs, **kw):
        self._deps(q, (b,), ())
        ds = self._dsem(b)
        for o, i in pairs:
            self.eng[q].dma_start(out=o, in_=i, **{'allow_slow_non_contiguous': True, **kw}).then_inc(self.sems[ds], 16)
            self.cnt[ds] += 16
        b.r[ds] = self.cnt[ds]

    def barrier(self):
        for e in self.eng:
            for k, v in self.cnt.items():
                if k != e and v > 0:
                    self._need(e, (k, v), raw=False)


def linear(kb, XT, K, T, Wfn, N, epi, tm=False, NW=512, TT=512, wq='pool', xq='sp'):
    KC = K // 128
    assert K % 128 == 0
    wts = kb.rot('lw', [128, KC, NW], BF16, 2)
    nt = (T + TT - 1) // TT
    xts = kb.rot('lx', [128, KC, min(TT, T)], BF16, 1 if nt == 1 else 2)
    pss = kb.rot('lp', [128, 512], F32, 4, psum=True)
    XTv = XT.rearrange("(kc p) t -> p kc t", p=128)
    xt = None
    for n0g in range(0, N, NW):
        nsg = min(NW, N - n0g)
        wt = wts.next()
        W = Wfn(n0g, nsg)
        kb.load(wq, wt, [(wt[:, :, :nsg], W.rearrange("(kc p) n -> p kc n", p=128))])
        for ti in range(nt):
            t0 = ti * TT
            ts = min(TT, T - t0)
            if not (nt == 1 and xt is not None):
                xt = xts.next()
                kb.load(xq, xt, [(xt[:, :, :ts], XTv[:, :, t0:t0 + ts])])
            if not tm:
                for c0 in range(0, nsg, 128):
                    cs = min(128, nsg - c0)
                    ps = pss.next()
                    for kc in range(KC):
                        kb.op('pe', lambda e, kc=kc: e.matmul(ps[:cs, :ts], lhsT=wt[:, kc, c0:c0 + cs],
                                                              rhs=xt[:, kc, :ts], start=(kc == 0),
                                                              stop=(kc == KC - 1)),
                              reads=(wt, xt), writes=(ps,))
                    epi(n0g + c0, cs, t0, ts, ps)
            else:
                for s0 in range(0, ts, 128):
                    ss = min(128, ts - s0)
                    ps = pss.next()
                    for kc in range(KC):
                        kb.op('pe', lambda e, kc=kc: e.matmul(ps[:ss, :nsg], lhsT=xt[:, kc, s0:s0 + ss],
                                                              rhs=wt[:, kc, :nsg], start=(kc == 0),
                                                              stop=(kc == KC - 1)),
                              reads=(wt, xt), writes=(ps,))
                    epi(n0g, nsg, t0 + s0, ss, ps)


def store_epi(kb, dst, dt, scale=None, bias_buf=None, func=None, eng='act', q='act', tm=False, coloff=0):
    obs = kb.rot('eo', [128, 512], dt, 3)

    def epi(n0, ns, t0, ts, ps):
        ob = obs.next()
        if tm:
            p, f = ts, ns
        else:
            p, f = ns, ts
        kb.op('act', lambda e: e.activation(out=ob[:p, :f], in_=ps[:p, :f], func=func or AF.Identity,
                                            scale=1.0 if scale is None else scale),
              reads=(ps,), writes=(ob,))
        if tm:
            kb.store(q, ob, [(dst[t0:t0 + ts, coloff + n0:coloff + n0 + ns], ob[:p, :f])])
        else:
            kb.store(q, ob, [(dst[coloff + n0:coloff + n0 + ns, t0:t0 + ts], ob[:p, :f])])
    return epi


def rmsnorm_fm(kb, C, src, dst, F, T, gam, bet, eps=1e-6, dst_dt=BF16, TT=256):
    FC = F // 128
    xs = kb.rot('nx', [128, FC, TT], F32, 2)
    sq = kb.rot('nq', [128, TT], F32, 2)
    pr = kb.rot('np', [128, 512], F32, 2, psum=True)
    rs = kb.rot('nr', [128, TT], F32, 2)
    ob = kb.rot('no', [128, FC, TT], dst_dt, 2)
    ones = C['ones_f32']
    sv = src.rearrange("(fc p) t -> p fc t", p=128)
    dv = dst.rearrange("(fc p) t -> p fc t", p=128)
    for t0 in range(0, T, TT):
        ts = min(TT, T - t0)
        x = xs.next()
        kb.load('sp', x, [(x[:, :, :ts], sv[:, :, t0:t0 + ts])])
        p = pr.next()
        for fc in range(FC):
            s = sq.next()
            kb.op('act', lambda e: e.activation(out=s[:, :ts], in_=x[:, fc, :ts], func=AF.Square), reads=(x,), writes=(s,))
            kb.op('pe', lambda e: e.matmul(p[:, :ts], lhsT=ones[:, :], rhs=s[:, :ts], start=(fc == 0), stop=(fc == FC - 1)),
                  reads=(s, ones), writes=(p,))
        r = rs.next()
        kb.op('dve', lambda e: e.tensor_scalar(out=r[:, :ts], in0=p[:, :ts], scalar1=1.0 / F, scalar2=eps,
                                               op0=ALU.mult, op1=ALU.add), reads=(p,), writes=(r,))
        kb.op('act', lambda e: e.activation(out=r[:, :ts], in_=r[:, :ts], func=AF.Ln), reads=(r,), writes=(r,))
        kb.op('act', lambda e: e.activation(out=r[:, :ts], in_=r[:, :ts], func=AF.Exp, scale=-0.5), reads=(r,), writes=(r,))
        o = ob.next()
        for fc in range(FC):
            kb.op('dve', lambda e: e.tensor_tensor(out=x[:, fc, :ts], in0=x[:, fc, :ts], in1=r[:, :ts], op=ALU.mult),
                  reads=(x, r), writes=(x,))
            if bet is not None:
                kb.op('act', lambda e: e.activation(out=o[:, fc, :ts], in_=x[:, fc, :ts], func=AF.Identity,
                                                    scale=gam[:, fc:fc + 1], bias=bet[:, fc:fc + 1]),
                      reads=(x, gam, bet), writes=(o,))
            else:
                kb.op('act', lambda e: e.activation(out=o[:, fc, :ts], in_=x[:, fc, :ts], func=AF.Identity,
                                                    scale=gam[:, fc:fc + 1]),
                      reads=(x, gam), writes=(o,))
        kb.store('act', o, [(dv[:, :, t0:t0 + ts], o[:, :, :ts])])


def ew_combine(kb, srcs, dst, F, T, dst_dt, gate=None, base=None, TT=512):
    n = len(srcs) + (1 if base is not None else 0)
    tl = kb.rot('ec', [128, n, TT], F32, 2)
    ob = kb.rot('eo', [128, TT], dst_dt, 2)
    for fc in range(F // 128):
        r0 = fc * 128
        for t0 in range(0, T, TT):
            ts = min(TT, T - t0)
            x = tl.next()
            pairs = [(x[:, i, :ts], s[r0:r0 + 128, t0:t0 + ts]) for i, s in enumerate(srcs)]
            if base is not None:
                pairs.append((x[:, n - 1, :ts], base[r0:r0 + 128, t0:t0 + ts]))
            kb.load('sp', x, pairs)
            for i in range(1, len(srcs)):
                kb.op('dve', lambda e: e.tensor_tensor(out=x[:, 0, :ts], in0=x[:, 0, :ts], in1=x[:, i, :ts], op=ALU.add),
                      reads=(x,), writes=(x,))
            o = ob.next()
            if base is not None:
                kb.op('dve', lambda e: e.scalar_tensor_tensor(out=o[:, :ts], in0=x[:, 0, :ts], scalar=gate[:, fc:fc + 1],
                                                              in1=x[:, n - 1, :ts], op0=ALU.mult, op1=ALU.add),
                      reads=(x, gate), writes=(o,))
            else:
                kb.op('act', lambda e: e.activation(out=o[:, :ts], in_=x[:, 0, :ts], func=AF.Identity), reads=(x,), writes=(o,))
            kb.store('act', o, [(dst[r0:r0 + 128, t0:t0 + ts], o[:, :ts])])


def attn_head(kb, C, c, parts, v_ap, dv, out_ap, scale, slope, mode, A, selT=None, sinkcol=None):
    S = c['S']
    NKT = S // 128
    QT = 128 if mode == 'swa' else 256
    qs, ks = [], []
    for i, (q_ap, k_ap, dk) in enumerate(parts):
        qb = A['q%d' % i].next()
        kb.load('sp', qb, [(qb[:dk, :], q_ap)])
        kbuf = A['k%d' % i].next()
        kb.load('sp', kbuf, [(kbuf[:dk, :], k_ap)])
        qs.append(qb)
        ks.append(kbuf)
    vb = A['v'].next()
    kb.load('sp', vb, [(vb[:, :, :dv], v_ap.rearrange("(t p) d -> p t d", p=128))])
    masks = C['masks']
    for qi in range(S // QT):
        q0 = qi * QT
        if mode == 'swa':
            kts = ([(qi - 1, 2)] if qi >= 1 else []) + [(qi, 0)]
        else:
            kts = []
            for kt in range(2 * qi + 2):
                if kt // 2 == qi:
                    kts.append((kt, kt % 2))
                else:
                    kts.append((kt, 'sel' if mode == 'moba' else None))
        o_ps = A['o_ps'].next()
        d_ps = A['d_ps'].next()
        for j, (kt, mk) in enumerate(kts):
            s_ps = A['s_ps'].next()
            for i, (q_ap, k_ap, dk) in enumerate(parts):
                kb.op('pe', lambda e: e.matmul(s_ps[:, :QT], lhsT=ks[i][:dk, kt * 128:(kt + 1) * 128],
                                               rhs=qs[i][:dk, q0:q0 + QT], start=(i == 0), stop=(i == len(parts) - 1)),
                      reads=(ks[i], qs[i]), writes=(s_ps,))
            ex = A['ex'].next()
            if slope is not None:
                ds = A['ds'].next()
                kb.op('dve', lambda e: e.tensor_scalar(out=ds[:, :QT], in0=A['posq'][:, q0:q0 + QT], scalar1=A['posk'][:, kt:kt + 1],
                                                       scalar2=None, op0=ALU.subtract),
                      reads=(A['posq'], A['posk']), writes=(ds,))
                kb.op('dve', lambda e: e.scalar_tensor_tensor(out=ds[:, :QT], in0=ds[:, :QT], scalar=-1.0, in1=ds[:, :QT],
                                                              op0=ALU.mult, op1=ALU.max), reads=(ds,), writes=(ds,))
                kb.op('dve', lambda e: e.scalar_tensor_tensor(out=ds[:, :QT], in0=ds[:, :QT], scalar=-slope / scale, in1=s_ps[:, :QT],
                                                              op0=ALU.mult, op1=ALU.add), reads=(ds, s_ps), writes=(ds,))
                src = ds
            else:
                src = s_ps
            if mk is None:
                pb = A['pb'].next()
                kb.op('act', lambda e: e.activation(out=pb[:, :QT], in_=src[:, :QT], func=AF.Exp, scale=scale), reads=(src,), writes=(pb,))
            else:
                kb.op('act', lambda e: e.activation(out=ex[:, :QT], in_=src[:, :QT], func=AF.Exp, scale=scale), reads=(src,), writes=(ex,))
                pb = A['pb'].next()
                if mk == 'sel':
                    m_ps = A['m_ps'].next()
                    kb.op('pe', lambda e: e.matmul(m_ps[:, :QT], lhsT=C['esel'][:, kt // 2, :], rhs=selT[:, q0:q0 + QT], start=True, stop=True),
                          reads=(C['esel'], selT), writes=(m_ps,))
                    kb.op('dve', lambda e: e.tensor_tensor(out=pb[:, :QT], in0=ex[:, :QT], in1=m_ps[:, :QT], op=ALU.mult),
                          reads=(ex, m_ps), writes=(pb,))
                else:
                    kb.op('dve', lambda e: e.tensor_tensor(out=pb[:, :QT], in0=ex[:, :QT], in1=masks[:, mk, :QT], op=ALU.mult),
                          reads=(ex, masks), writes=(pb,))
            kb.op('pe', lambda e: e.matmul(o_ps[:dv, :QT], lhsT=vb[:, kt, :dv], rhs=pb[:, :QT], start=(j == 0), stop=(j == len(kts) - 1)),
                  reads=(vb, pb), writes=(o_ps,))
            kb.op('pe', lambda e: e.matmul(d_ps[:, :QT], lhsT=C['ones_bf'][:, :], rhs=pb[:, :QT], start=(j == 0), stop=(j == len(kts) - 1)),
                  reads=(C['ones_bf'], pb), writes=(d_ps,))
        rc = A['rc'].next()
        if sinkcol is not None:
            kb.op('dve', lambda e: e.tensor_scalar(out=rc[:, :QT], in0=d_ps[:, :QT], scalar1=sinkcol, scalar2=None, op0=ALU.add),
                  reads=(d_ps, A['esink']), writes=(rc,))
            kb.op('dve', lambda e: e.reciprocal(out=rc[:, :QT], in_=rc[:, :QT]), reads=(rc,), writes=(rc,))
        else:
            kb.op('dve', lambda e: e.reciprocal(out=rc[:, :QT], in_=d_ps[:, :QT]), reads=(d_ps,), writes=(rc,))
        ob = A['ob'].next()
        kb.op('dve', lambda e: e.tensor_tensor(out=ob[:dv, :QT], in0=o_ps[:dv, :QT], in1=rc[:dv, :QT], op=ALU.mult),
              reads=(o_ps, rc), writes=(ob,))
        kb.store('act', ob, [(out_ap[:, q0:q0 + QT], ob[:dv, :QT])])


def attn_bufs(kb, c, nparts, dks, need_pos, posf):
    S = c['S']
    A = {}
    for i in range(nparts):
        A['q%d' % i] = kb.rot('aq%d' % i, [128, S], BF16, 2)
        A['k%d' % i] = kb.rot('ak%d' % i, [128, S], BF16, 2)
    A['v'] = kb.rot('av', [128, S // 128, 128], BF16, 2)
    A['s_ps'] = kb.rot('as', [128, 512], F32, 2, psum=True)
    A['m_ps'] = kb.rot('am', [128, 512], F32, 2, psum=True)
    A['o_ps'] = kb.rot('ao', [128, 512], F32, 2, psum=True)
    A['d_ps'] = kb.rot('ad', [128, 512], F32, 2, psum=True)
    A['ex'] = kb.rot('aex', [128, 256], F32, 3)
    A['ds'] = kb.rot('ads', [128, 256], F32, 3)
    A['pb'] = kb.rot('apb', [128, 256], BF16, 3)
    A['rc'] = kb.rot('arc', [128, 256], F32, 2)
    A['ob'] = kb.rot('aob', [128, 256], BF16, 2)
    if need_pos:
        A['posq'] = kb.sb('posq', [128, S], F32)
        kb.load('sp', A['posq'], [(A['posq'][:, :], posf[0:1, :].to_broadcast([128, S]))])
        A['posk'] = kb.sb('posk', [128, S // 128], F32)
        kb.load('sp', A['posk'], [(A['posk'][:, :], posf[0, :].rearrange("(t p) -> p t", p=128))], allow_slow_non_contiguous=True)
    return A


def alibi_slopes(c):
    i = np.arange(1, c['NALIBI'] + 1, dtype=np.float32)
    s = np.exp2(np.float32(-8.0) * i / np.float32(c['NALIBI'])).astype(np.float32)
    return [float(v) for v in s[:c['WH']]], [float(v) for v in s[c['WH']:]]


def moba_phase(kb, C, c, ins, mqT, mkT, mv, ocT, posf):
    S, MH, NB = c['S'], c['MH'], c['S'] // c['MBLK']
    swa_sl, moba_sl = alibi_slopes(c)
    kb.begin()
    A = attn_bufs(kb, c, 1, [128], True, posf)
    NT = S // 128
    el = kb.sb('elig', [128, NT, NB], F32)
    kb.load('sp', el, [(el[:, :, :], ins['elig'].rearrange("(t p) n -> p t n", p=128))])
    eln = kb.sb('eln', [128, NT, NB], F32)
    kb.op('dve', lambda e: e.tensor_scalar(out=eln[:, :, :], in0=el[:, :, :], scalar1=-1.0, scalar2=1e30, op0=ALU.add, op1=ALU.mult),
          reads=(el,), writes=(eln,))
    W8 = max(NB, 8)
    gms = kb.rot('gm', [128, W8], F32, 2)
    for b in gms.bufs:
        kb.op('dve', lambda e: e.memset(b[:, :], -1e30), writes=(b,))
    mx = kb.rot('mx', [128, 8], F32, 2)
    sl = kb.rot('sl', [128, NB], F32, 2)
    km = kb.sb('km', [128, NB], F32)
    kmb = kb.sb('kmb', [128, NB], BF16)
    selT = kb.rot('selT', [NB, S], BF16, 2)
    kfull = kb.rot('kfull', [128, S], BF16, 2)
    qfull = kb.rot('qfull', [128, S], BF16, 2)
    for h in range(MH):
        kf = kfull.next()
        qf = qfull.next()
        kb.load('sp', kf, [(kf[:, :], mkT[h * 128:(h + 1) * 128, :])])
        kb.load('sp', qf, [(qf[:, :], mqT[h * 128:(h + 1) * 128, :])])
        kb.op('dve', lambda e: e.tensor_reduce(out=km[:, :], in_=kf[:, :].rearrange("p (n k) -> p n k", n=NB), axis=AX.X, op=ALU.add),
              reads=(kf,), writes=(km,))
        kb.op('act', lambda e: e.activation(out=kmb[:, :], in_=km[:, :], func=AF.Identity, scale=1.0 / c['MBLK']), reads=(km,), writes=(kmb,))
        st = selT.next()
        for qt in range(NT):
            g_ps = A['s_ps'].next()
            kb.op('pe', lambda e: e.matmul(g_ps[:, :NB], lhsT=qf[:, qt * 128:(qt + 1) * 128], rhs=kmb[:, :], start=True, stop=True),
                  reads=(qf, kmb), writes=(g_ps,))
            gm = gms.next()
            kb.op('dve', lambda e: e.tensor_tensor(out=gm[:, :NB], in0=g_ps[:, :NB], in1=el[:, qt, :], op=ALU.mult), reads=(g_ps, el), writes=(gm,))
            kb.op('dve', lambda e: e.tensor_tensor(out=gm[:, :NB], in0=gm[:, :NB], in1=eln[:, qt, :], op=ALU.add), reads=(gm, eln), writes=(gm,))
            m = mx.next()
            kb.op('dve', lambda e: e.max(out=m[:, :], in_=gm[:, :]), reads=(gm,), writes=(m,))
            s = sl.next()
            kk = min(c['MTOPK'], NB) - 1
            kb.op('dve', lambda e: e.scalar_tensor_tensor(out=s[:, :], in0=gm[:, :NB], scalar=m[:, kk:kk + 1], in1=el[:, qt, :],
                                                          op0=ALU.is_ge, op1=ALU.mult), reads=(gm, m, el), writes=(s,))
            t_ps = A['m_ps'].next()
            kb.op('pe', lambda e: e.matmul(t_ps[:NB, :128], lhsT=s[:, :], rhs=C['ident_f32'][:, :], start=True, stop=True),
                  reads=(s, C['ident_f32']), writes=(t_ps,))
            kb.op('act', lambda e: e.activation(out=st[:, qt * 128:(qt + 1) * 128], in_=t_ps[:NB, :128], func=AF.Identity),
                  reads=(t_ps,), writes=(st,))
        attn_head(kb, C, c, [(mqT[h * 128:(h + 1) * 128, :], mkT[h * 128:(h + 1) * 128, :], 128)], mv[:, h * 128:(h + 1) * 128], 128,
                  ocT[h * 128:(h + 1) * 128, :], 128 ** -0.5, moba_sl[h], 'moba', A, selT=st)
    kb.end()


def swa_phase(kb, C, c, ins, l, wqT, wkT, wv, ocT, posf):
    S, WH, WKV, WD = c['S'], c['WH'], c['WKV'], c['WD']
    swa_sl, moba_sl = alibi_slopes(c)
    kb.begin()
    A = attn_bufs(kb, c, 1, [WD], True, posf)
    A['esink'] = kb.sb('esink', [128, WH], F32)
    kb.load('sp', A['esink'], [(A['esink'][:, :], ins['sinks'][l:l + 1, :].to_broadcast([128, WH]))])
    kb.op('act', lambda e: e.activation(out=A['esink'][:, :], in_=A['esink'][:, :], func=AF.Exp), reads=(A['esink'],), writes=(A['esink'],))
    R = WH // WKV
    base = 3 * c['BW']
    for h in range(WH):
        g = h // R
        attn_head(kb, C, c, [(wqT[h * WD:(h + 1) * WD, :], wkT[g * WD:(g + 1) * WD, :], WD)], wv[:, g * WD:(g + 1) * WD], WD,
                  ocT[base + h * WD:base + (h + 1) * WD, :], WD ** -0.5, swa_sl[h], 'swa', A, sinkcol=A['esink'][:, h:h + 1])
    kb.end()


def mla_phase(kb, C, c, ins, l, qlT, kvlT, krT, krswT, ocT, posf):
    S, LH, QL, KVL, RP = c['S'], c['LH'], c['QL'], c['KVL'], c['ROPE']
    qnT = kb.dram([QL, S], BF16)
    kvnT = kb.dram([KVL, S], BF16)
    kb.begin()
    gq = kb.sb('gq', [128, QL // 128], F32)
    gk = kb.sb('gk', [128, KVL // 128], F32)
    kb.load('sp', gq, [(gq[:, :], ins['qn_pk'][l])])
    kb.load('sp', gk, [(gk[:, :], ins['kvn_pk'][l])])
    kb.begin(); rmsnorm_fm(kb, C, qlT, qnT, QL, S, gq, None); kb.end()
    kb.begin(); rmsnorm_fm(kb, C, kvlT, kvnT, KVL, S, gk, None); kb.end()
    kb.end()
    qnopeT = kb.dram([LH * 128, S], BF16)
    qr_raw = kb.dram([LH * RP, S], F32)
    qr_sw = kb.dram([LH * RP, S], F32)
    knopeT = kb.dram([LH * 128, S], BF16)
    vtm = kb.dram([S, LH * 128], BF16)
    wq = ins['wq_perm'][l]
    wkv = ins['wkv_perm'][l]
    n1 = LH * 128
    n2 = LH * RP
    kb.begin(); linear(kb, qnT, QL, S, lambda n0, ns: wq[:, n0:n0 + ns], n1, store_epi(kb, qnopeT, BF16)); kb.end()
    kb.begin(); linear(kb, qnT, QL, S, lambda n0, ns: wq[:, n1 + n0:n1 + n0 + ns], n2, store_epi(kb, qr_raw, F32)); kb.end()
    kb.begin(); linear(kb, qnT, QL, S, lambda n0, ns: wq[:, n1 + n2 + n0:n1 + n2 + n0 + ns], n2, store_epi(kb, qr_sw, F32)); kb.end()
    kb.begin(); linear(kb, kvnT, KVL, S, lambda n0, ns: wkv[:, n0:n0 + ns], n1, store_epi(kb, knopeT, BF16)); kb.end()
    kb.begin(); linear(kb, kvnT, KVL, S, lambda n0, ns: wkv[:, n1 + n0:n1 + n0 + ns], n1, store_epi(kb, vtm, BF16, tm=True), tm=True); kb.end()
    qrT = kb.dram([LH * RP, S], BF16)
    krotT = kb.dram([RP, S], BF16)
    kb.begin()
    pq = kb.sb('pq', [128, S], F32)
    kb.load('sp', pq, [(pq[:, :], posf[0:1, :].to_broadcast([128, S]))])
    rc = kb.sb('ropec', [128, 2], F32)
    kb.load('sp', rc, [(rc[:, :], ins['ropec'])])
    cs = kb.sb('cos', [128, S], F32)
    sn = kb.sb('sin', [128, S], F32)
    kb.op('dve', lambda e: e.tensor_scalar(out=pq[:, :], in0=pq[:, :], scalar1=rc[:, 0:1], scalar2=None, op0=ALU.mult), reads=(pq, rc), writes=(pq,))
    ki = kb.sb('ki', [128, S], I32)
    kf = kb.sb('kf', [128, S], F32)
    yy = kb.sb('yy', [128, S], F32)
    for tb, sh in ((sn, 0.0), (cs, 0.5 * math.pi)):
        kb.op('dve', lambda e: e.tensor_scalar(out=yy[:, :], in0=pq[:, :], scalar1=1.0 / (2 * math.pi), scalar2=sh / (2 * math.pi) + 0.5,
                                               op0=ALU.mult, op1=ALU.add), reads=(pq,), writes=(yy,))
        kb.op('dve', lambda e: e.tensor_copy(out=ki[:, :], in_=yy[:, :]), reads=(yy,), writes=(ki,))
        kb.op('dve', lambda e: e.tensor_copy(out=kf[:, :], in_=ki[:, :]), reads=(ki,), writes=(kf,))
        kb.op('dve', lambda e: e.tensor_tensor(out=yy[:, :], in0=kf[:, :], in1=yy[:, :], op=ALU.is_gt), reads=(kf, yy), writes=(yy,))
        kb.op('dve', lambda e: e.tensor_tensor(out=kf[:, :], in0=kf[:, :], in1=yy[:, :], op=ALU.subtract), reads=(kf, yy), writes=(kf,))
        kb.op('dve', lambda e: e.tensor_scalar(out=tb[:, :], in0=pq[:, :], scalar1=sh, scalar2=None, op0=ALU.add), reads=(pq,), writes=(tb,))
        kb.op('dve', lambda e: e.scalar_tensor_tensor(out=tb[:, :], in0=kf[:, :], scalar=-2 * math.pi, in1=tb[:, :], op0=ALU.mult, op1=ALU.add),
              reads=(kf, tb), writes=(tb,))
        kb.op('act', lambda e: e.activation(out=tb[:, :], in_=tb[:, :], func=AF.Sin), reads=(tb,), writes=(tb,))
    kb.op('dve', lambda e: e.tensor_scalar(out=sn[:, :], in0=sn[:, :], scalar1=rc[:, 1:2], scalar2=None, op0=ALU.mult), reads=(sn, rc), writes=(sn,))
    ra = kb.rot('ra', [128, 2, 512], F32, 2)
    ro = kb.rot('ro', [128, 512], BF16, 2)
    jobs = [(qr_raw[r0:r0 + 128, :], qr_sw[r0:r0 + 128, :], qrT[r0:r0 + 128, :], 128) for r0 in range(0, LH * RP, 128)]
    jobs.append((krT, krswT, krotT, RP))
    for a_ap, b_ap, o_ap, p in jobs:
        for t0 in range(0, S, 512):
            x = ra.next()
            kb.load('sp', x, [(x[:p, 0, :], a_ap[:, t0:t0 + 512]), (x[:p, 1, :], b_ap[:, t0:t0 + 512])])
            kb.op('dve', lambda e: e.tensor_tensor(out=x[:p, 0, :], in0=x[:p, 0, :], in1=cs[:p, t0:t0 + 512], op=ALU.mult), reads=(x, cs), writes=(x,))
            kb.op('dve', lambda e: e.tensor_tensor(out=x[:p, 1, :], in0=x[:p, 1, :], in1=sn[:p, t0:t0 + 512], op=ALU.mult), reads=(x, sn), writes=(x,))
            o = ro.next()
            kb.op('dve', lambda e: e.tensor_tensor(out=o[:p, :], in0=x[:p, 0, :], in1=x[:p, 1, :], op=ALU.add), reads=(x,), writes=(o,))
            kb.store('act', o, [(o_ap[:, t0:t0 + 512], o[:p, :])])
    kb.end()
    kb.begin()
    A = attn_bufs(kb, c, 2, [128, RP], False, posf)
    base = 2 * c['BW']
    for h in range(LH):
        attn_head(kb, C, c, [(qnopeT[h * 128:(h + 1) * 128, :], knopeT[h * 128:(h + 1) * 128, :], 128),
                             (qrT[h * RP:(h + 1) * RP, :], krotT, RP)], vtm[:, h * 128:(h + 1) * 128], 128,
                  ocT[base + h * 128:base + (h + 1) * 128, :], (c['NOPE'] + RP) ** -0.5, None, 'causal', A)
    kb.end()


def ssm_phase(kb, C, c, ins, l, zt, xbcT, dtr, ocT):
    S, SDI, SH, SG, SN, CC = c['S'], c['SDI'], c['SH'], c['SG'], c['SN'], c['CONVC'] // 128
    assert SN == 128 and c['SHD'] == 64
    NXC = SDI // 128
    HPG = SH // SG
    xcT = kb.dram([c['CONVC'], S], F32)
    kb.begin()
    cw = kb.sb('cw', [128, CC, 4], F32)
    cb = kb.sb('cb', [128, CC], F32)
    kb.load('sp', cw, [(cw[:, :, :], ins['conv_w_pk'][l])])
    kb.load('sp', cb, [(cb[:, :], ins['conv_b_pk'][l])])
    TT = 512
    ci = kb.rot('ci', [128, TT + 3], F32, 2)
    ca = kb.rot('ca', [128, TT], F32, 2)
    for cc in range(CC):
        for t0 in range(0, S, TT):
            u = ci.next()
            if t0 == 0:
                kb.op('dve', lambda e: e.memset(u[:, 0:3], 0.0), writes=(u,))
                kb.load('sp', u, [(u[:, 3:], xbcT[cc * 128:(cc + 1) * 128, 0:TT])])
            else:
                kb.load('sp', u, [(u[:, :], xbcT[cc * 128:(cc + 1) * 128, t0 - 3:t0 + TT])])
            a = ca.next()
            kb.op('dve', lambda e: e.tensor_scalar(out=a[:, :], in0=u[:, 0:TT], scalar1=cw[:, cc, 0:1], scalar2=None, op0=ALU.mult),
                  reads=(u, cw), writes=(a,))
            for k in range(1, 4):
                kb.op('dve', lambda e: e.scalar_tensor_tensor(out=a[:, :], in0=u[:, k:k + TT], scalar=cw[:, cc, k:k + 1], in1=a[:, :],
                                                              op0=ALU.mult, op1=ALU.add), reads=(u, cw, a), writes=(a,))
            kb.op('act', lambda e: e.activation(out=a[:, :], in_=a[:, :], func=AF.Silu, bias=cb[:, cc:cc + 1]), reads=(a, cb), writes=(a,))
            kb.store('act', a, [(xcT[cc * 128:(cc + 1) * 128, t0:t0 + TT], a[:, :])])
    kb.end()
    kb.begin()
    tri = C['masks']
    mneg = C['masks']
    ident = C['ident_f32']

    def bc(name, src, n):
        b = kb.sb(name, [128, n], F32)
        kb.load('sp', b, [(b[:, :], src.to_broadcast([128, n]))])
        return b
    dtb = bc('dtb', ins['dt_bias'][l:l + 1, :], SH)
    Abc = bc('Abc', ins['a_log'][l:l + 1, :], SH)
    kb.op('act', lambda e: e.activation(out=Abc[:, :], in_=Abc[:, :], func=AF.Exp), reads=(Abc,), writes=(Abc,))
    kb.op('dve', lambda e: e.tensor_scalar(out=Abc[:, :], in0=Abc[:, :], scalar1=-1.0, scalar2=None, op0=ALU.mult), reads=(Abc,), writes=(Abc,))
    Dbc = bc('Dbc', ins['d_skip'][l:l + 1, :], SH)
    nwb = bc('nwb', ins['ssm_norm'][l:l + 1, :], SDI)
    state = kb.sb('state', [128, SDI], F32)
    kb.op('dve', lambda e: e.memset(state[:, :], 0.0), writes=(state,))
    xcs = kb.rot('xc', [128, CC, 128], F32, 2)
    zs = kb.rot('z', [128, SDI], F32, 2)
    dts = kb.rot('dtr', [128, SH], F32, 2)
    pA = kb.rot('pA', [128, 512], F32, 3, psum=True)
    yps = [kb.ps('yps%d' % i, [128, 512], F32) for i in range((SDI + 511) // 512)]
    sps = [kb.ps('sps%d' % i, [128, 512], F32) for i in range((SDI + 511) // 512)]
    sm = {n: kb.sb(n, [128, SH], F32) for n in ('dt', 'a', 'acs', 'dec', 'cdec', 'tmp')}
    xs = kb.sb('xs', [128, SDI], F32)
    xdt = kb.sb('xdt', [128, SDI], F32)
    xdt_bf = kb.sb('xdtb', [128, SDI], BF16)
    xdec_bf = kb.sb('xdec', [128, SDI], BF16)
    st_bf = kb.sb('stb', [128, SDI], BF16)
    btm = kb.sb('btm', [128, SG, 128], BF16)
    BT = kb.sb('BT', [128, SG, 128], BF16)
    CT = kb.sb('CT', [128, SG, 128], BF16)
    gt = kb.sb('gt', [128, SG, 128], F32)
    arep = kb.rot('arep', [128, 128], F32, 2)
    dif = kb.rot('dif', [128, 128], F32, 2)
    edec = kb.rot('edec', [128, 128], F32, 2)
    MT = kb.rot('MT', [128, 128], BF16, 2)
    Cd = kb.rot('Cd', [128, 128], BF16, 2)
    ysb = kb.sb('ysb', [128, SDI], F32)
    gsb = kb.sb('gsb', [128, SDI], F32)
    ssq = kb.sb('ssq', [128, SG], F32)
    otb = kb.rot('otb', [128, NXC, 128], BF16, 2)
    v3 = lambda b: b[:, :].rearrange("p (h d) -> p h d", d=64)
    b3 = lambda b: b[:, :].unsqueeze(2).to_broadcast([128, SH, 64])
    GW = HPG * 64
    for ch in range(S // 128):
        t0 = ch * 128
        xc = xcs.next()
        kb.load('sp', xc, [(xc[:, :, :], xcT[:, t0:t0 + 128].rearrange("(cc p) t -> p cc t", p=128))])
        z = zs.next()
        kb.load('sp', z, [(z[:, :], zt[t0:t0 + 128, :])])
        dr = dts.next()
        kb.load('sp', dr, [(dr[:, :], dtr[t0:t0 + 128, :])])
        dt, a, acs, dec, cdec, tmp = (sm[n] for n in ('dt', 'a', 'acs', 'dec', 'cdec', 'tmp'))
        kb.op('dve', lambda e: e.tensor_tensor(out=dt[:, :], in0=dr[:, :], in1=dtb[:, :], op=ALU.add), reads=(dr, dtb), writes=(dt,))
        kb.op('act', lambda e: e.activation(out=dt[:, :], in_=dt[:, :], func=AF.Exp), reads=(dt,), writes=(dt,))
        kb.op('dve', lambda e: e.tensor_scalar(out=dt[:, :], in0=dt[:, :], scalar1=1.0, scalar2=None, op0=ALU.add), reads=(dt,), writes=(dt,))
        kb.op('act', lambda e: e.activation(out=dt[:, :], in_=dt[:, :], func=AF.Ln), reads=(dt,), writes=(dt,))
        kb.op('dve', lambda e: e.tensor_tensor(out=a[:, :], in0=dt[:, :], in1=Abc[:, :], op=ALU.mult), reads=(dt, Abc), writes=(a,))
        p1 = pA.next()
        kb.op('pe', lambda e: e.matmul(p1[:, 0:SH], lhsT=tri[:, 0, 0:128], rhs=a[:, :], start=True, stop=True), reads=(tri, a), writes=(p1,))
        kb.op('pe', lambda e: e.matmul(p1[:, 256:256 + SH], lhsT=C['ones_f32'][:, :], rhs=a[:, :], start=True, stop=True),
              reads=(C['ones_f32'], a), writes=(p1,))
        kb.op('act', lambda e: e.activation(out=acs[:, :], in_=p1[:, 0:SH], func=AF.Identity), reads=(p1,), writes=(acs,))
        kb.op('dve', lambda e: e.tensor_tensor(out=tmp[:, :], in0=p1[:, 256:256 + SH], in1=acs[:, :], op=ALU.subtract), reads=(p1, acs), writes=(tmp,))
        kb.op('act', lambda e: e.activation(out=dec[:, :], in_=tmp[:, :], func=AF.Exp), reads=(tmp,), writes=(dec,))
        kb.op('act', lambda e: e.activation(out=cdec[:, :], in_=p1[:, 256:256 + SH], func=AF.Exp), reads=(p1,), writes=(cdec,))
        for j0 in range(0, NXC, 4):
            pt = pA.next()
            nj = min(4, NXC - j0)
            for j in range(j0, j0 + nj):
                kb.op('pe', lambda e: e.matmul(pt[:, (j - j0) * 128:(j - j0 + 1) * 128], lhsT=xc[:, j, :], rhs=ident[:, :], start=True, stop=True),
                      reads=(xc, ident), writes=(pt,))
            kb.op('act', lambda e: e.activation(out=xs[:, j0 * 128:(j0 + nj) * 128], in_=pt[:, :nj * 128], func=AF.Identity), reads=(pt,), writes=(xs,))
        pt = pA.next()
        for g in range(SG):
            kb.op('pe', lambda e: e.matmul(pt[:, g * 128:(g + 1) * 128], lhsT=xc[:, NXC + g, :], rhs=ident[:, :], start=True, stop=True),
                  reads=(xc, ident), writes=(pt,))
        kb.op('act', lambda e: e.activation(out=btm[:, :, :].rearrange("p g n -> p (g n)"), in_=pt[:, :SG * 128], func=AF.Identity), reads=(pt,), writes=(btm,))
        kb.op('dve', lambda e: e.tensor_tensor(out=v3(xdt), in0=v3(xs), in1=b3(dt), op=ALU.mult), reads=(xs, dt), writes=(xdt,))
        kb.op('act', lambda e: e.activation(out=xdt_bf[:, :], in_=xdt[:, :], func=AF.Identity), reads=(xdt,), writes=(xdt_bf,))
        kb.op('dve', lambda e: e.tensor_tensor(out=v3(xdec_bf), in0=v3(xdt), in1=b3(dec), op=ALU.mult), reads=(xdt, dec), writes=(xdec_bf,))
        kb.op('act', lambda e: e.activation(out=BT[:, :, :], in_=xc[:, NXC:NXC + SG, :], func=AF.Identity), reads=(xc,), writes=(BT,))
        kb.op('act', lambda e: e.activation(out=CT[:, :, :], in_=xc[:, NXC + SG:NXC + 2 * SG, :], func=AF.Identity), reads=(xc,), writes=(CT,))
        pg = pA.next()
        for g in range(SG):
            kb.op('pe', lambda e: e.matmul(pg[:, g * 128:(g + 1) * 128], lhsT=BT[:, g, :], rhs=CT[:, g, :], start=True, stop=True),
                  reads=(BT, CT), writes=(pg,))
        kb.op('act', lambda e: e.activation(out=gt[:, :, :].rearrange("p g n -> p (g n)"), in_=pg[:, :SG * 128], func=AF.Identity), reads=(pg,), writes=(gt,))
        kb.op('act', lambda e: e.activation(out=st_bf[:, :], in_=state[:, :], func=AF.Identity), reads=(state,), writes=(st_bf,))
        for h in range(SH):
            g = h // HPG
            ar = arep.next()
            kb.op('dve', lambda e: e.tensor_copy(out=ar[:, :], in_=a[:, h:h + 1].to_broadcast([128, 128])), reads=(a,), writes=(ar,))
            pb = pA.next()
            kb.op('pe', lambda e: e.matmul(pb[:, 0:128], lhsT=ar[:, :], rhs=tri[:, 0, 0:128], start=True, stop=True), reads=(ar, tri), writes=(pb,))
            df = dif.next()
            kb.op('dve', lambda e: e.scalar_tensor_tensor(out=df[:, :], in0=pb[:, 0:128], scalar=acs[:, h:h + 1], in1=mneg[:, 3, 0:128],
                                                          op0=ALU.subtract, op1=ALU.add), reads=(pb, acs, mneg), writes=(df,))
            kb.op('act', lambda e: e.activation(out=df[:, :], in_=df[:, :], func=AF.Exp), reads=(df,), writes=(df,))
            mt = MT.next()
            kb.op('dve', lambda e: e.tensor_tensor(out=mt[:, :], in0=df[:, :], in1=gt[:, g, :], op=ALU.mult), reads=(df, gt), writes=(mt,))
            ed = edec.next()
            kb.op('act', lambda e: e.activation(out=ed[:, :], in_=pb[:, 0:128], func=AF.Exp), reads=(pb,), writes=(ed,))
            cd = Cd.next()
            kb.op('dve', lambda e: e.tensor_tensor(out=cd[:, :], in0=xc[:, NXC + SG + g, :], in1=ed[:, :], op=ALU.mult), reads=(xc, ed), writes=(cd,))
            yp = yps[(h * 64) // 512]
            yc = (h * 64) % 512
            kb.op('pe', lambda e: e.matmul(yp[:, yc:yc + 64], lhsT=mt[:, :], rhs=xdt_bf[:, h * 64:(h + 1) * 64], start=True, stop=False),
                  reads=(mt, xdt_bf), writes=(yp,))
            kb.op('pe', lambda e: e.matmul(yp[:, yc:yc + 64], lhsT=cd[:, :], rhs=st_bf[:, h * 64:(h + 1) * 64], start=False, stop=True),
                  reads=(cd, st_bf), writes=(yp,))
        for g in range(SG):
            sp_ = sps[(g * GW) // 512]
            sc = (g * GW) % 512
            kb.op('pe', lambda e: e.matmul(sp_[:, sc:sc + GW], lhsT=btm[:, g, :], rhs=xdec_bf[:, g * GW:(g + 1) * GW], start=True, stop=True),
                  reads=(btm, xdec_bf), writes=(sp_,))
        kb.op('dve', lambda e: e.tensor_tensor(out=v3(state), in0=v3(state), in1=b3(cdec), op=ALU.mult), reads=(state, cdec), writes=(state,))
        for i, sp_ in enumerate(sps):
            w = min(512, SDI - i * 512)
            kb.op('dve', lambda e: e.tensor_tensor(out=state[:, i * 512:i * 512 + w], in0=state[:, i * 512:i * 512 + w], in1=sp_[:, :w], op=ALU.add),
                  reads=(state, sp_), writes=(state,))
        kb.op('dve', lambda e: e.tensor_tensor(out=v3(ysb), in0=v3(xs), in1=b3(Dbc), op=ALU.mult), reads=(xs, Dbc), writes=(ysb,))
        for i, yp in enumerate(yps):
            w = min(512, SDI - i * 512)
            kb.op('dve', lambda e: e.tensor_tensor(out=ysb[:, i * 512:i * 512 + w], in0=ysb[:, i * 512:i * 512 + w], in1=yp[:, :w], op=ALU.add),
                  reads=(ysb, yp), writes=(ysb,))
        kb.op('act', lambda e: e.activation(out=z[:, :], in_=z[:, :], func=AF.Silu), reads=(z,), writes=(z,))
        kb.op('dve', lambda e: e.tensor_tensor(out=gsb[:, :], in0=ysb[:, :], in1=z[:, :], op=ALU.mult), reads=(ysb, z), writes=(gsb,))
        kb.op('dve', lambda e: e.tensor_tensor(out=ysb[:, :], in0=gsb[:, :], in1=gsb[:, :], op=ALU.mult), reads=(gsb,), writes=(ysb,))
        kb.op('dve', lambda e: e.tensor_reduce(out=ssq[:, :], in_=ysb[:, :].rearrange("p (g k) -> p g k", g=SG), axis=AX.X, op=ALU.add),
              reads=(ysb,), writes=(ssq,))
        kb.op('dve', lambda e: e.tensor_scalar(out=ssq[:, :], in0=ssq[:, :], scalar1=float(SG) / SDI, scalar2=1e-6, op0=ALU.mult, op1=ALU.add),
              reads=(ssq,), writes=(ssq,))
        kb.op('act', lambda e: e.activation(out=ssq[:, :], in_=ssq[:, :], func=AF.Ln), reads=(ssq,), writes=(ssq,))
        kb.op('act', lambda e: e.activation(out=ssq[:, :], in_=ssq[:, :], func=AF.Exp, scale=-0.5), reads=(ssq,), writes=(ssq,))
        kb.op('dve', lambda e: e.tensor_tensor(out=gsb[:, :].rearrange("p (g k) -> p g k", g=SG), in0=gsb[:, :].rearrange("p (g k) -> p g k", g=SG),
                                               in1=ssq[:, :].unsqueeze(2).to_broadcast([128, SG, SDI // SG]), op=ALU.mult), reads=(gsb, ssq), writes=(gsb,))
        kb.op('dve', lambda e: e.tensor_tensor(out=gsb[:, :], in0=gsb[:, :], in1=nwb[:, :], op=ALU.mult), reads=(gsb, nwb), writes=(gsb,))
        ot = otb.next()
        for j0 in range(0, NXC, 4):
            pt = pA.next()
            nj = min(4, NXC - j0)
            for j in range(j0, j0 + nj):
                kb.op('pe', lambda e: e.matmul(pt[:, (j - j0) * 128:(j - j0 + 1) * 128], lhsT=gsb[:, j * 128:(j + 1) * 128], rhs=ident[:, :], start=True, stop=True),
                      reads=(gsb, ident), writes=(pt,))
            kb.op('act', lambda e: e.activation(out=ot[:, j0:j0 + nj, :].rearrange("p j t -> p (j t)"), in_=pt[:, :nj * 128], func=AF.Identity),
                  reads=(pt,), writes=(ot,))
        kb.store('act', ot, [(ocT[c['BW']:c['BW'] + SDI, t0:t0 + 128].rearrange("(j p) t -> p j t", p=128), ot[:, :, :])])
    kb.end()


def win_phase(kb, C, c, ins, l, hT):
    D, S = c['D'], c['S']
    off = c['IN_OFF']
    w = ins['w_in'][l]
    o = {}

    def seg(i, name, dt, tm=False, func=None, wsrc=None, woff=None, n=None):
        n_ = n if n is not None else off[i + 1] - off[i]
        dst = kb.dram([S, n_] if tm else [n_, S], dt)
        ws = w if wsrc is None else wsrc
        w0 = off[i] if woff is None else woff
        kb.begin()
        linear(kb, hT, D, S, lambda n0, ns: ws[:, w0 + n0:w0 + n0 + ns], n_, store_epi(kb, dst, dt, func=func, tm=tm), tm=tm)
        kb.end()
        o[name] = dst
    M = c['MH'] * 128
    seg(0, 'mqT', BF16, n=M)
    seg(0, 'mkT', BF16, woff=off[0] + M, n=M)
    seg(0, 'mv', BF16, tm=True, woff=off[0] + 2 * M, n=M)
    seg(1, 'z', F32, tm=True)
    seg(2, 'xbcT', F32)
    seg(3, 'dtr', F32, tm=True)
    seg(4, 'qlT', F32)
    seg(5, 'kvlT', F32)
    seg(6, 'krT', F32)
    seg(6, 'krswT', F32, wsrc=ins['w_krsw'][l], woff=0)
    seg(7, 'wqT', BF16)
    seg(8, 'wkT', BF16)
    seg(9, 'wv', BF16, tm=True)
    seg(10, 'gatesT', F32, func=AF.Sigmoid)
    return o


def merge_phase(kb, C, c, ins, l, ocT, gatesT, xT, g1, x1T):
    D, S, BW = c['D'], c['S'], c['BW']
    parts = [kb.dram([D, S], F32) for _ in range(4)]
    for r in range(4):
        kb.begin()
        gts = kb.rot('gt', [128, 512], F32, 2)
        obs = kb.rot('mo', [128, 512], F32, 2)

        def epi(n0, ns, t0, ts, ps, r=r):
            g = gts.next()
            kb.load('sp', g, [(g[:ns, :ts], gatesT[r * D + n0:r * D + n0 + ns, t0:t0 + ts])])
            ob = obs.next()
            kb.op('dve', lambda e: e.tensor_tensor(out=ob[:ns, :ts], in0=ps[:ns, :ts], in1=g[:ns, :ts], op=ALU.mult), reads=(ps, g), writes=(ob,))
            kb.store('act', ob, [(parts[r][n0:n0 + ns, t0:t0 + ts], ob[:ns, :ts])])
        wb = ins['w_branch'][l, r]
        linear(kb, ocT[r * BW:(r + 1) * BW, :], BW, S, lambda n0, ns: wb[:, n0:n0 + ns], D, epi)
        kb.end()
    mT = kb.dram([D, S], BF16)
    kb.begin(); ew_combine(kb, parts, mT, D, S, BF16); kb.end()
    kb.begin()
    xts = kb.rot('xr', [128, 512], F32, 2)
    obs = kb.rot('xo', [128, 512], F32, 2)

    def epi2(n0, ns, t0, ts, ps):
        x = xts.next()
        kb.load('sp', x, [(x[:ns, :ts], xT[n0:n0 + ns, t0:t0 + ts])])
        ob = obs.next()
        fc = n0 // 128
        kb.op('dve', lambda e: e.scalar_tensor_tensor(out=ob[:ns, :ts], in0=ps[:ns, :ts], scalar=g1[:ns, fc:fc + 1], in1=x[:ns, :ts],
                                                      op0=ALU.mult, op1=ALU.add), reads=(ps, g1, x), writes=(ob,))
        kb.store('act', ob, [(x1T[n0:n0 + ns, t0:t0 + ts], ob[:ns, :ts])])
    wo = ins['w_out'][l]
    linear(kb, mT, D, S, lambda n0, ns: wo[:, n0:n0 + ns], D, epi2)
    kb.end()


def moe_phase(kb, C, c, ins, l, h2T, x1T, g2, x2T):
    D, S, NG, EPG, NE, EH = c['D'], c['S'], c['NG'], c['EPG'], c['NE'], c['EH']
    NR = NG + NE
    wTd = kb.dram([NE, S], F32)
    kb.begin()
    brt = kb.sb('brt', [128, NR], F32)
    kb.load('sp', brt, [(brt[:, :], ins['b_rt'][l:l + 1, :].to_broadcast([128, NR]))])
    WT = kb.sb('WT', [NE, S], F32)
    lg = kb.rot('lg', [128, NR], F32, 2)
    s1 = kb.rot('s1', [128, 8], F32, 2)
    mx8 = kb.rot('mx8', [128, 8], F32, 2)
    pen = kb.rot('pen', [128, NG], F32, 2)
    lm = kb.rot('lm', [128, NE], F32, 2)
    sel = kb.rot('sel', [128, NE], F32, 2)
    ex = kb.rot('exr', [128, NE], F32, 2)
    tps = kb.rot('tps', [128, 512], F32, 2, psum=True)
    junk = kb.rot('junk', [128, NG], F32, 2)

    def epi(n0, ns, t0, ts, ps):
        L_ = lg.next()
        kb.op('dve', lambda e: e.tensor_tensor(out=L_[:, :], in0=ps[:, :NR], in1=brt[:, :], op=ALU.add), reads=(ps, brt), writes=(L_,))
        s = s1.next()
        kb.op('dve', lambda e: e.tensor_reduce(out=s[:, 0:1], in_=L_[:, 0:NG], axis=AX.X, op=ALU.max), reads=(L_,), writes=(s,))
        kb.op('dve', lambda e: e.tensor_scalar(out=s[:, 1:2], in0=s[:, 0:1], scalar1=-1.0, scalar2=None, op0=ALU.mult), reads=(s,), writes=(s,))
        jk = junk.next()
        kb.op('act', lambda e: e.activation(out=jk[:, :], in_=L_[:, 0:NG], func=AF.Exp, bias=s[:, 1:2], accum_out=s[:, 2:3]), reads=(L_, s), writes=(jk, s))
        p = pen.next()
        kb.op('dve', lambda e: e.tensor_scalar(out=p[:, :], in0=L_[:, 0:NG], scalar1=s[:, 0:1], scalar2=None, op0=ALU.is_ge), reads=(L_, s), writes=(p,))
        kb.op('dve', lambda e: e.tensor_scalar(out=p[:, :], in0=p[:, :], scalar1=-1.0, scalar2=1e30, op0=ALU.add, op1=ALU.mult), reads=(p,), writes=(p,))
        m = lm.next()
        kb.op('dve', lambda e: e.tensor_tensor(out=m[:, :].rearrange("p (g k) -> p g k", g=NG), in0=L_[:, NG:NR].rearrange("p (g k) -> p g k", g=NG),
                                               in1=p[:, :].unsqueeze(2).to_broadcast([128, NG, EPG]), op=ALU.add), reads=(L_, p), writes=(m,))
        x8 = mx8.next()
        kb.op('dve', lambda e: e.max(out=x8[:, :], in_=m[:, :]), reads=(m,), writes=(x8,))
        sl = sel.next()
        kb.op('dve', lambda e: e.tensor_scalar(out=sl[:, :], in0=m[:, :], scalar1=x8[:, 1:2], scalar2=None, op0=ALU.is_ge), reads=(m, x8), writes=(sl,))
        kb.op('dve', lambda e: e.tensor_scalar(out=s[:, 4:5], in0=x8[:, 0:1], scalar1=-1.0, scalar2=None, op0=ALU.mult), reads=(x8, s), writes=(s,))
        e_ = ex.next()
        kb.op('act', lambda e: e.activation(out=e_[:, :], in_=m[:, :], func=AF.Exp, bias=s[:, 4:5]), reads=(m, s), writes=(e_,))
        kb.op('act', lambda e: e.activation(out=s[:, 5:6], in_=x8[:, 1:2], func=AF.Exp, bias=s[:, 4:5]), reads=(x8, s), writes=(s,))
        kb.op('dve', lambda e: e.scalar_tensor_tensor(out=s[:, 5:6], in0=s[:, 5:6], scalar=1.0, in1=s[:, 2:3], op0=ALU.add, op1=ALU.mult), reads=(s,), writes=(s,))
        kb.op('dve', lambda e: e.reciprocal(out=s[:, 3:4], in_=s[:, 5:6]), reads=(s,), writes=(s,))
        kb.op('dve', lambda e: e.scalar_tensor_tensor(out=e_[:, :], in0=e_[:, :], scalar=s[:, 3:4], in1=sl[:, :], op0=ALU.mult, op1=ALU.mult),
              reads=(e_, s, sl), writes=(e_,))
        tp = tps.next()
        kb.op('pe', lambda e: e.matmul(tp[:NE, :128], lhsT=e_[:, :], rhs=C['ident_f32'][:, :], start=True, stop=True), reads=(e_, C['ident_f32']), writes=(tp,))
        kb.op('act', lambda e: e.activation(out=WT[:, t0:t0 + 128], in_=tp[:NE, :128], func=AF.Identity), reads=(tp,), writes=(WT,))
    wr = ins['w_rt'][l]
    linear(kb, h2T, D, S, lambda n0, ns: wr[:, n0:n0 + ns], NR, epi, tm=True)
    kb.store('act', WT, [(wTd[:, :], WT[:, :])])
    kb.end()
    sgT = kb.dram([EH, S], BF16)
    hidT = kb.dram([NE * EH, S], BF16)
    for ei in range(NE):
        wg = ins['ewg'][l, ei]
        wu = ins['ewu'][l, ei]
        kb.begin(); linear(kb, h2T, D, S, lambda n0, ns: wg[:, n0:n0 + ns], EH, store_epi(kb, sgT, BF16, func=AF.Silu)); kb.end()
        kb.begin()
        sgs = kb.rot('sg', [128, 512], BF16, 2)
        wbs = kb.rot('wb', [128, 512], F32, 2)
        hos = kb.rot('ho', [128, 512], BF16, 2)
        tus = kb.rot('tu', [128, 512], F32, 2)
        cache = {}

        def epi_u(n0, ns, t0, ts, ps, ei=ei):
            if cache.get('t0') != t0:
                wb = wbs.next()
                kb.load('sp', wb, [(wb[:, :ts], wTd[ei:ei + 1, t0:t0 + ts].to_broadcast([128, ts]))])
                cache['t0'] = t0
                cache['wb'] = wb
            wb = cache['wb']
            sg = sgs.next()
            kb.load('sp', sg, [(sg[:ns, :ts], sgT[n0:n0 + ns, t0:t0 + ts])])
            ho = hos.next()
            tmpb = tus.next()
            kb.op('dve', lambda e: e.tensor_tensor(out=tmpb[:ns, :ts], in0=ps[:ns, :ts], in1=wb[:ns, :ts], op=ALU.mult), reads=(ps, wb), writes=(tmpb,))
            kb.op('dve', lambda e: e.tensor_tensor(out=ho[:ns, :ts], in0=tmpb[:ns, :ts], in1=sg[:ns, :ts], op=ALU.mult), reads=(tmpb, sg), writes=(ho,))
            kb.store('act', ho, [(hidT[ei * EH + n0:ei * EH + n0 + ns, t0:t0 + ts], ho[:ns, :ts])])
        linear(kb, h2T, D, S, lambda n0, ns: wu[:, n0:n0 + ns], EH, epi_u)
        kb.end()
    KG = EPG * EH
    parts = [kb.dram([D, S], F32) for _ in range(NG)]
    wd = ins['ewd'][l].rearrange("e h d -> (e h) d")
    for g in range(NG):
        kb.begin()
        linear(kb, hidT[g * KG:(g + 1) * KG, :], KG, S, lambda n0, ns, g=g: wd[g * KG:(g + 1) * KG, n0:n0 + ns], D, store_epi(kb, parts[g], F32),
               NW=512 if KG <= 4096 else 256)
        kb.end()
    kb.begin(); ew_combine(kb, parts, x2T, D, S, F32, gate=g2, base=x1T); kb.end()


def in_specs(c):
    D, S, L = c['D'], c['S'], c['L']
    DC = D // 128
    NB = S // c['MBLK']
    CC = c['CONVC'] // 128
    sp = [
        ('xT', [D, S], F32), ('cT', [D, 1], F32), ('pos', [1, S], I32),
        ('ada_w', [L, D, 6 * D], F32), ('ada_b_pk', [L, 128, 6 * DC], F32),
        ('nm_pk', [L, 128, DC], F32), ('nf_pk', [L, 128, DC], F32), ('fin_pk', [128, DC], F32),
        ('w_in', [L, D, c['N_IN']], F32), ('w_krsw', [L, D, c['ROPE']], F32),
        ('conv_w_pk', [L, 128, CC, 4], F32), ('conv_b_pk', [L, 128, CC], F32),
        ('dt_bias', [L, c['SH']], F32), ('a_log', [L, c['SH']], F32), ('d_skip', [L, c['SH']], F32), ('ssm_norm', [L, c['SDI']], F32),
        ('qn_pk', [L, 128, c['QL'] // 128], F32), ('kvn_pk', [L, 128, c['KVL'] // 128], F32),
        ('wq_perm', [L, c['QL'], c['LH'] * (128 + 2 * c['ROPE'])], F32), ('wkv_perm', [L, c['KVL'], c['LH'] * 256], F32),
        ('sinks', [L, c['WH']], F32), ('w_branch', [L, 4, c['BW'], D], F32), ('w_out', [L, D, D], F32),
        ('w_rt', [L, D, c['NG'] + c['NE']], F32), ('b_rt', [L, c['NG'] + c['NE']], F32),
        ('ewg', [L, c['NE'], D, c['EH']], F32), ('ewu', [L, c['NE'], D, c['EH']], F32), ('ewd', [L, c['NE'], c['EH'], D], F32),
        ('masks', [128, 4, 256], F32), ('ident', [128, 128], F32), ('elig', [S, NB], F32), ('esel', [NB, NB * 128], F32),
        ('ropec', [128, 2], F32),
    ]
    return sp


def build(cfg):
    c = derive(cfg)
    D, S, L = c['D'], c['S'], c['L']
    DC = D // 128
    NB = S // c['MBLK']
    nc = bass.Bass("TRN2", target_bir_lowering=False)
    ins = {n: nc.dram_tensor(n, sh, dt, kind="ExternalInput").ap() for n, sh, dt in in_specs(c)}
    outT = nc.dram_tensor("outT", [D, S], F32, kind="ExternalOutput").ap()
    kb = KB(nc)
    kb.begin()
    C = {}
    C['ones_f32'] = kb.sb('ones', [128, 128], F32)
    kb.op('dve', lambda e: e.memset(C['ones_f32'][:, :], 1.0), writes=(C['ones_f32'],))
    C['ones_bf'] = kb.sb('onesb', [128, 128], BF16)
    kb.op('dve', lambda e: e.memset(C['ones_bf'][:, :], 1.0), writes=(C['ones_bf'],))
    C['ident_f32'] = kb.sb('ident', [128, 128], F32)
    kb.load('sp', C['ident_f32'], [(C['ident_f32'][:, :], ins['ident'])])
    C['masks'] = kb.sb('masks', [128, 4, 256], F32)
    kb.load('sp', C['masks'], [(C['masks'][:, :, :], ins['masks'])])
    C['esel'] = kb.sb('esel', [NB, NB, 128], BF16)
    kb.load('pool', C['esel'], [(C['esel'][:, :, :], ins['esel'].rearrange("k (n m) -> k n m", m=128))])
    posf = kb.dram([1, S], F32)
    kb.begin()
    pi = kb.sb('posi', [1, S], I32)
    pf = kb.sb('posf', [1, S], F32)
    kb.load('sp', pi, [(pi[:, :], ins['pos'])])
    kb.op('dve', lambda e: e.tensor_copy(out=pf[:, :], in_=pi[:, :]), reads=(pi,), writes=(pf,))
    kb.store('act', pf, [(posf[:, :], pf[:, :])])
    kb.end()
    mods = []
    for l in range(L):
        m = kb.sb('mod%d' % l, [128, 6 * DC], F32)
        ab = kb.sb('adab%d' % l, [128, 6 * DC], F32)
        kb.load('sp', ab, [(ab[:, :], ins['ada_b_pk'][l])])
        kb.begin()

        def epi(n0, ns, t0, ts, ps, m=m, ab=ab):
            j = n0 // 128
            kb.op('dve', lambda e: e.tensor_tensor(out=m[:, j:j + 1], in0=ps[:, 0:1], in1=ab[:, j:j + 1], op=ALU.add), reads=(ps, ab), writes=(m,))
        aw = ins['ada_w'][l]
        linear(kb, ins['cT'], D, 1, lambda n0, ns: aw[:, n0:n0 + ns], 6 * D, epi, xq='pool')
        kb.end()
        nm = kb.sb('nm%d' % l, [128, DC], F32)
        nf = kb.sb('nf%d' % l, [128, DC], F32)
        kb.load('sp', nm, [(nm[:, :], ins['nm_pk'][l])])
        kb.load('sp', nf, [(nf[:, :], ins['nf_pk'][l])])
        gam1 = kb.sb('gam1_%d' % l, [128, DC], F32)
        gam2 = kb.sb('gam2_%d' % l, [128, DC], F32)
        kb.op('dve', lambda e: e.scalar_tensor_tensor(out=gam1[:, :], in0=m[:, DC:2 * DC], scalar=1.0, in1=nm[:, :], op0=ALU.add, op1=ALU.mult),
              reads=(m, nm), writes=(gam1,))
        kb.op('dve', lambda e: e.scalar_tensor_tensor(out=gam2[:, :], in0=m[:, 4 * DC:5 * DC], scalar=1.0, in1=nf[:, :], op0=ALU.add, op1=ALU.mult),
              reads=(m, nf), writes=(gam2,))
        sh1 = kb.sb('sh1_%d' % l, [128, DC], F32)
        g1 = kb.sb('g1_%d' % l, [128, DC], F32)
        sh2 = kb.sb('sh2_%d' % l, [128, DC], F32)
        g2 = kb.sb('g2_%d' % l, [128, DC], F32)
        for dst, k in ((sh1, 0), (g1, 2), (sh2, 3), (g2, 5)):
            kb.op('dve', lambda e: e.tensor_copy(out=dst[:, :], in_=m[:, k * DC:(k + 1) * DC]), reads=(m,), writes=(dst,))
        mods.append(dict(gam1=gam1, sh1=sh1, g1=g1, gam2=gam2, sh2=sh2, g2=g2))
    fin = kb.sb('fin', [128, DC], F32)
    kb.load('sp', fin, [(fin[:, :], ins['fin_pk'])])
    xT = ins['xT']
    for l in range(L):
        md = mods[l]
        hT = kb.dram([D, S], BF16)
        kb.begin(); rmsnorm_fm(kb, C, xT, hT, D, S, md['gam1'], md['sh1']); kb.end()
        o = win_phase(kb, C, c, ins, l, hT)
        ocT = kb.dram([4 * c['BW'], S], BF16)
        moba_phase(kb, C, c, ins, o['mqT'], o['mkT'], o['mv'], ocT, posf)
        ssm_phase(kb, C, c, ins, l, o['z'], o['xbcT'], o['dtr'], ocT)
        mla_phase(kb, C, c, ins, l, o['qlT'], o['kvlT'], o['krT'], o['krswT'], ocT, posf)
        swa_phase(kb, C, c, ins, l, o['wqT'], o['wkT'], o['wv'], ocT, posf)
        x1T = kb.dram([D, S], F32)
        merge_phase(kb, C, c, ins, l, ocT, o['gatesT'], xT, md['g1'], x1T)
        h2T = kb.dram([D, S], BF16)
        kb.begin(); rmsnorm_fm(kb, C, x1T, h2T, D, S, md['gam2'], md['sh2']); kb.end()
        x2T = kb.dram([D, S], F32)
        moe_phase(kb, C, c, ins, l, h2T, x1T, md['g2'], x2T)
        xT = x2T
    kb.begin(); rmsnorm_fm(kb, C, xT, outT, D, S, fin, None, dst_dt=F32); kb.end()
    kb.end()
    return nc


def host_consts(c):
    S = c['S']
    NB = S // c['MBLK']
    p = np.arange(128)[:, None]
    q = np.arange(256)[None, :]
    masks = np.zeros((128, 4, 256), np.float32)
    masks[:, 0] = (p <= q)
    masks[:, 1] = (128 + p <= q)
    masks[:, 2, :128] = (p > q[:, :128])
    masks[:, 3] = np.where(p <= q, 0.0, NEG)
    ident = np.eye(128, dtype=np.float32)
    qb = (np.arange(S) // c['MBLK'])[:, None]
    elig = (np.arange(NB)[None, :] < qb).astype(np.float32)
    esel = np.zeros((NB, NB, 128), np.float32)
    for n in range(NB):
        esel[n, n, :] = 1.0
    half = c['ROPE'] // 2
    inv = (np.float32(10000.0) ** (-np.arange(half, dtype=np.float32) / np.float32(half))).astype(np.float32)
    pp = np.arange(128) % c['ROPE']
    ropec = np.stack([inv[pp % half], np.where(pp < half, -1.0, 1.0)], 1).astype(np.float32)
    return dict(masks=masks, ident=ident, elig=elig, esel=esel.reshape(NB, NB * 128), ropec=ropec)


def pk(v):
    v = np.asarray(v)
    return np.ascontiguousarray(np.swapaxes(v.reshape(v.shape[:-1] + (-1, 128)), -1, -2))


def prep_shared(inp, c):
    L = c['L']
    off = c['IN_OFF']
    half = c['ROPE'] // 2
    LH, RP = c['LH'], c['ROPE']
    w_in = np.asarray(inp['w_in'])
    kr = w_in[:, :, off[6]:off[7]]
    d = {}
    d['ada_w'] = np.asarray(inp['ada_w'])
    d['ada_b_pk'] = pk(inp['ada_b'])
    d['nm_pk'] = pk(inp['norm_mix'])
    d['nf_pk'] = pk(inp['norm_ffn'])
    d['fin_pk'] = pk(inp['final_norm'])
    d['w_in'] = w_in
    d['w_krsw'] = np.ascontiguousarray(np.concatenate([kr[:, :, half:], kr[:, :, :half]], -1))
    CC = c['CONVC'] // 128
    cw = np.asarray(inp['conv_w'])
    d['conv_w_pk'] = np.ascontiguousarray(cw.reshape(L, 4, CC, 128).transpose(0, 3, 2, 1))
    d['conv_b_pk'] = pk(inp['conv_b'])
    for k in ('dt_bias', 'a_log', 'd_skip', 'ssm_norm'):
        d[k] = np.asarray(inp[k])
    d['qn_pk'] = pk(inp['mla_q_norm'])
    d['kvn_pk'] = pk(inp['mla_kv_norm'])
    wq = np.asarray(inp['mla_wq_b']).reshape(L, c['QL'], LH, c['NOPE'] + RP)
    nope = wq[..., :c['NOPE']].reshape(L, c['QL'], -1)
    rope = wq[..., c['NOPE']:]
    rsw = np.concatenate([rope[..., half:], rope[..., :half]], -1)
    d['wq_perm'] = np.ascontiguousarray(np.concatenate([nope, rope.reshape(L, c['QL'], -1), rsw.reshape(L, c['QL'], -1)], -1))
    wkv = np.asarray(inp['mla_wkv_b']).reshape(L, c['KVL'], LH, c['NOPE'] + c['LV'])
    d['wkv_perm'] = np.ascontiguousarray(np.concatenate([wkv[..., :c['NOPE']].reshape(L, c['KVL'], -1),
                                                         wkv[..., c['NOPE']:].reshape(L, c['KVL'], -1)], -1))
    d['sinks'] = np.asarray(inp['swa_sinks'])
    d['w_branch'] = np.asarray(inp['w_branch'])
    d['w_out'] = np.asarray(inp['w_out'])
    d['w_rt'] = np.ascontiguousarray(np.concatenate([inp['router_group_w'], inp['router_w']], -1))
    d['b_rt'] = np.ascontiguousarray(np.concatenate([inp['router_group_b'], inp['router_b']], -1))
    d['ewg'] = np.asarray(inp['exp_w_gate'])
    d['ewu'] = np.asarray(inp['exp_w_up'])
    d['ewd'] = np.asarray(inp['exp_w_down'])
    d.update(host_consts(c))
    return d


_NC_CACHE = {}


def run(inp, cfg):
    c = derive(cfg)
    key = tuple(sorted((k, v) for k, v in cfg.items()))
    if key not in _NC_CACHE:
        _NC_CACHE[key] = build(cfg)
    nc = _NC_CACHE[key]
    shared = prep_shared(inp, c)
    x = np.asarray(inp['x'])
    B = x.shape[0]
    maps = []
    for b in range(B):
        m = dict(shared)
        m['xT'] = np.ascontiguousarray(x[b].T)
        m['cT'] = np.ascontiguousarray(np.asarray(inp['c'])[b][:, None])
        m['pos'] = np.ascontiguousarray(np.asarray(inp['positions'])[b][None, :].astype(np.int32))
        maps.append(m)
    res = run_bass_kernel_spmd(nc, maps, core_ids=list(range(B)))
    out = np.stack([np.ascontiguousarray(res.results[b]['outT'].T) for b in range(B)], 0)
    return out.astype(np.float32)


def kernel(**inputs):
    return run(inputs, FULL)
```

```python
import math
from contextlib import ExitStack
import numpy as np
import concourse.bass as bass
import concourse.mybir as mybir
from concourse.bass_utils import run_bass_kernel_spmd

F32 = mybir.dt.float32
BF16 = mybir.dt.bfloat16
I32 = mybir.dt.int32
AF = mybir.ActivationFunctionType
ALU = mybir.AluOpType
AX = mybir.AxisListType

FULL = dict(D=4096, S=4096, L=2, MH=8, MBLK=256, MTOPK=3, SDI=1024, SHD=64, SG=2, SN=128, SCONV=4,
            LH=8, QL=768, KVL=512, NOPE=128, ROPE=64, LV=128, WH=16, WKV=2, WD=64, WW=128, BW=1024,
            NG=4, EPG=8, EH=512, B=2)
NEG = -30000.0
TNW = 256


def derive(c):
    c = dict(c)
    c['SH'] = c['SDI'] // c['SHD']
    c['CONVC'] = c['SDI'] + 2 * c['SG'] * c['SN']
    c['NE'] = c['NG'] * c['EPG']
    sizes = [3 * c['MH'] * 128, c['SDI'], c['CONVC'], c['SH'], c['QL'], c['KVL'], c['ROPE'],
             c['WH'] * c['WD'], c['WKV'] * c['WD'], c['WKV'] * c['WD'], 4 * c['D']]
    offs = np.concatenate([[0], np.cumsum(sizes)]).tolist()
    c['IN_OFF'] = offs
    c['N_IN'] = offs[-1]
    c['NALIBI'] = c['MH'] + c['WH']
    M = c['MH'] * 128
    segs = [('mqT', offs[0], M), ('mkT', offs[0] + M, M), ('mv', offs[0] + 2 * M, M)]
    for nm, i in (('z', 1), ('xbcT', 2), ('dtr', 3), ('qlT', 4), ('kvlT', 5), ('krT', 6), ('wqT', 7), ('wkT', 8), ('wv', 9), ('gatesT', 10)):
        segs.append((nm, offs[i], sizes[i]))
    go = 0
    tab = {}
    for nm, c0, n in segs:
        tab[nm] = (c0, n, go)
        go += -(-n // TNW)
    c['WSEG'] = tab
    c['WG'] = go
    c['NWE'] = min(TNW, c['EH'])
    return c


class Buf:
    __slots__ = ('t', 'w', 'r', 'ds')

    def __init__(self, t):
        self.t = t
        self.w = None
        self.r = {}
        self.ds = None

    def __getitem__(self, k):
        return self.t[k]


class Rot:
    def __init__(self, bufs):
        self.bufs = bufs
        self.i = 0

    def next(self):
        b = self.bufs[self.i % len(self.bufs)]
        self.i += 1
        return b


class KB:
    def __init__(self, nc):
        self.nc = nc
        self.eng = {'pe': nc.tensor, 'act': nc.scalar, 'dve': nc.vector, 'pool': nc.gpsimd, 'sp': nc.sync}
        self.sems = {}
        self.cnt = {}
        for k in self.eng:
            self.sems[k] = nc.alloc_semaphore('e_' + k)
            self.cnt[k] = 0
        self.seen = {k: {} for k in self.eng}
        self.free_ds = []
        self.nds = 0
        self.scopes = []
        self.uid = 0
        self.ndram = 0

    def begin(self):
        self.scopes.append((ExitStack(), []))

    def end(self):
        self.barrier()
        st, bufs = self.scopes.pop()
        for b in bufs:
            if b.ds is not None:
                self.free_ds.append(b.ds)
                b.ds = None
        st.close()

    def dram(self, shape, dt):
        self.ndram += 1
        return self.nc.dram_tensor(f'scr{self.ndram}', list(shape), dt).ap()

    def sb(self, name, shape, dt):
        self.uid += 1
        st, bufs = self.scopes[-1]
        t = st.enter_context(self.nc.sbuf_tensor(f'{name}_{self.uid}', list(shape), dt))
        b = Buf(t)
        bufs.append(b)
        return b

    def ps(self, name, shape, dt=F32):
        self.uid += 1
        st, bufs = self.scopes[-1]
        t = st.enter_context(self.nc.psum_tensor(f'{name}_{self.uid}', list(shape), dt))
        b = Buf(t)
        bufs.append(b)
        return b

    def rot(self, name, shape, dt, n, psum=False):
        return Rot([(self.ps if psum else self.sb)(f'{name}{i}', shape, dt) for i in range(n)])

    def _dsem(self, b):
        if b.ds is None:
            if self.free_ds:
                b.ds = self.free_ds.pop()
            else:
                k = f'd{self.nds}'
                self.nds += 1
                self.sems[k] = self.nc.alloc_semaphore(k)
                self.cnt[k] = 0
                b.ds = k
        return b.ds

    def _need(self, e, dep, raw=True):
        if dep is None:
            return
        k, v = dep
        if k == e and (e == 'pe' or not raw):
            return
        if self.seen[e].get(k, 0) >= v:
            return
        self.eng[e].wait_ge(self.sems[k], v)
        self.seen[e][k] = v

    def _deps(self, e, reads, writes):
        for b in reads:
            self._need(e, b.w, raw=True)
        for b in writes:
            self._need(e, b.w, raw=False)
            for k, v in b.r.items():
                self._need(e, (k, v), raw=False)

    def op(self, e, ins_fn, reads=(), writes=()):
        self._deps(e, reads, writes)
        ins = ins_fn(self.eng[e])
        self.cnt[e] += 1
        ins.then_inc(self.sems[e], 1)
        v = self.cnt[e]
        for b in reads:
            if b.r.get(e, 0) < v:
                b.r[e] = v
        for b in writes:
            b.w = (e, v)
            b.r = {}
        return ins

    def load(self, q, b, pairs, **kw):
        self._deps(q, (), (b,))
        ds = self._dsem(b)
        for o, i in pairs:
            self.eng[q].dma_start(out=o, in_=i, **{'allow_slow_non_contiguous': True, **kw}).then_inc(self.sems[ds], 16)
            self.cnt[ds] += 16
        b.w = (ds, self.cnt[ds])
        b.r = {}

    def store(self, q, b, pairs, **kw):
        self._deps(q, (b,), ())
        ds = self._dsem(b)
        for o, i in pairs:
            self.eng[q].dma_start(out=o, in_=i, **{'allow_slow_non_contiguous': True, **kw}).then_inc(self.sems[ds], 16)
            self.cnt[ds] += 16
        b.r[ds] = self.cnt[ds]

    def barrier(self):
        for e in self.eng:
            for k, v in self.cnt.items():
                if k != e and v > 0:
                    self._need(e, (k, v), raw=False)


class FnEpi:
    def __init__(self, fn):
        self.fn = fn

    def start(self, n0, ns, e0, es):
        pass

    def tile(self, n0, ns, t0, ts, ps):
        self.fn(n0, ns, t0, ts, ps)

    def finish(self, n0, ns, e0, es):
        pass


EB = 1024
TR_CAP = None


def linear(kb, XT, K, T, Wfn, N, epi, tm=False, NW=None, wq='pool', xq='sp'):
    if not hasattr(epi, 'tile'):
        epi = FnEpi(epi)
    KC = K // 128
    assert K % 128 == 0
    if NW is None:
        NW = 256 if KC >= 32 else 512
    TR = min(T, max(512, (65536 // KC) // 512 * 512))
    if TR_CAP:
        TR = min(TR, TR_CAP)
    wts = kb.rot('lw', [128, KC, NW], BF16, 2)
    xres = kb.sb('lx', [128, KC, TR], BF16)
    pss = kb.rot('lp', [128, 512], F32, 4, psum=True)
    XTv = XT.rearrange("(kc p) t -> p kc t", p=128)
    for tb0 in range(0, T, TR):
        tbs = min(TR, T - tb0)
        kb.load(xq, xres, [(xres[:, :, :tbs], XTv[:, :, tb0:tb0 + tbs])])
        for n0g in range(0, N, NW):
            nsg = min(NW, N - n0g)
            wt = wts.next()
            W = Wfn(n0g, nsg)
            if len(W.shape) == 2:
                W = W.rearrange("(kc p) n -> p kc n", p=128)
            kb.load(wq, wt, [(wt[:, :, :nsg], W)])
            if not tm:
                for c0 in range(0, nsg, 128):
                    cs = min(128, nsg - c0)
                    for e0 in range(tb0, tb0 + tbs, EB):
                        es = min(EB, tb0 + tbs - e0)
                        epi.start(n0g + c0, cs, e0, es)
                        for t0 in range(e0, e0 + es, 512):
                            ts = min(512, e0 + es - t0)
                            ps = pss.next()
                            for kc in range(KC):
                                kb.op('pe', lambda e, kc=kc: e.matmul(ps[:cs, :ts], lhsT=wt[:, kc, c0:c0 + cs],
                                                                      rhs=xres[:, kc, t0 - tb0:t0 - tb0 + ts], start=(kc == 0),
                                                                      stop=(kc == KC - 1)),
                                      reads=(wt, xres), writes=(ps,))
                            epi.tile(n0g + c0, cs, t0, ts, ps)
                        epi.finish(n0g + c0, cs, e0, es)
            else:
                for s0 in range(0, tbs, 128):
                    ss = min(128, tbs - s0)
                    ps = pss.next()
                    for kc in range(KC):
                        kb.op('pe', lambda e, kc=kc: e.matmul(ps[:ss, :nsg], lhsT=xres[:, kc, s0:s0 + ss],
                                                              rhs=wt[:, kc, :nsg], start=(kc == 0),
                                                              stop=(kc == KC - 1)),
                              reads=(wt, xres), writes=(ps,))
                    epi.tile(n0g, nsg, tb0 + s0, ss, ps)


class store_epi:
    def __init__(self, kb, dst, dt, scale=None, func=None, q='act', tm=False, coloff=0):
        self.kb, self.dst, self.dt, self.scale, self.func, self.q, self.tm, self.coloff = kb, dst, dt, scale, func, q, tm, coloff
        self.obs = kb.rot('eo', [128, 512 if tm else EB], dt, 3 if tm else 2)

    def start(self, n0, ns, e0, es):
        self.ob = self.obs.next()
        self.e0 = e0

    def tile(self, n0, ns, t0, ts, ps):
        kb = self.kb
        sc = 1.0 if self.scale is None else self.scale
        fn = self.func or AF.Identity
        if self.tm:
            ob = self.obs.next()
            kb.op('act', lambda e: e.activation(out=ob[:ts, :ns], in_=ps[:ts, :ns], func=fn, scale=sc), reads=(ps,), writes=(ob,))
            kb.store(self.q, ob, [(self.dst[t0:t0 + ts, self.coloff + n0:self.coloff + n0 + ns], ob[:ts, :ns])])
        else:
            ob = self.ob
            o = t0 - self.e0
            kb.op('act', lambda e: e.activation(out=ob[:ns, o:o + ts], in_=ps[:ns, :ts], func=fn, scale=sc), reads=(ps,), writes=(ob,))

    def finish(self, n0, ns, e0, es):
        if not self.tm:
            self.kb.store(self.q, self.ob, [(self.dst[self.coloff + n0:self.coloff + n0 + ns, e0:e0 + es], self.ob[:ns, :es])])


def rmsnorm_fm(kb, C, src, dst, F, T, gam, bet, eps=1e-6, dst_dt=BF16, TT=256):
    FC = F // 128
    xs = kb.rot('nx', [128, FC, TT], F32, 2)
    sq = kb.rot('nq', [128, TT], F32, 2)
    pr = kb.rot('np', [128, 512], F32, 2, psum=True)
    rs = kb.rot('nr', [128, TT], F32, 2)
    ob = kb.rot('no', [128, FC, TT], dst_dt, 2)
    ones = C['ones_f32']
    sv = src.rearrange("(fc p) t -> p fc t", p=128)
    dv = dst.rearrange("(fc p) t -> p fc t", p=128)
    for t0 in range(0, T, TT):
        ts = min(TT, T - t0)
        x = xs.next()
        kb.load('sp', x, [(x[:, :, :ts], sv[:, :, t0:t0 + ts])])
        p = pr.next()
        for fc in range(FC):
            s = sq.next()
            kb.op('act', lambda e: e.activation(out=s[:, :ts], in_=x[:, fc, :ts], func=AF.Square), reads=(x,), writes=(s,))
            kb.op('pe', lambda e: e.matmul(p[:, :ts], lhsT=ones[:, :], rhs=s[:, :ts], start=(fc == 0), stop=(fc == FC - 1)),
                  reads=(s, ones), writes=(p,))
        r = rs.next()
        kb.op('dve', lambda e: e.tensor_scalar(out=r[:, :ts], in0=p[:, :ts], scalar1=1.0 / F, scalar2=eps,
                                               op0=ALU.mult, op1=ALU.add), reads=(p,), writes=(r,))
        kb.op('act', lambda e: e.activation(out=r[:, :ts], in_=r[:, :ts], func=AF.Ln), reads=(r,), writes=(r,))
        kb.op('act', lambda e: e.activation(out=r[:, :ts], in_=r[:, :ts], func=AF.Exp, scale=-0.5), reads=(r,), writes=(r,))
        o = ob.next()
        for fc in range(FC):
            kb.op('dve', lambda e: e.tensor_tensor(out=x[:, fc, :ts], in0=x[:, fc, :ts], in1=r[:, :ts], op=ALU.mult),
                  reads=(x, r), writes=(x,))
            if bet is not None:
                kb.op('act', lambda e: e.activation(out=o[:, fc, :ts], in_=x[:, fc, :ts], func=AF.Identity,
                                                    scale=gam[:, fc:fc + 1], bias=bet[:, fc:fc + 1]),
                      reads=(x, gam, bet), writes=(o,))
            else:
                kb.op('act', lambda e: e.activation(out=o[:, fc, :ts], in_=x[:, fc, :ts], func=AF.Identity,
                                                    scale=gam[:, fc:fc + 1]),
                      reads=(x, gam), writes=(o,))
        kb.store('act', o, [(dv[:, :, t0:t0 + ts], o[:, :, :ts])])


def ew_combine(kb, srcs, dst, F, T, dst_dt, gate=None, base=None, TT=512):
    n = len(srcs) + (1 if base is not None else 0)
    tl = kb.rot('ec', [128, n, TT], F32, 2)
    ob = kb.rot('eo', [128, TT], dst_dt, 2)
    for fc in range(F // 128):
        r0 = fc * 128
        for t0 in range(0, T, TT):
            ts = min(TT, T - t0)
            x = tl.next()
            pairs = [(x[:, i, :ts], s[r0:r0 + 128, t0:t0 + ts]) for i, s in enumerate(srcs)]
            if base is not None:
                pairs.append((x[:, n - 1, :ts], base[r0:r0 + 128, t0:t0 + ts]))
            kb.load('sp', x, pairs)
            for i in range(1, len(srcs)):
                kb.op('dve', lambda e: e.tensor_tensor(out=x[:, 0, :ts], in0=x[:, 0, :ts], in1=x[:, i, :ts], op=ALU.add),
                      reads=(x,), writes=(x,))
            o = ob.next()
            if base is not None:
                kb.op('dve', lambda e: e.scalar_tensor_tensor(out=o[:, :ts], in0=x[:, 0, :ts], scalar=gate[:, fc:fc + 1],
                                                              in1=x[:, n - 1, :ts], op0=ALU.mult, op1=ALU.add),
                      reads=(x, gate), writes=(o,))
            else:
                kb.op('act', lambda e: e.activation(out=o[:, :ts], in_=x[:, 0, :ts], func=AF.Identity), reads=(x,), writes=(o,))
            kb.store('act', o, [(dst[r0:r0 + 128, t0:t0 + ts], o[:, :ts])])


def attn_head(kb, C, c, parts, v_ap, dv, out_ap, scale, slope, mode, A, selT=None, sinkcol=None):
    S = c['S']
    NKT = S // 128
    QT = 128 if mode == 'swa' else 256
    qs, ks = [], []
    for i, (q_ap, k_ap, dk) in enumerate(parts):
        qb = A['q%d' % i].next()
        kb.load('sp', qb, [(qb[:dk, :], q_ap)])
        kbuf = A['k%d' % i].next()
        kb.load('sp', kbuf, [(kbuf[:dk, :], k_ap)])
        qs.append(qb)
        ks.append(kbuf)
    vb = A['v'].next()
    kb.load('sp', vb, [(vb[:, :, :dv], v_ap.rearrange("(t p) d -> p t d", p=128))])
    masks = C['masks']
    for qi in range(S // QT):
        q0 = qi * QT
        if mode == 'swa':
            kts = ([(qi - 1, 2)] if qi >= 1 else []) + [(qi, 0)]
        else:
            kts = []
            for kt in range(2 * qi + 2):
                if kt // 2 == qi:
                    kts.append((kt, kt % 2))
                else:
                    kts.append((kt, 'sel' if mode == 'moba' else None))
        o_ps = A['o_ps'].next()
        d_ps = A['d_ps'].next()
        for j, (kt, mk) in enumerate(kts):
            s_ps = A['s_ps'].next()
            for i, (q_ap, k_ap, dk) in enumerate(parts):
                kb.op('pe', lambda e: e.matmul(s_ps[:, :QT], lhsT=ks[i][:dk, kt * 128:(kt + 1) * 128],
                                               rhs=qs[i][:dk, q0:q0 + QT], start=(i == 0), stop=(i == len(parts) - 1)),
                      reads=(ks[i], qs[i]), writes=(s_ps,))
            ex = A['ex'].next()
            if slope is not None:
                ds = A['ds'].next()
                kb.op('dve', lambda e: e.tensor_scalar(out=ds[:, :QT], in0=A['posq'][:, q0:q0 + QT], scalar1=A['posk'][:, kt:kt + 1],
                                                       scalar2=None, op0=ALU.subtract),
                      reads=(A['posq'], A['posk']), writes=(ds,))
                kb.op('dve', lambda e: e.scalar_tensor_tensor(out=ds[:, :QT], in0=ds[:, :QT], scalar=-1.0, in1=ds[:, :QT],
                                                              op0=ALU.mult, op1=ALU.max), reads=(ds,), writes=(ds,))
                kb.op('dve', lambda e: e.scalar_tensor_tensor(out=ds[:, :QT], in0=ds[:, :QT], scalar=-slope / scale, in1=s_ps[:, :QT],
                                                              op0=ALU.mult, op1=ALU.add), reads=(ds, s_ps), writes=(ds,))
                src = ds
            else:
                src = s_ps
            if mk is None:
                pb = A['pb'].next()
                kb.op('act', lambda e: e.activation(out=pb[:, :QT], in_=src[:, :QT], func=AF.Exp, scale=scale), reads=(src,), writes=(pb,))
            else:
                kb.op('act', lambda e: e.activation(out=ex[:, :QT], in_=src[:, :QT], func=AF.Exp, scale=scale), reads=(src,), writes=(ex,))
                pb = A['pb'].next()
                if mk == 'sel':
                    m_ps = A['m_ps'].next()
                    kb.op('pe', lambda e: e.matmul(m_ps[:, :QT], lhsT=C['esel'][:, kt // 2, :], rhs=selT[:, q0:q0 + QT], start=True, stop=True),
                          reads=(C['esel'], selT), writes=(m_ps,))
                    kb.op('dve', lambda e: e.tensor_tensor(out=pb[:, :QT], in0=ex[:, :QT], in1=m_ps[:, :QT], op=ALU.mult),
                          reads=(ex, m_ps), writes=(pb,))
                else:
                    kb.op('dve', lambda e: e.tensor_tensor(out=pb[:, :QT], in0=ex[:, :QT], in1=masks[:, mk, :QT], op=ALU.mult),
                          reads=(ex, masks), writes=(pb,))
            kb.op('pe', lambda e: e.matmul(o_ps[:dv, :QT], lhsT=vb[:, kt, :dv], rhs=pb[:, :QT], start=(j == 0), stop=(j == len(kts) - 1)),
                  reads=(vb, pb), writes=(o_ps,))
            kb.op('pe', lambda e: e.matmul(d_ps[:, :QT], lhsT=C['ones_bf'][:, :], rhs=pb[:, :QT], start=(j == 0), stop=(j == len(kts) - 1)),
                  reads=(C['ones_bf'], pb), writes=(d_ps,))
        rc = A['rc'].next()
        if sinkcol is not None:
            kb.op('dve', lambda e: e.tensor_scalar(out=rc[:, :QT], in0=d_ps[:, :QT], scalar1=sinkcol, scalar2=None, op0=ALU.add),
                  reads=(d_ps, A['esink']), writes=(rc,))
            kb.op('dve', lambda e: e.reciprocal(out=rc[:, :QT], in_=rc[:, :QT]), reads=(rc,), writes=(rc,))
        else:
            kb.op('dve', lambda e: e.reciprocal(out=rc[:, :QT], in_=d_ps[:, :QT]), reads=(d_ps,), writes=(rc,))
        ob = A['ob'].next()
        kb.op('dve', lambda e: e.tensor_tensor(out=ob[:dv, :QT], in0=o_ps[:dv, :QT], in1=rc[:dv, :QT], op=ALU.mult),
              reads=(o_ps, rc), writes=(ob,))
        kb.store('act', ob, [(out_ap[:, q0:q0 + QT], ob[:dv, :QT])])


def attn_bufs(kb, c, nparts, dks, need_pos, posf):
    S = c['S']
    A = {}
    for i in range(nparts):
        A['q%d' % i] = kb.rot('aq%d' % i, [128, S], BF16, 2)
        A['k%d' % i] = kb.rot('ak%d' % i, [128, S], BF16, 2)
    A['v'] = kb.rot('av', [128, S // 128, 128], BF16, 2)
    A['s_ps'] = kb.rot('as', [128, 512], F32, 2, psum=True)
    A['m_ps'] = kb.rot('am', [128, 512], F32, 2, psum=True)
    A['o_ps'] = kb.rot('ao', [128, 512], F32, 2, psum=True)
    A['d_ps'] = kb.rot('ad', [128, 512], F32, 2, psum=True)
    A['ex'] = kb.rot('aex', [128, 256], F32, 3)
    A['ds'] = kb.rot('ads', [128, 256], F32, 3)
    A['pb'] = kb.rot('apb', [128, 256], BF16, 3)
    A['rc'] = kb.rot('arc', [128, 256], F32, 2)
    A['ob'] = kb.rot('aob', [128, 256], BF16, 2)
    if need_pos:
        A['posq'] = kb.sb('posq', [128, S], F32)
        kb.load('sp', A['posq'], [(A['posq'][:, :], posf[0:1, :].to_broadcast([128, S]))])
        A['posk'] = kb.sb('posk', [128, S // 128], F32)
        kb.load('sp', A['posk'], [(A['posk'][:, :], posf[0, :].rearrange("(t p) -> p t", p=128))], allow_slow_non_contiguous=True)
    return A


def alibi_slopes(c):
    i = np.arange(1, c['NALIBI'] + 1, dtype=np.float32)
    s = np.exp2(np.float32(-8.0) * i / np.float32(c['NALIBI'])).astype(np.float32)
    return [float(v) for v in s[:c['WH']]], [float(v) for v in s[c['WH']:]]


def moba_phase(kb, C, c, ins, mqT, mkT, mv, ocT, posf):
    S, MH, NB = c['S'], c['MH'], c['S'] // c['MBLK']
    swa_sl, moba_sl = alibi_slopes(c)
    kb.begin()
    A = attn_bufs(kb, c, 1, [128], True, posf)
    NT = S // 128
    el = kb.sb('elig', [128, NT, NB], F32)
    kb.load('sp', el, [(el[:, :, :], ins['elig'].rearrange("(t p) n -> p t n", p=128))])
    eln = kb.sb('eln', [128, NT, NB], F32)
    kb.op('dve', lambda e: e.tensor_scalar(out=eln[:, :, :], in0=el[:, :, :], scalar1=-1.0, scalar2=1e30, op0=ALU.add, op1=ALU.mult),
          reads=(el,), writes=(eln,))
    W8 = max(NB, 8)
    gms = kb.rot('gm', [128, W8], F32, 2)
    for b in gms.bufs:
        kb.op('dve', lambda e: e.memset(b[:, :], -1e30), writes=(b,))
    mx = kb.rot('mx', [128, 8], F32, 2)
    sl = kb.rot('sl', [128, NB], F32, 2)
    km = kb.sb('km', [128, NB], F32)
    kmb = kb.sb('kmb', [128, NB], BF16)
    selT = kb.rot('selT', [NB, S], BF16, 2)
    kfull = kb.rot('kfull', [128, S], BF16, 2)
    qfull = kb.rot('qfull', [128, S], BF16, 2)
    for h in range(MH):
        kf = kfull.next()
        qf = qfull.next()
        kb.load('sp', kf, [(kf[:, :], mkT[h * 128:(h + 1) * 128, :])])
        kb.load('sp', qf, [(qf[:, :], mqT[h * 128:(h + 1) * 128, :])])
        kb.op('dve', lambda e: e.tensor_reduce(out=km[:, :], in_=kf[:, :].rearrange("p (n k) -> p n k", n=NB), axis=AX.X, op=ALU.add),
              reads=(kf,), writes=(km,))
        kb.op('act', lambda e: e.activation(out=kmb[:, :], in_=km[:, :], func=AF.Identity, scale=1.0 / c['MBLK']), reads=(km,), writes=(kmb,))
        st = selT.next()
        for qt in range(NT):
            g_ps = A['s_ps'].next()
            kb.op('pe', lambda e: e.matmul(g_ps[:, :NB], lhsT=qf[:, qt * 128:(qt + 1) * 128], rhs=kmb[:, :], start=True, stop=True),
                  reads=(qf, kmb), writes=(g_ps,))
            gm = gms.next()
            kb.op('dve', lambda e: e.tensor_tensor(out=gm[:, :NB], in0=g_ps[:, :NB], in1=el[:, qt, :], op=ALU.mult), reads=(g_ps, el), writes=(gm,))
            kb.op('dve', lambda e: e.tensor_tensor(out=gm[:, :NB], in0=gm[:, :NB], in1=eln[:, qt, :], op=ALU.add), reads=(gm, eln), writes=(gm,))
            m = mx.next()
            kb.op('dve', lambda e: e.max(out=m[:, :], in_=gm[:, :]), reads=(gm,), writes=(m,))
            s = sl.next()
            kk = min(c['MTOPK'], NB) - 1
            kb.op('dve', lambda e: e.scalar_tensor_tensor(out=s[:, :], in0=gm[:, :NB], scalar=m[:, kk:kk + 1], in1=el[:, qt, :],
                                                          op0=ALU.is_ge, op1=ALU.mult), reads=(gm, m, el), writes=(s,))
            t_ps = A['m_ps'].next()
            kb.op('pe', lambda e: e.matmul(t_ps[:NB, :128], lhsT=s[:, :], rhs=C['ident_f32'][:, :], start=True, stop=True),
                  reads=(s, C['ident_f32']), writes=(t_ps,))
            kb.op('act', lambda e: e.activation(out=st[:, qt * 128:(qt + 1) * 128], in_=t_ps[:NB, :128], func=AF.Identity),
                  reads=(t_ps,), writes=(st,))
        attn_head(kb, C, c, [(mqT[h * 128:(h + 1) * 128, :], mkT[h * 128:(h + 1) * 128, :], 128)], mv[:, h * 128:(h + 1) * 128], 128,
                  ocT[h * 128:(h + 1) * 128, :], 128 ** -0.5, moba_sl[h], 'moba', A, selT=st)
    kb.end()


def swa_phase(kb, C, c, ins, l, wqT, wkT, wv, ocT, posf):
    S, WH, WKV, WD = c['S'], c['WH'], c['WKV'], c['WD']
    swa_sl, moba_sl = alibi_slopes(c)
    kb.begin()
    A = attn_bufs(kb, c, 1, [WD], True, posf)
    A['esink'] = kb.sb('esink', [128, WH], F32)
    kb.load('sp', A['esink'], [(A['esink'][:, :], ins['sinks'][l:l + 1, :].to_broadcast([128, WH]))])
    kb.op('act', lambda e: e.activation(out=A['esink'][:, :], in_=A['esink'][:, :], func=AF.Exp), reads=(A['esink'],), writes=(A['esink'],))
    R = WH // WKV
    base = 3 * c['BW']
    for h in range(WH):
        g = h // R
        attn_head(kb, C, c, [(wqT[h * WD:(h + 1) * WD, :], wkT[g * WD:(g + 1) * WD, :], WD)], wv[:, g * WD:(g + 1) * WD], WD,
                  ocT[base + h * WD:base + (h + 1) * WD, :], WD ** -0.5, swa_sl[h], 'swa', A, sinkcol=A['esink'][:, h:h + 1])
    kb.end()


def mla_phase(kb, C, c, ins, l, qlT, kvlT, krT, krswT, ocT, posf):
    S, LH, QL, KVL, RP = c['S'], c['LH'], c['QL'], c['KVL'], c['ROPE']
    qnT = kb.dram([QL, S], BF16)
    kvnT = kb.dram([KVL, S], BF16)
    kb.begin()
    gq = kb.sb('gq', [128, QL // 128], F32)
    gk = kb.sb('gk', [128, KVL // 128], F32)
    kb.load('sp', gq, [(gq[:, :], ins['qn_pk'][l])])
    kb.load('sp', gk, [(gk[:, :], ins['kvn_pk'][l])])
    kb.begin(); rmsnorm_fm(kb, C, qlT, qnT, QL, S, gq, None); kb.end()
    kb.begin(); rmsnorm_fm(kb, C, kvlT, kvnT, KVL, S, gk, None); kb.end()
    kb.end()
    qnopeT = kb.dram([LH * 128, S], BF16)
    qr_raw = kb.dram([LH * RP, S], F32)
    qr_sw = kb.dram([LH * RP, S], F32)
    knopeT = kb.dram([LH * 128, S], BF16)
    vtm = kb.dram([S, LH * 128], BF16)
    wq = ins['wq_perm'][l]
    wkv = ins['wkv_perm'][l]
    n1 = LH * 128
    n2 = LH * RP
    kb.begin(); linear(kb, qnT, QL, S, lambda n0, ns: wq[:, n0:n0 + ns], n1, store_epi(kb, qnopeT, BF16)); kb.end()
    kb.begin(); linear(kb, qnT, QL, S, lambda n0, ns: wq[:, n1 + n0:n1 + n0 + ns], n2, store_epi(kb, qr_raw, F32)); kb.end()
    kb.begin(); linear(kb, qnT, QL, S, lambda n0, ns: wq[:, n1 + n2 + n0:n1 + n2 + n0 + ns], n2, store_epi(kb, qr_sw, F32)); kb.end()
    kb.begin(); linear(kb, kvnT, KVL, S, lambda n0, ns: wkv[:, n0:n0 + ns], n1, store_epi(kb, knopeT, BF16)); kb.end()
    kb.begin(); linear(kb, kvnT, KVL, S, lambda n0, ns: wkv[:, n1 + n0:n1 + n0 + ns], n1, store_epi(kb, vtm, BF16, tm=True), tm=True); kb.end()
    qrT = kb.dram([LH * RP, S], BF16)
    krotT = kb.dram([RP, S], BF16)
    kb.begin()
    pq = kb.sb('pq', [128, S], F32)
    kb.load('sp', pq, [(pq[:, :], posf[0:1, :].to_broadcast([128, S]))])
    rc = kb.sb('ropec', [128, 2], F32)
    kb.load('sp', rc, [(rc[:, :], ins['ropec'])])
    cs = kb.sb('cos', [128, S], F32)
    sn = kb.sb('sin', [128, S], F32)
    kb.op('dve', lambda e: e.tensor_scalar(out=pq[:, :], in0=pq[:, :], scalar1=rc[:, 0:1], scalar2=None, op0=ALU.mult), reads=(pq, rc), writes=(pq,))
    ki = kb.sb('ki', [128, S], I32)
    kf = kb.sb('kf', [128, S], F32)
    yy = kb.sb('yy', [128, S], F32)
    for tb, sh in ((sn, 0.0), (cs, 0.5 * math.pi)):
        kb.op('dve', lambda e: e.tensor_scalar(out=yy[:, :], in0=pq[:, :], scalar1=1.0 / (2 * math.pi), scalar2=sh / (2 * math.pi) + 0.5,
                                               op0=ALU.mult, op1=ALU.add), reads=(pq,), writes=(yy,))
        kb.op('dve', lambda e: e.tensor_copy(out=ki[:, :], in_=yy[:, :]), reads=(yy,), writes=(ki,))
        kb.op('dve', lambda e: e.tensor_copy(out=kf[:, :], in_=ki[:, :]), reads=(ki,), writes=(kf,))
        kb.op('dve', lambda e: e.tensor_tensor(out=yy[:, :], in0=kf[:, :], in1=yy[:, :], op=ALU.is_gt), reads=(kf, yy), writes=(yy,))
        kb.op('dve', lambda e: e.tensor_tensor(out=kf[:, :], in0=kf[:, :], in1=yy[:, :], op=ALU.subtract), reads=(kf, yy), writes=(kf,))
        kb.op('dve', lambda e: e.tensor_scalar(out=tb[:, :], in0=pq[:, :], scalar1=sh, scalar2=None, op0=ALU.add), reads=(pq,), writes=(tb,))
        kb.op('dve', lambda e: e.scalar_tensor_tensor(out=tb[:, :], in0=kf[:, :], scalar=-2 * math.pi, in1=tb[:, :], op0=ALU.mult, op1=ALU.add),
              reads=(kf, tb), writes=(tb,))
        kb.op('act', lambda e: e.activation(out=tb[:, :], in_=tb[:, :], func=AF.Sin), reads=(tb,), writes=(tb,))
    kb.op('dve', lambda e: e.tensor_scalar(out=sn[:, :], in0=sn[:, :], scalar1=rc[:, 1:2], scalar2=None, op0=ALU.mult), reads=(sn, rc), writes=(sn,))
    ra = kb.rot('ra', [128, 2, 512], F32, 2)
    ro = kb.rot('ro', [128, 512], BF16, 2)
    jobs = [(qr_raw[r0:r0 + 128, :], qr_sw[r0:r0 + 128, :], qrT[r0:r0 + 128, :], 128) for r0 in range(0, LH * RP, 128)]
    jobs.append((krT, krswT, krotT, RP))
    for a_ap, b_ap, o_ap, p in jobs:
        for t0 in range(0, S, 512):
            x = ra.next()
            kb.load('sp', x, [(x[:p, 0, :], a_ap[:, t0:t0 + 512]), (x[:p, 1, :], b_ap[:, t0:t0 + 512])])
            kb.op('dve', lambda e: e.tensor_tensor(out=x[:p, 0, :], in0=x[:p, 0, :], in1=cs[:p, t0:t0 + 512], op=ALU.mult), reads=(x, cs), writes=(x,))
            kb.op('dve', lambda e: e.tensor_tensor(out=x[:p, 1, :], in0=x[:p, 1, :], in1=sn[:p, t0:t0 + 512], op=ALU.mult), reads=(x, sn), writes=(x,))
            o = ro.next()
            kb.op('dve', lambda e: e.tensor_tensor(out=o[:p, :], in0=x[:p, 0, :], in1=x[:p, 1, :], op=ALU.add), reads=(x,), writes=(o,))
            kb.store('act', o, [(o_ap[:, t0:t0 + 512], o[:p, :])])
    kb.end()
    kb.begin()
    A = attn_bufs(kb, c, 2, [128, RP], False, posf)
    base = 2 * c['BW']
    for h in range(LH):
        attn_head(kb, C, c, [(qnopeT[h * 128:(h + 1) * 128, :], knopeT[h * 128:(h + 1) * 128, :], 128),
                             (qrT[h * RP:(h + 1) * RP, :], krotT, RP)], vtm[:, h * 128:(h + 1) * 128], 128,
                  ocT[base + h * 128:base + (h + 1) * 128, :], (c['NOPE'] + RP) ** -0.5, None, 'causal', A)
    kb.end()


def ssm_phase(kb, C, c, ins, l, zt, xbcT, dtr, ocT):
    S, SDI, SH, SG, SN, CC = c['S'], c['SDI'], c['SH'], c['SG'], c['SN'], c['CONVC'] // 128
    assert SN == 128 and c['SHD'] == 64
    NXC = SDI // 128
    HPG = SH // SG
    xcT = kb.dram([c['CONVC'], S], F32)
    kb.begin()
    cw = kb.sb('cw', [128, CC, 4], F32)
    cb = kb.sb('cb', [128, CC], F32)
    kb.load('sp', cw, [(cw[:, :, :], ins['conv_w_pk'][l])])
    kb.load('sp', cb, [(cb[:, :], ins['conv_b_pk'][l])])
    TT = 512
    ci = kb.rot('ci', [128, TT + 3], F32, 2)
    ca = kb.rot('ca', [128, TT], F32, 2)
    for cc in range(CC):
        for t0 in range(0, S, TT):
            u = ci.next()
            if t0 == 0:
                kb.op('dve', lambda e: e.memset(u[:, 0:3], 0.0), writes=(u,))
                kb.load('sp', u, [(u[:, 3:], xbcT[cc * 128:(cc + 1) * 128, 0:TT])])
            else:
                kb.load('sp', u, [(u[:, :], xbcT[cc * 128:(cc + 1) * 128, t0 - 3:t0 + TT])])
            a = ca.next()
            kb.op('dve', lambda e: e.tensor_scalar(out=a[:, :], in0=u[:, 0:TT], scalar1=cw[:, cc, 0:1], scalar2=None, op0=ALU.mult),
                  reads=(u, cw), writes=(a,))
            for k in range(1, 4):
                kb.op('dve', lambda e: e.scalar_tensor_tensor(out=a[:, :], in0=u[:, k:k + TT], scalar=cw[:, cc, k:k + 1], in1=a[:, :],
                                                              op0=ALU.mult, op1=ALU.add), reads=(u, cw, a), writes=(a,))
            kb.op('act', lambda e: e.activation(out=a[:, :], in_=a[:, :], func=AF.Silu, bias=cb[:, cc:cc + 1]), reads=(a, cb), writes=(a,))
            kb.store('act', a, [(xcT[cc * 128:(cc + 1) * 128, t0:t0 + TT], a[:, :])])
    kb.end()
    kb.begin()
    tri = C['masks']
    mneg = C['masks']
    ident = C['ident_f32']

    def bc(name, src, n):
        b = kb.sb(name, [128, n], F32)
        kb.load('sp', b, [(b[:, :], src.to_broadcast([128, n]))])
        return b
    dtb = bc('dtb', ins['dt_bias'][l:l + 1, :], SH)
    Abc = bc('Abc', ins['a_log'][l:l + 1, :], SH)
    kb.op('act', lambda e: e.activation(out=Abc[:, :], in_=Abc[:, :], func=AF.Exp), reads=(Abc,), writes=(Abc,))
    kb.op('dve', lambda e: e.tensor_scalar(out=Abc[:, :], in0=Abc[:, :], scalar1=-1.0, scalar2=None, op0=ALU.mult), reads=(Abc,), writes=(Abc,))
    Dbc = bc('Dbc', ins['d_skip'][l:l + 1, :], SH)
    nwb = bc('nwb', ins['ssm_norm'][l:l + 1, :], SDI)
    state = kb.sb('state', [128, SDI], F32)
    kb.op('dve', lambda e: e.memset(state[:, :], 0.0), writes=(state,))
    xcs = kb.rot('xc', [128, CC, 128], F32, 2)
    zs = kb.rot('z', [128, SDI], F32, 2)
    dts = kb.rot('dtr', [128, SH], F32, 2)
    pA = kb.rot('pA', [128, 512], F32, 3, psum=True)
    yps = [kb.ps('yps%d' % i, [128, 512], F32) for i in range((SDI + 511) // 512)]
    sps = [kb.ps('sps%d' % i, [128, 512], F32) for i in range((SDI + 511) // 512)]
    sm = {n: kb.sb(n, [128, SH], F32) for n in ('dt', 'a', 'acs', 'dec', 'cdec', 'tmp')}
    xs = kb.sb('xs', [128, SDI], F32)
    xdt = kb.sb('xdt', [128, SDI], F32)
    xdt_bf = kb.sb('xdtb', [128, SDI], BF16)
    xdec_bf = kb.sb('xdec', [128, SDI], BF16)
    st_bf = kb.sb('stb', [128, SDI], BF16)
    btm = kb.sb('btm', [128, SG, 128], BF16)
    BT = kb.sb('BT', [128, SG, 128], BF16)
    CT = kb.sb('CT', [128, SG, 128], BF16)
    gt = kb.sb('gt', [128, SG, 128], F32)
    arep = kb.rot('arep', [128, 128], F32, 2)
    dif = kb.rot('dif', [128, 128], F32, 2)
    edec = kb.rot('edec', [128, 128], F32, 2)
    MT = kb.rot('MT', [128, 128], BF16, 2)
    Cd = kb.rot('Cd', [128, 128], BF16, 2)
    ysb = kb.sb('ysb', [128, SDI], F32)
    gsb = kb.sb('gsb', [128, SDI], F32)
    ssq = kb.sb('ssq', [128, SG], F32)
    otb = kb.rot('otb', [128, NXC, 128], BF16, 2)
    v3 = lambda b: b[:, :].rearrange("p (h d) -> p h d", d=64)
    b3 = lambda b: b[:, :].unsqueeze(2).to_broadcast([128, SH, 64])
    GW = HPG * 64
    for ch in range(S // 128):
        t0 = ch * 128
        xc = xcs.next()
        kb.load('sp', xc, [(xc[:, :, :], xcT[:, t0:t0 + 128].rearrange("(cc p) t -> p cc t", p=128))])
        z = zs.next()
        kb.load('sp', z, [(z[:, :], zt[t0:t0 + 128, :])])
        dr = dts.next()
        kb.load('sp', dr, [(dr[:, :], dtr[t0:t0 + 128, :])])
        dt, a, acs, dec, cdec, tmp = (sm[n] for n in ('dt', 'a', 'acs', 'dec', 'cdec', 'tmp'))
        kb.op('dve', lambda e: e.tensor_tensor(out=dt[:, :], in0=dr[:, :], in1=dtb[:, :], op=ALU.add), reads=(dr, dtb), writes=(dt,))
        kb.op('act', lambda e: e.activation(out=dt[:, :], in_=dt[:, :], func=AF.Exp), reads=(dt,), writes=(dt,))
        kb.op('dve', lambda e: e.tensor_scalar(out=dt[:, :], in0=dt[:, :], scalar1=1.0, scalar2=None, op0=ALU.add), reads=(dt,), writes=(dt,))
        kb.op('act', lambda e: e.activation(out=dt[:, :], in_=dt[:, :], func=AF.Ln), reads=(dt,), writes=(dt,))
        kb.op('dve', lambda e: e.tensor_tensor(out=a[:, :], in0=dt[:, :], in1=Abc[:, :], op=ALU.mult), reads=(dt, Abc), writes=(a,))
        p1 = pA.next()
        kb.op('pe', lambda e: e.matmul(p1[:, 0:SH], lhsT=tri[:, 0, 0:128], rhs=a[:, :], start=True, stop=True), reads=(tri, a), writes=(p1,))
        kb.op('pe', lambda e: e.matmul(p1[:, 256:256 + SH], lhsT=C['ones_f32'][:, :], rhs=a[:, :], start=True, stop=True),
              reads=(C['ones_f32'], a), writes=(p1,))
        kb.op('act', lambda e: e.activation(out=acs[:, :], in_=p1[:, 0:SH], func=AF.Identity), reads=(p1,), writes=(acs,))
        kb.op('dve', lambda e: e.tensor_tensor(out=tmp[:, :], in0=p1[:, 256:256 + SH], in1=acs[:, :], op=ALU.subtract), reads=(p1, acs), writes=(tmp,))
        kb.op('act', lambda e: e.activation(out=dec[:, :], in_=tmp[:, :], func=AF.Exp), reads=(tmp,), writes=(dec,))
        kb.op('act', lambda e: e.activation(out=cdec[:, :], in_=p1[:, 256:256 + SH], func=AF.Exp), reads=(p1,), writes=(cdec,))
        for j0 in range(0, NXC, 4):
            pt = pA.next()
            nj = min(4, NXC - j0)
            for j in range(j0, j0 + nj):
                kb.op('pe', lambda e: e.matmul(pt[:, (j - j0) * 128:(j - j0 + 1) * 128], lhsT=xc[:, j, :], rhs=ident[:, :], start=True, stop=True),
                      reads=(xc, ident), writes=(pt,))
            kb.op('act', lambda e: e.activation(out=xs[:, j0 * 128:(j0 + nj) * 128], in_=pt[:, :nj * 128], func=AF.Identity), reads=(pt,), writes=(xs,))
        pt = pA.next()
        for g in range(SG):
            kb.op('pe', lambda e: e.matmul(pt[:, g * 128:(g + 1) * 128], lhsT=xc[:, NXC + g, :], rhs=ident[:, :], start=True, stop=True),
                  reads=(xc, ident), writes=(pt,))
        kb.op('act', lambda e: e.activation(out=btm[:, :, :].rearrange("p g n -> p (g n)"), in_=pt[:, :SG * 128], func=AF.Identity), reads=(pt,), writes=(btm,))
        kb.op('dve', lambda e: e.tensor_tensor(out=v3(xdt), in0=v3(xs), in1=b3(dt), op=ALU.mult), reads=(xs, dt), writes=(xdt,))
        kb.op('act', lambda e: e.activation(out=xdt_bf[:, :], in_=xdt[:, :], func=AF.Identity), reads=(xdt,), writes=(xdt_bf,))
        kb.op('dve', lambda e: e.tensor_tensor(out=v3(xdec_bf), in0=v3(xdt), in1=b3(dec), op=ALU.mult), reads=(xdt, dec), writes=(xdec_bf,))
        kb.op('act', lambda e: e.activation(out=BT[:, :, :], in_=xc[:, NXC:NXC + SG, :], func=AF.Identity), reads=(xc,), writes=(BT,))
        kb.op('act', lambda e: e.activation(out=CT[:, :, :], in_=xc[:, NXC + SG:NXC + 2 * SG, :], func=AF.Identity), reads=(xc,), writes=(CT,))
        pg = pA.next()
        for g in range(SG):
            kb.op('pe', lambda e: e.matmul(pg[:, g * 128:(g + 1) * 128], lhsT=BT[:, g, :], rhs=CT[:, g, :], start=True, stop=True),
                  reads=(BT, CT), writes=(pg,))
        kb.op('act', lambda e: e.activation(out=gt[:, :, :].rearrange("p g n -> p (g n)"), in_=pg[:, :SG * 128], func=AF.Identity), reads=(pg,), writes=(gt,))
        kb.op('act', lambda e: e.activation(out=st_bf[:, :], in_=state[:, :], func=AF.Identity), reads=(state,), writes=(st_bf,))
        for h in range(SH):
            g = h // HPG
            ar = arep.next()
            kb.op('dve', lambda e: e.tensor_copy(out=ar[:, :], in_=a[:, h:h + 1].to_broadcast([128, 128])), reads=(a,), writes=(ar,))
            pb = pA.next()
            kb.op('pe', lambda e: e.matmul(pb[:, 0:128], lhsT=ar[:, :], rhs=tri[:, 0, 0:128], start=True, stop=True), reads=(ar, tri), writes=(pb,))
            df = dif.next()
            kb.op('dve', lambda e: e.scalar_tensor_tensor(out=df[:, :], in0=pb[:, 0:128], scalar=acs[:, h:h + 1], in1=mneg[:, 3, 0:128],
                                                          op0=ALU.subtract, op1=ALU.add), reads=(pb, acs, mneg), writes=(df,))
            kb.op('act', lambda e: e.activation(out=df[:, :], in_=df[:, :], func=AF.Exp), reads=(df,), writes=(df,))
            mt = MT.next()
            kb.op('dve', lambda e: e.tensor_tensor(out=mt[:, :], in0=df[:, :], in1=gt[:, g, :], op=ALU.mult), reads=(df, gt), writes=(mt,))
            ed = edec.next()
            kb.op('act', lambda e: e.activation(out=ed[:, :], in_=pb[:, 0:128], func=AF.Exp), reads=(pb,), writes=(ed,))
            cd = Cd.next()
            kb.op('dve', lambda e: e.tensor_tensor(out=cd[:, :], in0=xc[:, NXC + SG + g, :], in1=ed[:, :], op=ALU.mult), reads=(xc, ed), writes=(cd,))
            yp = yps[(h * 64) // 512]
            yc = (h * 64) % 512
            kb.op('pe', lambda e: e.matmul(yp[:, yc:yc + 64], lhsT=mt[:, :], rhs=xdt_bf[:, h * 64:(h + 1) * 64], start=True, stop=False),
                  reads=(mt, xdt_bf), writes=(yp,))
            kb.op('pe', lambda e: e.matmul(yp[:, yc:yc + 64], lhsT=cd[:, :], rhs=st_bf[:, h * 64:(h + 1) * 64], start=False, stop=True),
                  reads=(cd, st_bf), writes=(yp,))
        for g in range(SG):
            sp_ = sps[(g * GW) // 512]
            sc = (g * GW) % 512
            kb.op('pe', lambda e: e.matmul(sp_[:, sc:sc + GW], lhsT=btm[:, g, :], rhs=xdec_bf[:, g * GW:(g + 1) * GW], start=True, stop=True),
                  reads=(btm, xdec_bf), writes=(sp_,))
        kb.op('dve', lambda e: e.tensor_tensor(out=v3(state), in0=v3(state), in1=b3(cdec), op=ALU.mult), reads=(state, cdec), writes=(state,))
        for i, sp_ in enumerate(sps):
            w = min(512, SDI - i * 512)
            kb.op('dve', lambda e: e.tensor_tensor(out=state[:, i * 512:i * 512 + w], in0=state[:, i * 512:i * 512 + w], in1=sp_[:, :w], op=ALU.add),
                  reads=(state, sp_), writes=(state,))
        kb.op('dve', lambda e: e.tensor_tensor(out=v3(ysb), in0=v3(xs), in1=b3(Dbc), op=ALU.mult), reads=(xs, Dbc), writes=(ysb,))
        for i, yp in enumerate(yps):
            w = min(512, SDI - i * 512)
            kb.op('dve', lambda e: e.tensor_tensor(out=ysb[:, i * 512:i * 512 + w], in0=ysb[:, i * 512:i * 512 + w], in1=yp[:, :w], op=ALU.add),
                  reads=(ysb, yp), writes=(ysb,))
        kb.op('act', lambda e: e.activation(out=z[:, :], in_=z[:, :], func=AF.Silu), reads=(z,), writes=(z,))
        kb.op('dve', lambda e: e.tensor_tensor(out=gsb[:, :], in0=ysb[:, :], in1=z[:, :], op=ALU.mult), reads=(ysb, z), writes=(gsb,))
        kb.op('dve', lambda e: e.tensor_tensor(out=ysb[:, :], in0=gsb[:, :], in1=gsb[:, :], op=ALU.mult), reads=(gsb,), writes=(ysb,))
        kb.op('dve', lambda e: e.tensor_reduce(out=ssq[:, :], in_=ysb[:, :].rearrange("p (g k) -> p g k", g=SG), axis=AX.X, op=ALU.add),
              reads=(ysb,), writes=(ssq,))
        kb.op('dve', lambda e: e.tensor_scalar(out=ssq[:, :], in0=ssq[:, :], scalar1=float(SG) / SDI, scalar2=1e-6, op0=ALU.mult, op1=ALU.add),
              reads=(ssq,), writes=(ssq,))
        kb.op('act', lambda e: e.activation(out=ssq[:, :], in_=ssq[:, :], func=AF.Ln), reads=(ssq,), writes=(ssq,))
        kb.op('act', lambda e: e.activation(out=ssq[:, :], in_=ssq[:, :], func=AF.Exp, scale=-0.5), reads=(ssq,), writes=(ssq,))
        kb.op('dve', lambda e: e.tensor_tensor(out=gsb[:, :].rearrange("p (g k) -> p g k", g=SG), in0=gsb[:, :].rearrange("p (g k) -> p g k", g=SG),
                                               in1=ssq[:, :].unsqueeze(2).to_broadcast([128, SG, SDI // SG]), op=ALU.mult), reads=(gsb, ssq), writes=(gsb,))
        kb.op('dve', lambda e: e.tensor_tensor(out=gsb[:, :], in0=gsb[:, :], in1=nwb[:, :], op=ALU.mult), reads=(gsb, nwb), writes=(gsb,))
        ot = otb.next()
        for j0 in range(0, NXC, 4):
            pt = pA.next()
            nj = min(4, NXC - j0)
            for j in range(j0, j0 + nj):
                kb.op('pe', lambda e: e.matmul(pt[:, (j - j0) * 128:(j - j0 + 1) * 128], lhsT=gsb[:, j * 128:(j + 1) * 128], rhs=ident[:, :], start=True, stop=True),
                      reads=(gsb, ident), writes=(pt,))
            kb.op('act', lambda e: e.activation(out=ot[:, j0:j0 + nj, :].rearrange("p j t -> p (j t)"), in_=pt[:, :nj * 128], func=AF.Identity),
                  reads=(pt,), writes=(ot,))
        kb.store('act', ot, [(ocT[c['BW']:c['BW'] + SDI, t0:t0 + 128].rearrange("(j p) t -> p j t", p=128), ot[:, :, :])])
    kb.end()


def win_phase(kb, C, c, ins, l, hT):
    D, S = c['D'], c['S']
    wt = ins['w_in_t']
    o = {}

    def seg(name, dt, tm=False, func=None):
        c0, n_, go = c['WSEG'][name]
        dst = kb.dram([S, n_] if tm else [n_, S], dt)
        kb.begin()
        linear(kb, hT, D, S, lambda n0, ns: wt[l, go + n0 // TNW][:, :, :ns], n_, store_epi(kb, dst, dt, func=func, tm=tm), tm=tm, NW=TNW)
        kb.end()
        o[name] = dst
    seg('mqT', BF16)
    seg('mkT', BF16)
    seg('mv', BF16, tm=True)
    seg('z', F32, tm=True)
    seg('xbcT', F32)
    seg('dtr', F32, tm=True)
    seg('qlT', F32)
    seg('kvlT', F32)
    seg('krT', F32)
    dst = kb.dram([c['ROPE'], S], F32)
    wk = ins['w_krsw'][l]
    kb.begin(); linear(kb, hT, D, S, lambda n0, ns: wk[:, n0:n0 + ns], c['ROPE'], store_epi(kb, dst, F32)); kb.end()
    o['krswT'] = dst
    seg('wqT', BF16)
    seg('wkT', BF16)
    seg('wv', BF16, tm=True)
    seg('gatesT', F32, func=AF.Sigmoid)
    return o


def merge_phase(kb, C, c, ins, l, ocT, gatesT, xT, g1, x1T):
    D, S, BW = c['D'], c['S'], c['BW']
    parts = [kb.dram([D, S], F32) for _ in range(4)]
    for r in range(4):
        kb.begin()
        gts = kb.rot('gt', [128, EB], F32, 2)
        obs = kb.rot('mo', [128, EB], F32, 2)

        class MergeEpi:
            def start(self, n0, ns, e0, es, r=r):
                self.g = gts.next()
                kb.load('sp', self.g, [(self.g[:ns, :es], gatesT[r * D + n0:r * D + n0 + ns, e0:e0 + es])])
                self.ob = obs.next()
                self.e0 = e0

            def tile(self, n0, ns, t0, ts, ps):
                g, ob, o = self.g, self.ob, t0 - self.e0
                kb.op('dve', lambda e: e.tensor_tensor(out=ob[:ns, o:o + ts], in0=ps[:ns, :ts], in1=g[:ns, o:o + ts], op=ALU.mult),
                      reads=(ps, g), writes=(ob,))

            def finish(self, n0, ns, e0, es, r=r):
                kb.store('act', self.ob, [(parts[r][n0:n0 + ns, e0:e0 + es], self.ob[:ns, :es])])
        epi = MergeEpi()
        wb = ins['w_branch'][l, r]
        linear(kb, ocT[r * BW:(r + 1) * BW, :], BW, S, lambda n0, ns: wb[:, n0:n0 + ns], D, epi)
        kb.end()
    mT = kb.dram([D, S], BF16)
    kb.begin(); ew_combine(kb, parts, mT, D, S, BF16); kb.end()
    kb.begin()
    xts = kb.rot('xr', [128, EB], F32, 2)
    obs = kb.rot('xo', [128, EB], F32, 2)

    class OutEpi:
        def start(self, n0, ns, e0, es):
            self.x = xts.next()
            kb.load('sp', self.x, [(self.x[:ns, :es], xT[n0:n0 + ns, e0:e0 + es])])
            self.ob = obs.next()
            self.e0 = e0

        def tile(self, n0, ns, t0, ts, ps):
            x, ob, o, fc = self.x, self.ob, t0 - self.e0, n0 // 128
            kb.op('dve', lambda e: e.scalar_tensor_tensor(out=ob[:ns, o:o + ts], in0=ps[:ns, :ts], scalar=g1[:ns, fc:fc + 1], in1=x[:ns, o:o + ts],
                                                          op0=ALU.mult, op1=ALU.add), reads=(ps, g1, x), writes=(ob,))

        def finish(self, n0, ns, e0, es):
            kb.store('act', self.ob, [(x1T[n0:n0 + ns, e0:e0 + es], self.ob[:ns, :es])])
    epi2 = OutEpi()
    wo = ins['w_out_t']
    linear(kb, mT, D, S, lambda n0, ns: wo[l, n0 // TNW][:, :, :ns], D, epi2, NW=TNW)
    kb.end()


def moe_phase(kb, C, c, ins, l, h2T, x1T, g2, x2T):
    D, S, NG, EPG, NE, EH = c['D'], c['S'], c['NG'], c['EPG'], c['NE'], c['EH']
    NR = NG + NE
    wTd = kb.dram([NE, S], F32)
    kb.begin()
    brt = kb.sb('brt', [128, NR], F32)
    kb.load('sp', brt, [(brt[:, :], ins['b_rt'][l:l + 1, :].to_broadcast([128, NR]))])
    WT = kb.sb('WT', [NE, S], F32)
    lg = kb.rot('lg', [128, NR], F32, 2)
    s1 = kb.rot('s1', [128, 8], F32, 2)
    mx8 = kb.rot('mx8', [128, 8], F32, 2)
    pen = kb.rot('pen', [128, NG], F32, 2)
    lm = kb.rot('lm', [128, NE], F32, 2)
    sel = kb.rot('sel', [128, NE], F32, 2)
    ex = kb.rot('exr', [128, NE], F32, 2)
    tps = kb.rot('tps', [128, 512], F32, 2, psum=True)
    junk = kb.rot('junk', [128, NG], F32, 2)

    def epi(n0, ns, t0, ts, ps):
        L_ = lg.next()
        kb.op('dve', lambda e: e.tensor_tensor(out=L_[:, :], in0=ps[:, :NR], in1=brt[:, :], op=ALU.add), reads=(ps, brt), writes=(L_,))
        s = s1.next()
        kb.op('dve', lambda e: e.tensor_reduce(out=s[:, 0:1], in_=L_[:, 0:NG], axis=AX.X, op=ALU.max), reads=(L_,), writes=(s,))
        kb.op('dve', lambda e: e.tensor_scalar(out=s[:, 1:2], in0=s[:, 0:1], scalar1=-1.0, scalar2=None, op0=ALU.mult), reads=(s,), writes=(s,))
        jk = junk.next()
        kb.op('act', lambda e: e.activation(out=jk[:, :], in_=L_[:, 0:NG], func=AF.Exp, bias=s[:, 1:2], accum_out=s[:, 2:3]), reads=(L_, s), writes=(jk, s))
        p = pen.next()
        kb.op('dve', lambda e: e.tensor_scalar(out=p[:, :], in0=L_[:, 0:NG], scalar1=s[:, 0:1], scalar2=None, op0=ALU.is_ge), reads=(L_, s), writes=(p,))
        kb.op('dve', lambda e: e.tensor_scalar(out=p[:, :], in0=p[:, :], scalar1=-1.0, scalar2=1e30, op0=ALU.add, op1=ALU.mult), reads=(p,), writes=(p,))
        m = lm.next()
        kb.op('dve', lambda e: e.tensor_tensor(out=m[:, :].rearrange("p (g k) -> p g k", g=NG), in0=L_[:, NG:NR].rearrange("p (g k) -> p g k", g=NG),
                                               in1=p[:, :].unsqueeze(2).to_broadcast([128, NG, EPG]), op=ALU.add), reads=(L_, p), writes=(m,))
        x8 = mx8.next()
        kb.op('dve', lambda e: e.max(out=x8[:, :], in_=m[:, :]), reads=(m,), writes=(x8,))
        sl = sel.next()
        kb.op('dve', lambda e: e.tensor_scalar(out=sl[:, :], in0=m[:, :], scalar1=x8[:, 1:2], scalar2=None, op0=ALU.is_ge), reads=(m, x8), writes=(sl,))
        kb.op('dve', lambda e: e.tensor_scalar(out=s[:, 4:5], in0=x8[:, 0:1], scalar1=-1.0, scalar2=None, op0=ALU.mult), reads=(x8, s), writes=(s,))
        e_ = ex.next()
        kb.op('act', lambda e: e.activation(out=e_[:, :], in_=m[:, :], func=AF.Exp, bias=s[:, 4:5]), reads=(m, s), writes=(e_,))
        kb.op('act', lambda e: e.activation(out=s[:, 5:6], in_=x8[:, 1:2], func=AF.Exp, bias=s[:, 4:5]), reads=(x8, s), writes=(s,))
        kb.op('dve', lambda e: e.scalar_tensor_tensor(out=s[:, 5:6], in0=s[:, 5:6], scalar=1.0, in1=s[:, 2:3], op0=ALU.add, op1=ALU.mult), reads=(s,), writes=(s,))
        kb.op('dve', lambda e: e.reciprocal(out=s[:, 3:4], in_=s[:, 5:6]), reads=(s,), writes=(s,))
        kb.op('dve', lambda e: e.scalar_tensor_tensor(out=e_[:, :], in0=e_[:, :], scalar=s[:, 3:4], in1=sl[:, :], op0=ALU.mult, op1=ALU.mult),
              reads=(e_, s, sl), writes=(e_,))
        tp = tps.next()
        kb.op('pe', lambda e: e.matmul(tp[:NE, :128], lhsT=e_[:, :], rhs=C['ident_f32'][:, :], start=True, stop=True), reads=(e_, C['ident_f32']), writes=(tp,))
        kb.op('act', lambda e: e.activation(out=WT[:, t0:t0 + 128], in_=tp[:NE, :128], func=AF.Identity), reads=(tp,), writes=(WT,))
    wr = ins['w_rt'][l]
    linear(kb, h2T, D, S, lambda n0, ns: wr[:, n0:n0 + ns], NR, epi, tm=True)
    kb.store('act', WT, [(wTd[:, :], WT[:, :])])
    kb.end()
    NWE = c['NWE']
    sgT = kb.dram([NE * EH, S], BF16)
    hidT = kb.dram([NE * EH, S], BF16)
    wg = ins['ewg_t']
    wu = ins['ewu_t']
    kb.begin()
    linear(kb, h2T, D, S, lambda n0, ns: wg[l, n0 // EH, (n0 % EH) // NWE][:, :, :ns], NE * EH, store_epi(kb, sgT, BF16, func=AF.Silu), NW=NWE)
    kb.end()
    kb.begin()
    sgs = kb.rot('sg', [128, EB], BF16, 2)
    wbs = kb.rot('wb', [128, EB], F32, 2)
    hos = kb.rot('ho', [128, EB], BF16, 2)
    tus = kb.rot('tu', [128, 512], F32, 2)

    class UpEpi:
        def start(self, n0, ns, e0, es):
            ei = n0 // EH
            self.wb = wbs.next()
            kb.load('sp', self.wb, [(self.wb[:, :es], wTd[ei:ei + 1, e0:e0 + es].to_broadcast([128, es]))])
            self.sg = sgs.next()
            kb.load('sp', self.sg, [(self.sg[:ns, :es], sgT[n0:n0 + ns, e0:e0 + es])])
            self.ho = hos.next()
            self.e0 = e0

        def tile(self, n0, ns, t0, ts, ps):
            wb, sg, ho, o = self.wb, self.sg, self.ho, t0 - self.e0
            tmpb = tus.next()
            kb.op('dve', lambda e: e.tensor_tensor(out=tmpb[:ns, :ts], in0=ps[:ns, :ts], in1=wb[:ns, o:o + ts], op=ALU.mult), reads=(ps, wb), writes=(tmpb,))
            kb.op('dve', lambda e: e.tensor_tensor(out=ho[:ns, o:o + ts], in0=tmpb[:ns, :ts], in1=sg[:ns, o:o + ts], op=ALU.mult), reads=(tmpb, sg), writes=(ho,))

        def finish(self, n0, ns, e0, es):
            kb.store('act', self.ho, [(hidT[n0:n0 + ns, e0:e0 + es], self.ho[:ns, :es])])
    linear(kb, h2T, D, S, lambda n0, ns: wu[l, n0 // EH, (n0 % EH) // NWE][:, :, :ns], NE * EH, UpEpi(), NW=NWE)
    kb.end()
    KG = EPG * EH
    parts = [kb.dram([D, S], F32) for _ in range(NG)]
    wd = ins['ewd_t']
    for g in range(NG):
        kb.begin()
        linear(kb, hidT[g * KG:(g + 1) * KG, :], KG, S, lambda n0, ns, g=g: wd[l, g, n0 // TNW][:, :, :ns], D, store_epi(kb, parts[g], F32), NW=TNW)
        kb.end()
    kb.begin(); ew_combine(kb, parts, x2T, D, S, F32, gate=g2, base=x1T); kb.end()


def in_specs(c):
    D, S, L = c['D'], c['S'], c['L']
    DC = D // 128
    NB = S // c['MBLK']
    CC = c['CONVC'] // 128
    sp = [
        ('xT', [D, S], F32), ('cT', [D, 1], F32), ('pos', [1, S], I32),
        ('ada_w_t', [L, 6 * D // TNW, 128, DC, TNW], F32), ('ada_b_pk', [L, 128, 6 * DC], F32),
        ('nm_pk', [L, 128, DC], F32), ('nf_pk', [L, 128, DC], F32), ('fin_pk', [128, DC], F32),
        ('w_in_t', [L, c['WG'], 128, DC, TNW], F32), ('w_krsw', [L, D, c['ROPE']], F32),
        ('conv_w_pk', [L, 128, CC, 4], F32), ('conv_b_pk', [L, 128, CC], F32),
        ('dt_bias', [L, c['SH']], F32), ('a_log', [L, c['SH']], F32), ('d_skip', [L, c['SH']], F32), ('ssm_norm', [L, c['SDI']], F32),
        ('qn_pk', [L, 128, c['QL'] // 128], F32), ('kvn_pk', [L, 128, c['KVL'] // 128], F32),
        ('wq_perm', [L, c['QL'], c['LH'] * (128 + 2 * c['ROPE'])], F32), ('wkv_perm', [L, c['KVL'], c['LH'] * 256], F32),
        ('sinks', [L, c['WH']], F32), ('w_branch', [L, 4, c['BW'], D], F32), ('w_out_t', [L, D // TNW, 128, DC, TNW], F32),
        ('w_rt', [L, D, c['NG'] + c['NE']], F32), ('b_rt', [L, c['NG'] + c['NE']], F32),
        ('ewg_t', [L, c['NE'], c['EH'] // c['NWE'], 128, DC, c['NWE']], F32), ('ewu_t', [L, c['NE'], c['EH'] // c['NWE'], 128, DC, c['NWE']], F32),
        ('ewd_t', [L, c['NG'], D // TNW, 128, c['EPG'] * c['EH'] // 128, TNW], F32),
        ('masks', [128, 4, 256], F32), ('ident', [128, 128], F32), ('elig', [S, NB], F32), ('esel', [NB, NB * 128], F32),
        ('ropec', [128, 2], F32),
    ]
    return sp


def build(cfg):
    c = derive(cfg)
    D, S, L = c['D'], c['S'], c['L']
    DC = D // 128
    NB = S // c['MBLK']
    nc = bass.Bass("TRN2", target_bir_lowering=False)
    ins = {n: nc.dram_tensor(n, sh, dt, kind="ExternalInput").ap() for n, sh, dt in in_specs(c)}
    outT = nc.dram_tensor("outT", [D, S], F32, kind="ExternalOutput").ap()
    kb = KB(nc)
    kb.begin()
    C = {}
    C['ones_f32'] = kb.sb('ones', [128, 128], F32)
    kb.op('dve', lambda e: e.memset(C['ones_f32'][:, :], 1.0), writes=(C['ones_f32'],))
    C['ones_bf'] = kb.sb('onesb', [128, 128], BF16)
    kb.op('dve', lambda e: e.memset(C['ones_bf'][:, :], 1.0), writes=(C['ones_bf'],))
    C['ident_f32'] = kb.sb('ident', [128, 128], F32)
    kb.load('sp', C['ident_f32'], [(C['ident_f32'][:, :], ins['ident'])])
    C['masks'] = kb.sb('masks', [128, 4, 256], F32)
    kb.load('sp', C['masks'], [(C['masks'][:, :, :], ins['masks'])])
    C['esel'] = kb.sb('esel', [NB, NB, 128], BF16)
    kb.load('pool', C['esel'], [(C['esel'][:, :, :], ins['esel'].rearrange("k (n m) -> k n m", m=128))])
    posf = kb.dram([1, S], F32)
    kb.begin()
    pi = kb.sb('posi', [1, S], I32)
    pf = kb.sb('posf', [1, S], F32)
    kb.load('sp', pi, [(pi[:, :], ins['pos'])])
    kb.op('dve', lambda e: e.tensor_copy(out=pf[:, :], in_=pi[:, :]), reads=(pi,), writes=(pf,))
    kb.store('act', pf, [(posf[:, :], pf[:, :])])
    kb.end()
    mods = []
    for l in range(L):
        m = kb.sb('mod%d' % l, [128, 6 * DC], F32)
        ab = kb.sb('adab%d' % l, [128, 6 * DC], F32)
        kb.load('sp', ab, [(ab[:, :], ins['ada_b_pk'][l])])
        kb.begin()

        def epi(n0, ns, t0, ts, ps, m=m, ab=ab):
            j = n0 // 128
            kb.op('dve', lambda e: e.tensor_tensor(out=m[:, j:j + 1], in0=ps[:, 0:1], in1=ab[:, j:j + 1], op=ALU.add), reads=(ps, ab), writes=(m,))
        aw = ins['ada_w_t']
        linear(kb, ins['cT'], D, 1, lambda n0, ns, l=l: aw[l, n0 // TNW][:, :, :ns], 6 * D, epi, xq='pool', NW=TNW)
        kb.end()
        nm = kb.sb('nm%d' % l, [128, DC], F32)
        nf = kb.sb('nf%d' % l, [128, DC], F32)
        kb.load('sp', nm, [(nm[:, :], ins['nm_pk'][l])])
        kb.load('sp', nf, [(nf[:, :], ins['nf_pk'][l])])
        gam1 = kb.sb('gam1_%d' % l, [128, DC], F32)
        gam2 = kb.sb('gam2_%d' % l, [128, DC], F32)
        kb.op('dve', lambda e: e.scalar_tensor_tensor(out=gam1[:, :], in0=m[:, DC:2 * DC], scalar=1.0, in1=nm[:, :], op0=ALU.add, op1=ALU.mult),
              reads=(m, nm), writes=(gam1,))
        kb.op('dve', lambda e: e.scalar_tensor_tensor(out=gam2[:, :], in0=m[:, 4 * DC:5 * DC], scalar=1.0, in1=nf[:, :], op0=ALU.add, op1=ALU.mult),
              reads=(m, nf), writes=(gam2,))
        sh1 = kb.sb('sh1_%d' % l, [128, DC], F32)
        g1 = kb.sb('g1_%d' % l, [128, DC], F32)
        sh2 = kb.sb('sh2_%d' % l, [128, DC], F32)
        g2 = kb.sb('g2_%d' % l, [128, DC], F32)
        for dst, k in ((sh1, 0), (g1, 2), (sh2, 3), (g2, 5)):
            kb.op('dve', lambda e: e.tensor_copy(out=dst[:, :], in_=m[:, k * DC:(k + 1) * DC]), reads=(m,), writes=(dst,))
        mods.append(dict(gam1=gam1, sh1=sh1, g1=g1, gam2=gam2, sh2=sh2, g2=g2))
    fin = kb.sb('fin', [128, DC], F32)
    kb.load('sp', fin, [(fin[:, :], ins['fin_pk'])])
    xT = ins['xT']
    for l in range(L):
        md = mods[l]
        hT = kb.dram([D, S], BF16)
        kb.begin(); rmsnorm_fm(kb, C, xT, hT, D, S, md['gam1'], md['sh1']); kb.end()
        o = win_phase(kb, C, c, ins, l, hT)
        ocT = kb.dram([4 * c['BW'], S], BF16)
        moba_phase(kb, C, c, ins, o['mqT'], o['mkT'], o['mv'], ocT, posf)
        ssm_phase(kb, C, c, ins, l, o['z'], o['xbcT'], o['dtr'], ocT)
        mla_phase(kb, C, c, ins, l, o['qlT'], o['kvlT'], o['krT'], o['krswT'], ocT, posf)
        swa_phase(kb, C, c, ins, l, o['wqT'], o['wkT'], o['wv'], ocT, posf)
        x1T = kb.dram([D, S], F32)
        merge_phase(kb, C, c, ins, l, ocT, o['gatesT'], xT, md['g1'], x1T)
        h2T = kb.dram([D, S], BF16)
        kb.begin(); rmsnorm_fm(kb, C, x1T, h2T, D, S, md['gam2'], md['sh2']); kb.end()
        x2T = kb.dram([D, S], F32)
        moe_phase(kb, C, c, ins, l, h2T, x1T, md['g2'], x2T)
        xT = x2T
    kb.begin(); rmsnorm_fm(kb, C, xT, outT, D, S, fin, None, dst_dt=F32); kb.end()
    kb.end()
    return nc


def host_consts(c):
    S = c['S']
    NB = S // c['MBLK']
    p = np.arange(128)[:, None]
    q = np.arange(256)[None, :]
    masks = np.zeros((128, 4, 256), np.float32)
    masks[:, 0] = (p <= q)
    masks[:, 1] = (128 + p <= q)
    masks[:, 2, :128] = (p > q[:, :128])
    masks[:, 3] = np.where(p <= q, 0.0, NEG)
    ident = np.eye(128, dtype=np.float32)
    qb = (np.arange(S) // c['MBLK'])[:, None]
    elig = (np.arange(NB)[None, :] < qb).astype(np.float32)
    esel = np.zeros((NB, NB, 128), np.float32)
    for n in range(NB):
        esel[n, n, :] = 1.0
    half = c['ROPE'] // 2
    inv = (np.float32(10000.0) ** (-np.arange(half, dtype=np.float32) / np.float32(half))).astype(np.float32)
    pp = np.arange(128) % c['ROPE']
    ropec = np.stack([inv[pp % half], np.where(pp < half, -1.0, 1.0)], 1).astype(np.float32)
    return dict(masks=masks, ident=ident, elig=elig, esel=esel.reshape(NB, NB * 128), ropec=ropec)


def pk(v):
    v = np.asarray(v)
    return np.ascontiguousarray(np.swapaxes(v.reshape(v.shape[:-1] + (-1, 128)), -1, -2))


def tile_w(W, NW):
    W = np.asarray(W)
    K, N = W.shape[-2:]
    G = -(-N // NW)
    if G * NW != N:
        W = np.concatenate([W, np.zeros(W.shape[:-1] + (G * NW - N,), W.dtype)], -1)
    W = W.reshape(W.shape[:-2] + (K // 128, 128, G, NW))
    nd = W.ndim
    perm = tuple(range(nd - 4)) + (nd - 2, nd - 3, nd - 4, nd - 1)
    return np.ascontiguousarray(W.transpose(perm))


def prep_shared(inp, c):
    L = c['L']
    off = c['IN_OFF']
    half = c['ROPE'] // 2
    LH, RP = c['LH'], c['ROPE']
    w_in = np.asarray(inp['w_in'])
    kr = w_in[:, :, off[6]:off[7]]
    d = {}
    d['ada_w_t'] = tile_w(inp['ada_w'], TNW)
    d['ada_b_pk'] = pk(inp['ada_b'])
    d['nm_pk'] = pk(inp['norm_mix'])
    d['nf_pk'] = pk(inp['norm_ffn'])
    d['fin_pk'] = pk(inp['final_norm'])
    d['w_in_t'] = np.concatenate([tile_w(w_in[:, :, c0:c0 + n], TNW) for (c0, n, go) in c['WSEG'].values()], 1)
    d['w_krsw'] = np.ascontiguousarray(np.concatenate([kr[:, :, half:], kr[:, :, :half]], -1))
    CC = c['CONVC'] // 128
    cw = np.asarray(inp['conv_w'])
    d['conv_w_pk'] = np.ascontiguousarray(cw.reshape(L, 4, CC, 128).transpose(0, 3, 2, 1))
    d['conv_b_pk'] = pk(inp['conv_b'])
    for k in ('dt_bias', 'a_log', 'd_skip', 'ssm_norm'):
        d[k] = np.asarray(inp[k])
    d['qn_pk'] = pk(inp['mla_q_norm'])
    d['kvn_pk'] = pk(inp['mla_kv_norm'])
    wq = np.asarray(inp['mla_wq_b']).reshape(L, c['QL'], LH, c['NOPE'] + RP)
    nope = wq[..., :c['NOPE']].reshape(L, c['QL'], -1)
    rope = wq[..., c['NOPE']:]
    rsw = np.concatenate([rope[..., half:], rope[..., :half]], -1)
    d['wq_perm'] = np.ascontiguousarray(np.concatenate([nope, rope.reshape(L, c['QL'], -1), rsw.reshape(L, c['QL'], -1)], -1))
    wkv = np.asarray(inp['mla_wkv_b']).reshape(L, c['KVL'], LH, c['NOPE'] + c['LV'])
    d['wkv_perm'] = np.ascontiguousarray(np.concatenate([wkv[..., :c['NOPE']].reshape(L, c['KVL'], -1),
                                                         wkv[..., c['NOPE']:].reshape(L, c['KVL'], -1)], -1))
    d['sinks'] = np.asarray(inp['swa_sinks'])
    d['w_branch'] = np.asarray(inp['w_branch'])
    d['w_out_t'] = tile_w(inp['w_out'], TNW)
    d['w_rt'] = np.ascontiguousarray(np.concatenate([inp['router_group_w'], inp['router_w']], -1))
    d['b_rt'] = np.ascontiguousarray(np.concatenate([inp['router_group_b'], inp['router_b']], -1))
    d['ewg_t'] = tile_w(inp['exp_w_gate'], c['NWE'])
    d['ewu_t'] = tile_w(inp['exp_w_up'], c['NWE'])
    ed = np.asarray(inp['exp_w_down'])
    d['ewd_t'] = tile_w(ed.reshape(L, c['NG'], c['EPG'] * c['EH'], c['D']), TNW)
    d.update(host_consts(c))
    return d


_NC_CACHE = {}


def run(inp, cfg):
    c = derive(cfg)
    key = tuple(sorted((k, v) for k, v in cfg.items()))
    if key not in _NC_CACHE:
        _NC_CACHE[key] = build(cfg)
    nc = _NC_CACHE[key]
    shared = prep_shared(inp, c)
    x = np.asarray(inp['x'])
    B = x.shape[0]
    maps = []
    for b in range(B):
        m = dict(shared)
        m['xT'] = np.ascontiguousarray(x[b].T)
        m['cT'] = np.ascontiguousarray(np.asarray(inp['c'])[b][:, None])
        m['pos'] = np.ascontiguousarray(np.asarray(inp['positions'])[b][None, :].astype(np.int32))
        maps.append(m)
    res = run_bass_kernel_spmd(nc, maps, core_ids=list(range(B)))
    out = np.stack([np.ascontiguousarray(res.results[b]['outT'].T) for b in range(B)], 0)
    return out.astype(np.float32)


def kernel(**inputs):
    return run(inputs, FULL)
```

```python
import math
from contextlib import ExitStack
import numpy as np
import concourse.bass as bass
import concourse.mybir as mybir
from concourse.bass_utils import run_bass_kernel_spmd

F32 = mybir.dt.float32
BF16 = mybir.dt.bfloat16
I32 = mybir.dt.int32
AF = mybir.ActivationFunctionType
ALU = mybir.AluOpType
AX = mybir.AxisListType

FULL = dict(D=4096, S=4096, L=2, MH=8, MBLK=256, MTOPK=3, SDI=1024, SHD=64, SG=2, SN=128, SCONV=4,
            LH=8, QL=768, KVL=512, NOPE=128, ROPE=64, LV=128, WH=16, WKV=2, WD=64, WW=128, BW=1024,
            NG=4, EPG=8, EH=512, B=2)
NEG = -30000.0
TNW = 256


def derive(c):
    c = dict(c)
    c['SH'] = c['SDI'] // c['SHD']
    c['CONVC'] = c['SDI'] + 2 * c['SG'] * c['SN']
    c['NE'] = c['NG'] * c['EPG']
    sizes = [3 * c['MH'] * 128, c['SDI'], c['CONVC'], c['SH'], c['QL'], c['KVL'], c['ROPE'],
             c['WH'] * c['WD'], c['WKV'] * c['WD'], c['WKV'] * c['WD'], 4 * c['D']]
    offs = np.concatenate([[0], np.cumsum(sizes)]).tolist()
    c['IN_OFF'] = offs
    c['N_IN'] = offs[-1]
    c['NALIBI'] = c['MH'] + c['WH']
    M = c['MH'] * 128
    segs = [('mqT', offs[0], M), ('mkT', offs[0] + M, M), ('mv', offs[0] + 2 * M, M)]
    for nm, i in (('z', 1), ('xbcT', 2), ('dtr', 3), ('qlT', 4), ('kvlT', 5), ('krT', 6), ('wqT', 7), ('wkT', 8), ('wv', 9), ('gatesT', 10)):
        segs.append((nm, offs[i], sizes[i]))
    go = 0
    tab = {}
    for nm, c0, n in segs:
        tab[nm] = (c0, n, go)
        go += -(-n // TNW)
    c['WSEG'] = tab
    c['WG'] = go
    c['NWE'] = min(TNW, c['EH'])
    return c


class Buf:
    __slots__ = ('t', 'w', 'r', 'ds')

    def __init__(self, t):
        self.t = t
        self.w = None
        self.r = {}
        self.ds = None

    def __getitem__(self, k):
        return self.t[k]


class Rot:
    def __init__(self, bufs):
        self.bufs = bufs
        self.i = 0

    def next(self):
        b = self.bufs[self.i % len(self.bufs)]
        self.i += 1
        return b


class KB:
    def __init__(self, nc):
        self.nc = nc
        self.eng = {'pe': nc.tensor, 'act': nc.scalar, 'dve': nc.vector, 'pool': nc.gpsimd, 'sp': nc.sync}
        self.sems = {}
        self.cnt = {}
        for k in self.eng:
            self.sems[k] = nc.alloc_semaphore('e_' + k)
            self.cnt[k] = 0
        self.seen = {k: {} for k in self.eng}
        self.free_ds = []
        self.nds = 0
        self.scopes = []
        self.uid = 0
        self.ndram = 0

    def begin(self):
        self.scopes.append((ExitStack(), []))

    def end(self):
        self.barrier()
        st, bufs = self.scopes.pop()
        for b in bufs:
            if b.ds is not None:
                self.free_ds.append(b.ds)
                b.ds = None
        st.close()

    def dram(self, shape, dt):
        self.ndram += 1
        return self.nc.dram_tensor(f'scr{self.ndram}', list(shape), dt).ap()

    def sb(self, name, shape, dt):
        self.uid += 1
        st, bufs = self.scopes[-1]
        t = st.enter_context(self.nc.sbuf_tensor(f'{name}_{self.uid}', list(shape), dt))
        b = Buf(t)
        bufs.append(b)
        return b

    def ps(self, name, shape, dt=F32):
        self.uid += 1
        st, bufs = self.scopes[-1]
        t = st.enter_context(self.nc.psum_tensor(f'{name}_{self.uid}', list(shape), dt))
        b = Buf(t)
        bufs.append(b)
        return b

    def rot(self, name, shape, dt, n, psum=False):
        return Rot([(self.ps if psum else self.sb)(f'{name}{i}', shape, dt) for i in range(n)])

    def _dsem(self, b):
        if b.ds is None:
            if self.free_ds:
                b.ds = self.free_ds.pop()
            else:
                k = f'd{self.nds}'
                self.nds += 1
                self.sems[k] = self.nc.alloc_semaphore(k)
                self.cnt[k] = 0
                b.ds = k
        return b.ds

    def _need(self, e, dep, raw=True):
        if dep is None:
            return
        k, v = dep
        if k == e and (e == 'pe' or not raw):
            return
        if self.seen[e].get(k, 0) >= v:
            return
        self.eng[e].wait_ge(self.sems[k], v)
        self.seen[e][k] = v

    def _deps(self, e, reads, writes):
        for b in reads:
            self._need(e, b.w, raw=True)
        for b in writes:
            self._need(e, b.w, raw=False)
            for k, v in b.r.items():
                self._need(e, (k, v), raw=False)

    def op(self, e, ins_fn, reads=(), writes=()):
        self._deps(e, reads, writes)
        ins = ins_fn(self.eng[e])
        self.cnt[e] += 1
        ins.then_inc(self.sems[e], 1)
        v = self.cnt[e]
        for b in reads:
            if b.r.get(e, 0) < v:
                b.r[e] = v
        for b in writes:
            b.w = (e, v)
            b.r = {}
        return ins

    def load(self, q, b, pairs, **kw):
        self._deps(q, (), (b,))
        ds = self._dsem(b)
        for o, i in pairs:
            self.eng[q].dma_start(out=o, in_=i, **{'allow_slow_non_contiguous': True, **kw}).then_inc(self.sems[ds], 16)
            self.cnt[ds] += 16
        b.w = (ds, self.cnt[ds])
        b.r = {}

    def store(self, q, b, pairs, **kw):
        self._deps(q, (b,), ())
        ds = self._dsem(b)
        for o, i in pairs:
            self.eng[q].dma_start(out=o, in_=i, **{'allow_slow_non_contiguous': True, **kw}).then_inc(self.sems[ds], 16)
            self.cnt[ds] += 16
        b.r[ds] = self.cnt[ds]

    def barrier(self):
        for e in self.eng:
            for k, v in self.cnt.items():
                if k != e and v > 0:
                    self._need(e, (k, v), raw=False)


class FnEpi:
    def __init__(self, fn):
        self.fn = fn

    def start(self, n0, ns, e0, es):
        pass

    def tile(self, n0, ns, t0, ts, ps):
        self.fn(n0, ns, t0, ts, ps)

    def finish(self, n0, ns, e0, es):
        pass


EB = 1024
TR_CAP = None


def linear(kb, XT, K, T, Wfn, N, epi, tm=False, NW=None, wq='pool', xq='sp'):
    if not hasattr(epi, 'tile'):
        epi = FnEpi(epi)
    KC = K // 128
    assert K % 128 == 0
    if NW is None:
        NW = 256 if KC >= 32 else 512
    TR = min(T, max(512, (65536 // KC) // 512 * 512))
    if TR_CAP:
        TR = min(TR, TR_CAP)
    wts = kb.rot('lw', [128, KC, NW], BF16, 2)
    xres = kb.sb('lx', [128, KC, TR], BF16)
    pss = kb.rot('lp', [128, 512], F32, 4, psum=True)
    XTv = XT.rearrange("(kc p) t -> p kc t", p=128)
    for tb0 in range(0, T, TR):
        tbs = min(TR, T - tb0)
        kb.load(xq, xres, [(xres[:, :, :tbs], XTv[:, :, tb0:tb0 + tbs])])
        for n0g in range(0, N, NW):
            nsg = min(NW, N - n0g)
            wt = wts.next()
            W = Wfn(n0g, nsg)
            if len(W.shape) == 2:
                W = W.rearrange("(kc p) n -> p kc n", p=128)
            kb.load(wq, wt, [(wt[:, :, :nsg], W)])
            if not tm:
                for c0 in range(0, nsg, 128):
                    cs = min(128, nsg - c0)
                    for e0 in range(tb0, tb0 + tbs, EB):
                        es = min(EB, tb0 + tbs - e0)
                        epi.start(n0g + c0, cs, e0, es)
                        for t0 in range(e0, e0 + es, 512):
                            ts = min(512, e0 + es - t0)
                            ps = pss.next()
                            for kc in range(KC):
                                kb.op('pe', lambda e, kc=kc: e.matmul(ps[:cs, :ts], lhsT=wt[:, kc, c0:c0 + cs],
                                                                      rhs=xres[:, kc, t0 - tb0:t0 - tb0 + ts], start=(kc == 0),
                                                                      stop=(kc == KC - 1)),
                                      reads=(wt, xres), writes=(ps,))
                            epi.tile(n0g + c0, cs, t0, ts, ps)
                        epi.finish(n0g + c0, cs, e0, es)
            else:
                for s0 in range(0, tbs, 128):
                    ss = min(128, tbs - s0)
                    ps = pss.next()
                    for kc in range(KC):
                        kb.op('pe', lambda e, kc=kc: e.matmul(ps[:ss, :nsg], lhsT=xres[:, kc, s0:s0 + ss],
                                                              rhs=wt[:, kc, :nsg], start=(kc == 0),
                                                              stop=(kc == KC - 1)),
                              reads=(wt, xres), writes=(ps,))
                    epi.tile(n0g, nsg, tb0 + s0, ss, ps)


class store_epi:
    def __init__(self, kb, dst, dt, scale=None, func=None, q='act', tm=False, coloff=0):
        self.kb, self.dst, self.dt, self.scale, self.func, self.q, self.tm, self.coloff = kb, dst, dt, scale, func, q, tm, coloff
        self.obs = kb.rot('eo', [128, 512 if tm else EB], dt, 3 if tm else 2)

    def start(self, n0, ns, e0, es):
        self.ob = self.obs.next()
        self.e0 = e0

    def tile(self, n0, ns, t0, ts, ps):
        kb = self.kb
        sc = 1.0 if self.scale is None else self.scale
        fn = self.func or AF.Identity
        if self.tm:
            ob = self.obs.next()
            kb.op('act', lambda e: e.activation(out=ob[:ts, :ns], in_=ps[:ts, :ns], func=fn, scale=sc), reads=(ps,), writes=(ob,))
            kb.store(self.q, ob, [(self.dst[t0:t0 + ts, self.coloff + n0:self.coloff + n0 + ns], ob[:ts, :ns])])
        else:
            ob = self.ob
            o = t0 - self.e0
            kb.op('act', lambda e: e.activation(out=ob[:ns, o:o + ts], in_=ps[:ns, :ts], func=fn, scale=sc), reads=(ps,), writes=(ob,))

    def finish(self, n0, ns, e0, es):
        if not self.tm:
            self.kb.store(self.q, self.ob, [(self.dst[self.coloff + n0:self.coloff + n0 + ns, e0:e0 + es], self.ob[:ns, :es])])


def rmsnorm_fm(kb, C, src, dst, F, T, gam, bet, eps=1e-6, dst_dt=BF16, TT=256):
    FC = F // 128
    xs = kb.rot('nx', [128, FC, TT], F32, 2)
    sq = kb.rot('nq', [128, TT], F32, 2)
    pr = kb.rot('np', [128, 512], F32, 2, psum=True)
    rs = kb.rot('nr', [128, TT], F32, 2)
    ob = kb.rot('no', [128, FC, TT], dst_dt, 2)
    ones = C['ones_f32']
    sv = src.rearrange("(fc p) t -> p fc t", p=128)
    dv = dst.rearrange("(fc p) t -> p fc t", p=128)
    for t0 in range(0, T, TT):
        ts = min(TT, T - t0)
        x = xs.next()
        kb.load('sp', x, [(x[:, :, :ts], sv[:, :, t0:t0 + ts])])
        p = pr.next()
        for fc in range(FC):
            s = sq.next()
            kb.op('act', lambda e: e.activation(out=s[:, :ts], in_=x[:, fc, :ts], func=AF.Square), reads=(x,), writes=(s,))
            kb.op('pe', lambda e: e.matmul(p[:, :ts], lhsT=ones[:, :], rhs=s[:, :ts], start=(fc == 0), stop=(fc == FC - 1)),
                  reads=(s, ones), writes=(p,))
        r = rs.next()
        kb.op('dve', lambda e: e.tensor_scalar(out=r[:, :ts], in0=p[:, :ts], scalar1=1.0 / F, scalar2=eps,
                                               op0=ALU.mult, op1=ALU.add), reads=(p,), writes=(r,))
        kb.op('act', lambda e: e.activation(out=r[:, :ts], in_=r[:, :ts], func=AF.Ln), reads=(r,), writes=(r,))
        kb.op('act', lambda e: e.activation(out=r[:, :ts], in_=r[:, :ts], func=AF.Exp, scale=-0.5), reads=(r,), writes=(r,))
        o = ob.next()
        for fc in range(FC):
            kb.op('dve', lambda e: e.tensor_tensor(out=x[:, fc, :ts], in0=x[:, fc, :ts], in1=r[:, :ts], op=ALU.mult),
                  reads=(x, r), writes=(x,))
            if bet is not None:
                kb.op('act', lambda e: e.activation(out=o[:, fc, :ts], in_=x[:, fc, :ts], func=AF.Identity,
                                                    scale=gam[:, fc:fc + 1], bias=bet[:, fc:fc + 1]),
                      reads=(x, gam, bet), writes=(o,))
            else:
                kb.op('act', lambda e: e.activation(out=o[:, fc, :ts], in_=x[:, fc, :ts], func=AF.Identity,
                                                    scale=gam[:, fc:fc + 1]),
                      reads=(x, gam), writes=(o,))
        kb.store('act', o, [(dv[:, :, t0:t0 + ts], o[:, :, :ts])])


def ew_combine(kb, srcs, dst, F, T, dst_dt, gate=None, base=None, TT=512):
    n = len(srcs) + (1 if base is not None else 0)
    tl = kb.rot('ec', [128, n, TT], F32, 2)
    ob = kb.rot('eo', [128, TT], dst_dt, 2)
    for fc in range(F // 128):
        r0 = fc * 128
        for t0 in range(0, T, TT):
            ts = min(TT, T - t0)
            x = tl.next()
            pairs = [(x[:, i, :ts], s[r0:r0 + 128, t0:t0 + ts]) for i, s in enumerate(srcs)]
            if base is not None:
                pairs.append((x[:, n - 1, :ts], base[r0:r0 + 128, t0:t0 + ts]))
            kb.load('sp', x, pairs)
            for i in range(1, len(srcs)):
                kb.op('dve', lambda e: e.tensor_tensor(out=x[:, 0, :ts], in0=x[:, 0, :ts], in1=x[:, i, :ts], op=ALU.add),
                      reads=(x,), writes=(x,))
            o = ob.next()
            if base is not None:
                kb.op('dve', lambda e: e.scalar_tensor_tensor(out=o[:, :ts], in0=x[:, 0, :ts], scalar=gate[:, fc:fc + 1],
                                                              in1=x[:, n - 1, :ts], op0=ALU.mult, op1=ALU.add),
                      reads=(x, gate), writes=(o,))
            else:
                kb.op('act', lambda e: e.activation(out=o[:, :ts], in_=x[:, 0, :ts], func=AF.Identity), reads=(x,), writes=(o,))
            kb.store('act', o, [(dst[r0:r0 + 128, t0:t0 + ts], o[:, :ts])])


def attn_head(kb, C, c, parts, v_ap, dv, out_ap, scale, slope, mode, A, selT=None, sinkcol=None):
    S = c['S']
    NKT = S // 128
    QT = 128 if mode == 'swa' else 256
    qs, ks = [], []
    for i, (q_ap, k_ap, dk) in enumerate(parts):
        qb = A['q%d' % i].next()
        kb.load('sp', qb, [(qb[:dk, :], q_ap)])
        kbuf = A['k%d' % i].next()
        kb.load('sp', kbuf, [(kbuf[:dk, :], k_ap)])
        qs.append(qb)
        ks.append(kbuf)
    vb = A['v'].next()
    kb.load('sp', vb, [(vb[:, :, :dv], v_ap.rearrange("(t p) d -> p t d", p=128))])
    masks = C['masks']
    pairs = []
    for qi in range(S // QT):
        if mode == 'swa':
            kts = ([(qi - 1, 2)] if qi >= 1 else []) + [(qi, 0)]
        else:
            kts = []
            for kt in range(2 * qi + 2):
                if kt // 2 == qi:
                    kts.append((kt, kt % 2))
                else:
                    kts.append((kt, 'sel' if mode == 'moba' else None))
        for j, (kt, mk) in enumerate(kts):
            pairs.append(dict(qi=qi, q0=qi * QT, kt=kt, mk=mk, first=(j == 0), last=(j == len(kts) - 1)))

    def stage_a(p):
        q0, kt = p['q0'], p['kt']
        s_ps = A['s_ps'].next()
        for i, (q_ap, k_ap, dk) in enumerate(parts):
            kb.op('pe', lambda e: e.matmul(s_ps[:, :QT], lhsT=ks[i][:dk, kt * 128:(kt + 1) * 128],
                                           rhs=qs[i][:dk, q0:q0 + QT], start=(i == 0), stop=(i == len(parts) - 1)),
                  reads=(ks[i], qs[i]), writes=(s_ps,))
        p['s_ps'] = s_ps
        if p['mk'] == 'sel':
            m_ps = A['m_ps'].next()
            kb.op('pe', lambda e: e.matmul(m_ps[:, :QT], lhsT=C['esel'][:, kt // 2, :], rhs=selT[:, q0:q0 + QT], start=True, stop=True),
                  reads=(C['esel'], selT), writes=(m_ps,))
            p['m_ps'] = m_ps

    cur = {}

    def stage_b(p):
        q0, kt, mk, s_ps = p['q0'], p['kt'], p['mk'], p['s_ps']
        if p['first']:
            cur['o'] = A['o_ps'].next()
            cur['d'] = A['d_ps'].next()
        o_ps, d_ps = cur['o'], cur['d']
        if slope is not None:
            ds = A['ds'].next()
            kb.op('dve', lambda e: e.tensor_scalar(out=ds[:, :QT], in0=A['posq'][:, q0:q0 + QT], scalar1=A['posk'][:, kt:kt + 1],
                                                   scalar2=None, op0=ALU.subtract),
                  reads=(A['posq'], A['posk']), writes=(ds,))
            kb.op('dve', lambda e: e.scalar_tensor_tensor(out=ds[:, :QT], in0=ds[:, :QT], scalar=-1.0, in1=ds[:, :QT],
                                                          op0=ALU.mult, op1=ALU.max), reads=(ds,), writes=(ds,))
            kb.op('dve', lambda e: e.scalar_tensor_tensor(out=ds[:, :QT], in0=ds[:, :QT], scalar=-slope / scale, in1=s_ps[:, :QT],
                                                          op0=ALU.mult, op1=ALU.add), reads=(ds, s_ps), writes=(ds,))
            src = ds
        else:
            src = s_ps
        pb = A['pb'].next()
        if mk is None:
            kb.op('act', lambda e: e.activation(out=pb[:, :QT], in_=src[:, :QT], func=AF.Exp, scale=scale), reads=(src,), writes=(pb,))
        else:
            ex = A['ex'].next()
            kb.op('act', lambda e: e.activation(out=ex[:, :QT], in_=src[:, :QT], func=AF.Exp, scale=scale), reads=(src,), writes=(ex,))
            if mk == 'sel':
                m_ps = p['m_ps']
                kb.op('dve', lambda e: e.tensor_tensor(out=pb[:, :QT], in0=ex[:, :QT], in1=m_ps[:, :QT], op=ALU.mult),
                      reads=(ex, m_ps), writes=(pb,))
            else:
                kb.op('dve', lambda e: e.tensor_tensor(out=pb[:, :QT], in0=ex[:, :QT], in1=masks[:, mk, :QT], op=ALU.mult),
                      reads=(ex, masks), writes=(pb,))
        kb.op('pe', lambda e: e.matmul(o_ps[:dv, :QT], lhsT=vb[:, kt, :dv], rhs=pb[:, :QT], start=p['first'], stop=p['last']),
              reads=(vb, pb), writes=(o_ps,))
        kb.op('pe', lambda e: e.matmul(d_ps[:, :QT], lhsT=C['ones_bf'][:, :], rhs=pb[:, :QT], start=p['first'], stop=p['last']),
              reads=(C['ones_bf'], pb), writes=(d_ps,))
        if p['last']:
            rc = A['rc'].next()
            if sinkcol is not None:
                kb.op('dve', lambda e: e.tensor_scalar(out=rc[:, :QT], in0=d_ps[:, :QT], scalar1=sinkcol, scalar2=None, op0=ALU.add),
                      reads=(d_ps, A['esink']), writes=(rc,))
                kb.op('dve', lambda e: e.reciprocal(out=rc[:, :QT], in_=rc[:, :QT]), reads=(rc,), writes=(rc,))
            else:
                kb.op('dve', lambda e: e.reciprocal(out=rc[:, :QT], in_=d_ps[:, :QT]), reads=(d_ps,), writes=(rc,))
            ob = A['ob'].next()
            kb.op('dve', lambda e: e.tensor_tensor(out=ob[:dv, :QT], in0=o_ps[:dv, :QT], in1=rc[:dv, :QT], op=ALU.mult),
                  reads=(o_ps, rc), writes=(ob,))
            kb.store('act', ob, [(out_ap[:, q0:q0 + QT], ob[:dv, :QT])])

    stage_a(pairs[0])
    for i, p in enumerate(pairs):
        if i + 1 < len(pairs):
            stage_a(pairs[i + 1])
        stage_b(p)


def attn_bufs(kb, c, nparts, dks, need_pos, posf):
    S = c['S']
    A = {}
    for i in range(nparts):
        A['q%d' % i] = kb.rot('aq%d' % i, [128, S], BF16, 2)
        A['k%d' % i] = kb.rot('ak%d' % i, [128, S], BF16, 2)
    A['v'] = kb.rot('av', [128, S // 128, 128], BF16, 2)
    A['s_ps'] = kb.rot('as', [128, 512], F32, 2, psum=True)
    A['m_ps'] = kb.rot('am', [128, 512], F32, 2, psum=True)
    A['o_ps'] = kb.rot('ao', [128, 512], F32, 2, psum=True)
    A['d_ps'] = kb.rot('ad', [128, 512], F32, 2, psum=True)
    A['ex'] = kb.rot('aex', [128, 256], F32, 3)
    A['ds'] = kb.rot('ads', [128, 256], F32, 3)
    A['pb'] = kb.rot('apb', [128, 256], BF16, 3)
    A['rc'] = kb.rot('arc', [128, 256], F32, 2)
    A['ob'] = kb.rot('aob', [128, 256], BF16, 2)
    if need_pos:
        A['posq'] = kb.sb('posq', [128, S], F32)
        kb.load('sp', A['posq'], [(A['posq'][:, :], posf[0:1, :].to_broadcast([128, S]))])
        A['posk'] = kb.sb('posk', [128, S // 128], F32)
        kb.load('sp', A['posk'], [(A['posk'][:, :], posf[0, :].rearrange("(t p) -> p t", p=128))], allow_slow_non_contiguous=True)
    return A


def alibi_slopes(c):
    i = np.arange(1, c['NALIBI'] + 1, dtype=np.float32)
    s = np.exp2(np.float32(-8.0) * i / np.float32(c['NALIBI'])).astype(np.float32)
    return [float(v) for v in s[:c['WH']]], [float(v) for v in s[c['WH']:]]


def moba_phase(kb, C, c, ins, mqT, mkT, mv, ocT, posf):
    S, MH, NB = c['S'], c['MH'], c['S'] // c['MBLK']
    swa_sl, moba_sl = alibi_slopes(c)
    kb.begin()
    A = attn_bufs(kb, c, 1, [128], True, posf)
    NT = S // 128
    el = kb.sb('elig', [128, NT, NB], F32)
    kb.load('sp', el, [(el[:, :, :], ins['elig'].rearrange("(t p) n -> p t n", p=128))])
    eln = kb.sb('eln', [128, NT, NB], F32)
    kb.op('dve', lambda e: e.tensor_scalar(out=eln[:, :, :], in0=el[:, :, :], scalar1=-1.0, scalar2=1e30, op0=ALU.add, op1=ALU.mult),
          reads=(el,), writes=(eln,))
    W8 = max(NB, 8)
    gms = kb.rot('gm', [128, W8], F32, 2)
    for b in gms.bufs:
        kb.op('dve', lambda e: e.memset(b[:, :], -1e30), writes=(b,))
    mx = kb.rot('mx', [128, 8], F32, 2)
    sl = kb.rot('sl', [128, NB], F32, 2)
    km = kb.sb('km', [128, NB], F32)
    kmb = kb.sb('kmb', [128, NB], BF16)
    selT = kb.rot('selT', [NB, S], BF16, 2)
    kfull = kb.rot('kfull', [128, S], BF16, 2)
    qfull = kb.rot('qfull', [128, S], BF16, 2)
    for h in range(MH):
        kf = kfull.next()
        qf = qfull.next()
        kb.load('sp', kf, [(kf[:, :], mkT[h * 128:(h + 1) * 128, :])])
        kb.load('sp', qf, [(qf[:, :], mqT[h * 128:(h + 1) * 128, :])])
        kb.op('dve', lambda e: e.tensor_reduce(out=km[:, :], in_=kf[:, :].rearrange("p (n k) -> p n k", n=NB), axis=AX.X, op=ALU.add),
              reads=(kf,), writes=(km,))
        kb.op('act', lambda e: e.activation(out=kmb[:, :], in_=km[:, :], func=AF.Identity, scale=1.0 / c['MBLK']), reads=(km,), writes=(kmb,))
        st = selT.next()
        for qt in range(NT):
            g_ps = A['s_ps'].next()
            kb.op('pe', lambda e: e.matmul(g_ps[:, :NB], lhsT=qf[:, qt * 128:(qt + 1) * 128], rhs=kmb[:, :], start=True, stop=True),
                  reads=(qf, kmb), writes=(g_ps,))
            gm = gms.next()
            kb.op('dve', lambda e: e.tensor_tensor(out=gm[:, :NB], in0=g_ps[:, :NB], in1=el[:, qt, :], op=ALU.mult), reads=(g_ps, el), writes=(gm,))
            kb.op('dve', lambda e: e.tensor_tensor(out=gm[:, :NB], in0=gm[:, :NB], in1=eln[:, qt, :], op=ALU.add), reads=(gm, eln), writes=(gm,))
            m = mx.next()
            kb.op('dve', lambda e: e.max(out=m[:, :], in_=gm[:, :]), reads=(gm,), writes=(m,))
            s = sl.next()
            kk = min(c['MTOPK'], NB) - 1
            kb.op('dve', lambda e: e.scalar_tensor_tensor(out=s[:, :], in0=gm[:, :NB], scalar=m[:, kk:kk + 1], in1=el[:, qt, :],
                                                          op0=ALU.is_ge, op1=ALU.mult), reads=(gm, m, el), writes=(s,))
            t_ps = A['m_ps'].next()
            kb.op('pe', lambda e: e.matmul(t_ps[:NB, :128], lhsT=s[:, :], rhs=C['ident_f32'][:, :], start=True, stop=True),
                  reads=(s, C['ident_f32']), writes=(t_ps,))
            kb.op('act', lambda e: e.activation(out=st[:, qt * 128:(qt + 1) * 128], in_=t_ps[:NB, :128], func=AF.Identity),
                  reads=(t_ps,), writes=(st,))
        attn_head(kb, C, c, [(mqT[h * 128:(h + 1) * 128, :], mkT[h * 128:(h + 1) * 128, :], 128)], mv[:, h * 128:(h + 1) * 128], 128,
                  ocT[h * 128:(h + 1) * 128, :], 128 ** -0.5, moba_sl[h], 'moba', A, selT=st)
    kb.end()


def swa_phase(kb, C, c, ins, l, wqT, wkT, wv, ocT, posf):
    S, WH, WKV, WD = c['S'], c['WH'], c['WKV'], c['WD']
    swa_sl, moba_sl = alibi_slopes(c)
    kb.begin()
    A = attn_bufs(kb, c, 1, [WD], True, posf)
    A['esink'] = kb.sb('esink', [128, WH], F32)
    kb.load('sp', A['esink'], [(A['esink'][:, :], ins['sinks'][l:l + 1, :].to_broadcast([128, WH]))])
    kb.op('act', lambda e: e.activation(out=A['esink'][:, :], in_=A['esink'][:, :], func=AF.Exp), reads=(A['esink'],), writes=(A['esink'],))
    R = WH // WKV
    base = 3 * c['BW']
    for h in range(WH):
        g = h // R
        attn_head(kb, C, c, [(wqT[h * WD:(h + 1) * WD, :], wkT[g * WD:(g + 1) * WD, :], WD)], wv[:, g * WD:(g + 1) * WD], WD,
                  ocT[base + h * WD:base + (h + 1) * WD, :], WD ** -0.5, swa_sl[h], 'swa', A, sinkcol=A['esink'][:, h:h + 1])
    kb.end()


def mla_phase(kb, C, c, ins, l, qlT, kvlT, krT, krswT, ocT, posf):
    S, LH, QL, KVL, RP = c['S'], c['LH'], c['QL'], c['KVL'], c['ROPE']
    qnT = kb.dram([QL, S], BF16)
    kvnT = kb.dram([KVL, S], BF16)
    kb.begin()
    gq = kb.sb('gq', [128, QL // 128], F32)
    gk = kb.sb('gk', [128, KVL // 128], F32)
    kb.load('sp', gq, [(gq[:, :], ins['qn_pk'][l])])
    kb.load('sp', gk, [(gk[:, :], ins['kvn_pk'][l])])
    kb.begin(); rmsnorm_fm(kb, C, qlT, qnT, QL, S, gq, None); kb.end()
    kb.begin(); rmsnorm_fm(kb, C, kvlT, kvnT, KVL, S, gk, None); kb.end()
    kb.end()
    qnopeT = kb.dram([LH * 128, S], BF16)
    qr_raw = kb.dram([LH * RP, S], F32)
    qr_sw = kb.dram([LH * RP, S], F32)
    knopeT = kb.dram([LH * 128, S], BF16)
    vtm = kb.dram([S, LH * 128], BF16)
    wq = ins['wq_perm'][l]
    wkv = ins['wkv_perm'][l]
    n1 = LH * 128
    n2 = LH * RP
    kb.begin(); linear(kb, qnT, QL, S, lambda n0, ns: wq[:, n0:n0 + ns], n1, store_epi(kb, qnopeT, BF16)); kb.end()
    kb.begin(); linear(kb, qnT, QL, S, lambda n0, ns: wq[:, n1 + n0:n1 + n0 + ns], n2, store_epi(kb, qr_raw, F32)); kb.end()
    kb.begin(); linear(kb, qnT, QL, S, lambda n0, ns: wq[:, n1 + n2 + n0:n1 + n2 + n0 + ns], n2, store_epi(kb, qr_sw, F32)); kb.end()
    kb.begin(); linear(kb, kvnT, KVL, S, lambda n0, ns: wkv[:, n0:n0 + ns], n1, store_epi(kb, knopeT, BF16)); kb.end()
    kb.begin(); linear(kb, kvnT, KVL, S, lambda n0, ns: wkv[:, n1 + n0:n1 + n0 + ns], n1, store_epi(kb, vtm, BF16, tm=True), tm=True); kb.end()
    qrT = kb.dram([LH * RP, S], BF16)
    krotT = kb.dram([RP, S], BF16)
    kb.begin()
    pq = kb.sb('pq', [128, S], F32)
    kb.load('sp', pq, [(pq[:, :], posf[0:1, :].to_broadcast([128, S]))])
    rc = kb.sb('ropec', [128, 2], F32)
    kb.load('sp', rc, [(rc[:, :], ins['ropec'])])
    cs = kb.sb('cos', [128, S], F32)
    sn = kb.sb('sin', [128, S], F32)
    kb.op('dve', lambda e: e.tensor_scalar(out=pq[:, :], in0=pq[:, :], scalar1=rc[:, 0:1], scalar2=None, op0=ALU.mult), reads=(pq, rc), writes=(pq,))
    ki = kb.sb('ki', [128, S], I32)
    kf = kb.sb('kf', [128, S], F32)
    yy = kb.sb('yy', [128, S], F32)
    for tb, sh in ((sn, 0.0), (cs, 0.5 * math.pi)):
        kb.op('dve', lambda e: e.tensor_scalar(out=yy[:, :], in0=pq[:, :], scalar1=1.0 / (2 * math.pi), scalar2=sh / (2 * math.pi) + 0.5,
                                               op0=ALU.mult, op1=ALU.add), reads=(pq,), writes=(yy,))
        kb.op('dve', lambda e: e.tensor_copy(out=ki[:, :], in_=yy[:, :]), reads=(yy,), writes=(ki,))
        kb.op('dve', lambda e: e.tensor_copy(out=kf[:, :], in_=ki[:, :]), reads=(ki,), writes=(kf,))
        kb.op('dve', lambda e: e.tensor_tensor(out=yy[:, :], in0=kf[:, :], in1=yy[:, :], op=ALU.is_gt), reads=(kf, yy), writes=(yy,))
        kb.op('dve', lambda e: e.tensor_tensor(out=kf[:, :], in0=kf[:, :], in1=yy[:, :], op=ALU.subtract), reads=(kf, yy), writes=(kf,))
        kb.op('dve', lambda e: e.tensor_scalar(out=tb[:, :], in0=pq[:, :], scalar1=sh, scalar2=None, op0=ALU.add), reads=(pq,), writes=(tb,))
        kb.op('dve', lambda e: e.scalar_tensor_tensor(out=tb[:, :], in0=kf[:, :], scalar=-2 * math.pi, in1=tb[:, :], op0=ALU.mult, op1=ALU.add),
              reads=(kf, tb), writes=(tb,))
        kb.op('act', lambda e: e.activation(out=tb[:, :], in_=tb[:, :], func=AF.Sin), reads=(tb,), writes=(tb,))
    kb.op('dve', lambda e: e.tensor_scalar(out=sn[:, :], in0=sn[:, :], scalar1=rc[:, 1:2], scalar2=None, op0=ALU.mult), reads=(sn, rc), writes=(sn,))
    ra = kb.rot('ra', [128, 2, 512], F32, 2)
    ro = kb.rot('ro', [128, 512], BF16, 2)
    jobs = [(qr_raw[r0:r0 + 128, :], qr_sw[r0:r0 + 128, :], qrT[r0:r0 + 128, :], 128) for r0 in range(0, LH * RP, 128)]
    jobs.append((krT, krswT, krotT, RP))
    for a_ap, b_ap, o_ap, p in jobs:
        for t0 in range(0, S, 512):
            x = ra.next()
            kb.load('sp', x, [(x[:p, 0, :], a_ap[:, t0:t0 + 512]), (x[:p, 1, :], b_ap[:, t0:t0 + 512])])
            kb.op('dve', lambda e: e.tensor_tensor(out=x[:p, 0, :], in0=x[:p, 0, :], in1=cs[:p, t0:t0 + 512], op=ALU.mult), reads=(x, cs), writes=(x,))
            kb.op('dve', lambda e: e.tensor_tensor(out=x[:p, 1, :], in0=x[:p, 1, :], in1=sn[:p, t0:t0 + 512], op=ALU.mult), reads=(x, sn), writes=(x,))
            o = ro.next()
            kb.op('dve', lambda e: e.tensor_tensor(out=o[:p, :], in0=x[:p, 0, :], in1=x[:p, 1, :], op=ALU.add), reads=(x,), writes=(o,))
            kb.store('act', o, [(o_ap[:, t0:t0 + 512], o[:p, :])])
    kb.end()
    kb.begin()
    A = attn_bufs(kb, c, 2, [128, RP], False, posf)
    base = 2 * c['BW']
    for h in range(LH):
        attn_head(kb, C, c, [(qnopeT[h * 128:(h + 1) * 128, :], knopeT[h * 128:(h + 1) * 128, :], 128),
                             (qrT[h * RP:(h + 1) * RP, :], krotT, RP)], vtm[:, h * 128:(h + 1) * 128], 128,
                  ocT[base + h * 128:base + (h + 1) * 128, :], (c['NOPE'] + RP) ** -0.5, None, 'causal', A)
    kb.end()


def ssm_phase(kb, C, c, ins, l, zt, xbcT, dtr, ocT):
    S, SDI, SH, SG, SN, CC = c['S'], c['SDI'], c['SH'], c['SG'], c['SN'], c['CONVC'] // 128
    assert SN == 128 and c['SHD'] == 64
    NXC = SDI // 128
    HPG = SH // SG
    xcT = kb.dram([c['CONVC'], S], F32)
    kb.begin()
    cw = kb.sb('cw', [128, CC, 4], F32)
    cb = kb.sb('cb', [128, CC], F32)
    kb.load('sp', cw, [(cw[:, :, :], ins['conv_w_pk'][l])])
    kb.load('sp', cb, [(cb[:, :], ins['conv_b_pk'][l])])
    TT = 512
    ci = kb.rot('ci', [128, TT + 3], F32, 2)
    ca = kb.rot('ca', [128, TT], F32, 2)
    for cc in range(CC):
        for t0 in range(0, S, TT):
            u = ci.next()
            if t0 == 0:
                kb.op('dve', lambda e: e.memset(u[:, 0:3], 0.0), writes=(u,))
                kb.load('sp', u, [(u[:, 3:], xbcT[cc * 128:(cc + 1) * 128, 0:TT])])
            else:
                kb.load('sp', u, [(u[:, :], xbcT[cc * 128:(cc + 1) * 128, t0 - 3:t0 + TT])])
            a = ca.next()
            kb.op('dve', lambda e: e.tensor_scalar(out=a[:, :], in0=u[:, 0:TT], scalar1=cw[:, cc, 0:1], scalar2=None, op0=ALU.mult),
                  reads=(u, cw), writes=(a,))
            for k in range(1, 4):
                kb.op('dve', lambda e: e.scalar_tensor_tensor(out=a[:, :], in0=u[:, k:k + TT], scalar=cw[:, cc, k:k + 1], in1=a[:, :],
                                                              op0=ALU.mult, op1=ALU.add), reads=(u, cw, a), writes=(a,))
            kb.op('act', lambda e: e.activation(out=a[:, :], in_=a[:, :], func=AF.Silu, bias=cb[:, cc:cc + 1]), reads=(a, cb), writes=(a,))
            kb.store('act', a, [(xcT[cc * 128:(cc + 1) * 128, t0:t0 + TT], a[:, :])])
    kb.end()
    kb.begin()
    tri = C['masks']
    mneg = C['masks']
    ident = C['ident_f32']

    def bc(name, src, n):
        b = kb.sb(name, [128, n], F32)
        kb.load('sp', b, [(b[:, :], src.to_broadcast([128, n]))])
        return b
    dtb = bc('dtb', ins['dt_bias'][l:l + 1, :], SH)
    Abc = bc('Abc', ins['a_log'][l:l + 1, :], SH)
    kb.op('act', lambda e: e.activation(out=Abc[:, :], in_=Abc[:, :], func=AF.Exp), reads=(Abc,), writes=(Abc,))
    kb.op('dve', lambda e: e.tensor_scalar(out=Abc[:, :], in0=Abc[:, :], scalar1=-1.0, scalar2=None, op0=ALU.mult), reads=(Abc,), writes=(Abc,))
    Dbc = bc('Dbc', ins['d_skip'][l:l + 1, :], SH)
    nwb = bc('nwb', ins['ssm_norm'][l:l + 1, :], SDI)
    state = kb.sb('state', [128, SDI], F32)
    kb.op('dve', lambda e: e.memset(state[:, :], 0.0), writes=(state,))
    xcs = kb.rot('xc', [128, CC, 128], F32, 2)
    zs = kb.rot('z', [128, SDI], F32, 2)
    dts = kb.rot('dtr', [128, SH], F32, 2)
    pA = kb.rot('pA', [128, 512], F32, 3, psum=True)
    yps = [kb.ps('yps%d' % i, [128, 512], F32) for i in range((SDI + 511) // 512)]
    sps = [kb.ps('sps%d' % i, [128, 512], F32) for i in range((SDI + 511) // 512)]
    sm = {n: kb.sb(n, [128, SH], F32) for n in ('dt', 'a', 'acs', 'dec', 'cdec', 'tmp')}
    xs = kb.sb('xs', [128, SDI], F32)
    xdt = kb.sb('xdt', [128, SDI], F32)
    xdt_bf = kb.sb('xdtb', [128, SDI], BF16)
    xdec_bf = kb.sb('xdec', [128, SDI], BF16)
    st_bf = kb.sb('stb', [128, SDI], BF16)
    btm = kb.sb('btm', [128, SG, 128], BF16)
    BT = kb.sb('BT', [128, SG, 128], BF16)
    CT = kb.sb('CT', [128, SG, 128], BF16)
    gt = kb.sb('gt', [128, SG, 128], F32)
    arep = kb.rot('arep', [128, 128], F32, 2)
    dif = kb.rot('dif', [128, 128], F32, 2)
    edec = kb.rot('edec', [128, 128], F32, 2)
    MT = kb.rot('MT', [128, 128], BF16, 2)
    Cd = kb.rot('Cd', [128, 128], BF16, 2)
    ysb = kb.sb('ysb', [128, SDI], F32)
    gsb = kb.sb('gsb', [128, SDI], F32)
    ssq = kb.sb('ssq', [128, SG], F32)
    otb = kb.rot('otb', [128, NXC, 128], BF16, 2)
    v3 = lambda b: b[:, :].rearrange("p (h d) -> p h d", d=64)
    b3 = lambda b: b[:, :].unsqueeze(2).to_broadcast([128, SH, 64])
    GW = HPG * 64
    for ch in range(S // 128):
        t0 = ch * 128
        xc = xcs.next()
        kb.load('sp', xc, [(xc[:, :, :], xcT[:, t0:t0 + 128].rearrange("(cc p) t -> p cc t", p=128))])
        z = zs.next()
        kb.load('sp', z, [(z[:, :], zt[t0:t0 + 128, :])])
        dr = dts.next()
        kb.load('sp', dr, [(dr[:, :], dtr[t0:t0 + 128, :])])
        dt, a, acs, dec, cdec, tmp = (sm[n] for n in ('dt', 'a', 'acs', 'dec', 'cdec', 'tmp'))
        kb.op('dve', lambda e: e.tensor_tensor(out=dt[:, :], in0=dr[:, :], in1=dtb[:, :], op=ALU.add), reads=(dr, dtb), writes=(dt,))
        kb.op('act', lambda e: e.activation(out=dt[:, :], in_=dt[:, :], func=AF.Exp), reads=(dt,), writes=(dt,))
        kb.op('dve', lambda e: e.tensor_scalar(out=dt[:, :], in0=dt[:, :], scalar1=1.0, scalar2=None, op0=ALU.add), reads=(dt,), writes=(dt,))
        kb.op('act', lambda e: e.activation(out=dt[:, :], in_=dt[:, :], func=AF.Ln), reads=(dt,), writes=(dt,))
        kb.op('dve', lambda e: e.tensor_tensor(out=a[:, :], in0=dt[:, :], in1=Abc[:, :], op=ALU.mult), reads=(dt, Abc), writes=(a,))
        p1 = pA.next()
        kb.op('pe', lambda e: e.matmul(p1[:, 0:SH], lhsT=tri[:, 0, 0:128], rhs=a[:, :], start=True, stop=True), reads=(tri, a), writes=(p1,))
        kb.op('pe', lambda e: e.matmul(p1[:, 256:256 + SH], lhsT=C['ones_f32'][:, :], rhs=a[:, :], start=True, stop=True),
              reads=(C['ones_f32'], a), writes=(p1,))
        kb.op('act', lambda e: e.activation(out=acs[:, :], in_=p1[:, 0:SH], func=AF.Identity), reads=(p1,), writes=(acs,))
        kb.op('dve', lambda e: e.tensor_tensor(out=tmp[:, :], in0=p1[:, 256:256 + SH], in1=acs[:, :], op=ALU.subtract), reads=(p1, acs), writes=(tmp,))
        kb.op('act', lambda e: e.activation(out=dec[:, :], in_=tmp[:, :], func=AF.Exp), reads=(tmp,), writes=(dec,))
        kb.op('act', lambda e: e.activation(out=cdec[:, :], in_=p1[:, 256:256 + SH], func=AF.Exp), reads=(p1,), writes=(cdec,))
        for j0 in range(0, NXC, 4):
            pt = pA.next()
            nj = min(4, NXC - j0)
            for j in range(j0, j0 + nj):
                kb.op('pe', lambda e: e.matmul(pt[:, (j - j0) * 128:(j - j0 + 1) * 128], lhsT=xc[:, j, :], rhs=ident[:, :], start=True, stop=True),
                      reads=(xc, ident), writes=(pt,))
            kb.op('act', lambda e: e.activation(out=xs[:, j0 * 128:(j0 + nj) * 128], in_=pt[:, :nj * 128], func=AF.Identity), reads=(pt,), writes=(xs,))
        pt = pA.next()
        for g in range(SG):
            kb.op('pe', lambda e: e.matmul(pt[:, g * 128:(g + 1) * 128], lhsT=xc[:, NXC + g, :], rhs=ident[:, :], start=True, stop=True),
                  reads=(xc, ident), writes=(pt,))
        kb.op('act', lambda e: e.activation(out=btm[:, :, :].rearrange("p g n -> p (g n)"), in_=pt[:, :SG * 128], func=AF.Identity), reads=(pt,), writes=(btm,))
        kb.op('dve', lambda e: e.tensor_tensor(out=v3(xdt), in0=v3(xs), in1=b3(dt), op=ALU.mult), reads=(xs, dt), writes=(xdt,))
        kb.op('act', lambda e: e.activation(out=xdt_bf[:, :], in_=xdt[:, :], func=AF.Identity), reads=(xdt,), writes=(xdt_bf,))
        kb.op('dve', lambda e: e.tensor_tensor(out=v3(xdec_bf), in0=v3(xdt), in1=b3(dec), op=ALU.mult), reads=(xdt, dec), writes=(xdec_bf,))
        kb.op('act', lambda e: e.activation(out=BT[:, :, :], in_=xc[:, NXC:NXC + SG, :], func=AF.Identity), reads=(xc,), writes=(BT,))
        kb.op('act', lambda e: e.activation(out=CT[:, :, :], in_=xc[:, NXC + SG:NXC + 2 * SG, :], func=AF.Identity), reads=(xc,), writes=(CT,))
        pg = pA.next()
        for g in range(SG):
            kb.op('pe', lambda e: e.matmul(pg[:, g * 128:(g + 1) * 128], lhsT=BT[:, g, :], rhs=CT[:, g, :], start=True, stop=True),
                  reads=(BT, CT), writes=(pg,))
        kb.op('act', lambda e: e.activation(out=gt[:, :, :].rearrange("p g n -> p (g n)"), in_=pg[:, :SG * 128], func=AF.Identity), reads=(pg,), writes=(gt,))
        kb.op('act', lambda e: e.activation(out=st_bf[:, :], in_=state[:, :], func=AF.Identity), reads=(state,), writes=(st_bf,))
        pbs = {}

        def ssm_a(h):
            ar = arep.next()
            kb.op('dve', lambda e: e.tensor_copy(out=ar[:, :], in_=a[:, h:h + 1].to_broadcast([128, 128])), reads=(a,), writes=(ar,))
            pbh = pA.next()
            kb.op('pe', lambda e: e.matmul(pbh[:, 0:128], lhsT=ar[:, :], rhs=tri[:, 0, 0:128], start=True, stop=True), reads=(ar, tri), writes=(pbh,))
            pbs[h] = pbh
        ssm_a(0)
        for h in range(SH):
            g = h // HPG
            if h + 1 < SH:
                ssm_a(h + 1)
            pb = pbs.pop(h)
            df = dif.next()
            kb.op('dve', lambda e: e.scalar_tensor_tensor(out=df[:, :], in0=pb[:, 0:128], scalar=acs[:, h:h + 1], in1=mneg[:, 3, 0:128],
                                                          op0=ALU.subtract, op1=ALU.add), reads=(pb, acs, mneg), writes=(df,))
            kb.op('act', lambda e: e.activation(out=df[:, :], in_=df[:, :], func=AF.Exp), reads=(df,), writes=(df,))
            mt = MT.next()
            kb.op('dve', lambda e: e.tensor_tensor(out=mt[:, :], in0=df[:, :], in1=gt[:, g, :], op=ALU.mult), reads=(df, gt), writes=(mt,))
            ed = edec.next()
            kb.op('act', lambda e: e.activation(out=ed[:, :], in_=pb[:, 0:128], func=AF.Exp), reads=(pb,), writes=(ed,))
            cd = Cd.next()
            kb.op('dve', lambda e: e.tensor_tensor(out=cd[:, :], in0=xc[:, NXC + SG + g, :], in1=ed[:, :], op=ALU.mult), reads=(xc, ed), writes=(cd,))
            yp = yps[(h * 64) // 512]
            yc = (h * 64) % 512
            kb.op('pe', lambda e: e.matmul(yp[:, yc:yc + 64], lhsT=mt[:, :], rhs=xdt_bf[:, h * 64:(h + 1) * 64], start=True, stop=False),
                  reads=(mt, xdt_bf), writes=(yp,))
            kb.op('pe', lambda e: e.matmul(yp[:, yc:yc + 64], lhsT=cd[:, :], rhs=st_bf[:, h * 64:(h + 1) * 64], start=False, stop=True),
                  reads=(cd, st_bf), writes=(yp,))
        for g in range(SG):
            sp_ = sps[(g * GW) // 512]
            sc = (g * GW) % 512
            kb.op('pe', lambda e: e.matmul(sp_[:, sc:sc + GW], lhsT=btm[:, g, :], rhs=xdec_bf[:, g * GW:(g + 1) * GW], start=True, stop=True),
                  reads=(btm, xdec_bf), writes=(sp_,))
        kb.op('dve', lambda e: e.tensor_tensor(out=v3(state), in0=v3(state), in1=b3(cdec), op=ALU.mult), reads=(state, cdec), writes=(state,))
        for i, sp_ in enumerate(sps):
            w = min(512, SDI - i * 512)
            kb.op('dve', lambda e: e.tensor_tensor(out=state[:, i * 512:i * 512 + w], in0=state[:, i * 512:i * 512 + w], in1=sp_[:, :w], op=ALU.add),
                  reads=(state, sp_), writes=(state,))
        kb.op('dve', lambda e: e.tensor_tensor(out=v3(ysb), in0=v3(xs), in1=b3(Dbc), op=ALU.mult), reads=(xs, Dbc), writes=(ysb,))
        for i, yp in enumerate(yps):
            w = min(512, SDI - i * 512)
            kb.op('dve', lambda e: e.tensor_tensor(out=ysb[:, i * 512:i * 512 + w], in0=ysb[:, i * 512:i * 512 + w], in1=yp[:, :w], op=ALU.add),
                  reads=(ysb, yp), writes=(ysb,))
        kb.op('act', lambda e: e.activation(out=z[:, :], in_=z[:, :], func=AF.Silu), reads=(z,), writes=(z,))
        kb.op('dve', lambda e: e.tensor_tensor(out=gsb[:, :], in0=ysb[:, :], in1=z[:, :], op=ALU.mult), reads=(ysb, z), writes=(gsb,))
        kb.op('dve', lambda e: e.tensor_tensor(out=ysb[:, :], in0=gsb[:, :], in1=gsb[:, :], op=ALU.mult), reads=(gsb,), writes=(ysb,))
        kb.op('dve', lambda e: e.tensor_reduce(out=ssq[:, :], in_=ysb[:, :].rearrange("p (g k) -> p g k", g=SG), axis=AX.X, op=ALU.add),
              reads=(ysb,), writes=(ssq,))
        kb.op('dve', lambda e: e.tensor_scalar(out=ssq[:, :], in0=ssq[:, :], scalar1=float(SG) / SDI, scalar2=1e-6, op0=ALU.mult, op1=ALU.add),
              reads=(ssq,), writes=(ssq,))
        kb.op('act', lambda e: e.activation(out=ssq[:, :], in_=ssq[:, :], func=AF.Ln), reads=(ssq,), writes=(ssq,))
        kb.op('act', lambda e: e.activation(out=ssq[:, :], in_=ssq[:, :], func=AF.Exp, scale=-0.5), reads=(ssq,), writes=(ssq,))
        kb.op('dve', lambda e: e.tensor_tensor(out=gsb[:, :].rearrange("p (g k) -> p g k", g=SG), in0=gsb[:, :].rearrange("p (g k) -> p g k", g=SG),
                                               in1=ssq[:, :].unsqueeze(2).to_broadcast([128, SG, SDI // SG]), op=ALU.mult), reads=(gsb, ssq), writes=(gsb,))
        kb.op('dve', lambda e: e.tensor_tensor(out=gsb[:, :], in0=gsb[:, :], in1=nwb[:, :], op=ALU.mult), reads=(gsb, nwb), writes=(gsb,))
        ot = otb.next()
        for j0 in range(0, NXC, 4):
            pt = pA.next()
            nj = min(4, NXC - j0)
            for j in range(j0, j0 + nj):
                kb.op('pe', lambda e: e.matmul(pt[:, (j - j0) * 128:(j - j0 + 1) * 128], lhsT=gsb[:, j * 128:(j + 1) * 128], rhs=ident[:, :], start=True, stop=True),
                      reads=(gsb, ident), writes=(pt,))
            kb.op('act', lambda e: e.activation(out=ot[:, j0:j0 + nj, :].rearrange("p j t -> p (j t)"), in_=pt[:, :nj * 128], func=AF.Identity),
                  reads=(pt,), writes=(ot,))
        kb.store('act', ot, [(ocT[c['BW']:c['BW'] + SDI, t0:t0 + 128].rearrange("(j p) t -> p j t", p=128), ot[:, :, :])])
    kb.end()


def win_phase(kb, C, c, ins, l, hT):
    D, S = c['D'], c['S']
    wt = ins['w_in_t']
    o = {}

    def seg(name, dt, tm=False, func=None):
        c0, n_, go = c['WSEG'][name]
        dst = kb.dram([S, n_] if tm else [n_, S], dt)
        kb.begin()
        linear(kb, hT, D, S, lambda n0, ns: wt[l, go + n0 // TNW][:, :, :ns], n_, store_epi(kb, dst, dt, func=func, tm=tm), tm=tm, NW=TNW)
        kb.end()
        o[name] = dst
    seg('mqT', BF16)
    seg('mkT', BF16)
    seg('mv', BF16, tm=True)
    seg('z', F32, tm=True)
    seg('xbcT', F32)
    seg('dtr', F32, tm=True)
    seg('qlT', F32)
    seg('kvlT', F32)
    seg('krT', F32)
    dst = kb.dram([c['ROPE'], S], F32)
    wk = ins['w_krsw'][l]
    kb.begin(); linear(kb, hT, D, S, lambda n0, ns: wk[:, n0:n0 + ns], c['ROPE'], store_epi(kb, dst, F32)); kb.end()
    o['krswT'] = dst
    seg('wqT', BF16)
    seg('wkT', BF16)
    seg('wv', BF16, tm=True)
    seg('gatesT', F32, func=AF.Sigmoid)
    return o


def merge_phase(kb, C, c, ins, l, ocT, gatesT, xT, g1, x1T):
    D, S, BW = c['D'], c['S'], c['BW']
    parts = [kb.dram([D, S], F32) for _ in range(4)]
    for r in range(4):
        kb.begin()
        gts = kb.rot('gt', [128, EB], F32, 2)
        obs = kb.rot('mo', [128, EB], F32, 2)

        class MergeEpi:
            def start(self, n0, ns, e0, es, r=r):
                self.g = gts.next()
                kb.load('sp', self.g, [(self.g[:ns, :es], gatesT[r * D + n0:r * D + n0 + ns, e0:e0 + es])])
                self.ob = obs.next()
                self.e0 = e0

            def tile(self, n0, ns, t0, ts, ps):
                g, ob, o = self.g, self.ob, t0 - self.e0
                kb.op('dve', lambda e: e.tensor_tensor(out=ob[:ns, o:o + ts], in0=ps[:ns, :ts], in1=g[:ns, o:o + ts], op=ALU.mult),
                      reads=(ps, g), writes=(ob,))

            def finish(self, n0, ns, e0, es, r=r):
                kb.store('act', self.ob, [(parts[r][n0:n0 + ns, e0:e0 + es], self.ob[:ns, :es])])
        epi = MergeEpi()
        wb = ins['w_branch'][l, r]
        linear(kb, ocT[r * BW:(r + 1) * BW, :], BW, S, lambda n0, ns: wb[:, n0:n0 + ns], D, epi)
        kb.end()
    mT = kb.dram([D, S], BF16)
    kb.begin(); ew_combine(kb, parts, mT, D, S, BF16); kb.end()
    kb.begin()
    xts = kb.rot('xr', [128, EB], F32, 2)
    obs = kb.rot('xo', [128, EB], F32, 2)

    class OutEpi:
        def start(self, n0, ns, e0, es):
            self.x = xts.next()
            kb.load('sp', self.x, [(self.x[:ns, :es], xT[n0:n0 + ns, e0:e0 + es])])
            self.ob = obs.next()
            self.e0 = e0

        def tile(self, n0, ns, t0, ts, ps):
            x, ob, o, fc = self.x, self.ob, t0 - self.e0, n0 // 128
            kb.op('dve', lambda e: e.scalar_tensor_tensor(out=ob[:ns, o:o + ts], in0=ps[:ns, :ts], scalar=g1[:ns, fc:fc + 1], in1=x[:ns, o:o + ts],
                                                          op0=ALU.mult, op1=ALU.add), reads=(ps, g1, x), writes=(ob,))

        def finish(self, n0, ns, e0, es):
            kb.store('act', self.ob, [(x1T[n0:n0 + ns, e0:e0 + es], self.ob[:ns, :es])])
    epi2 = OutEpi()
    wo = ins['w_out_t']
    linear(kb, mT, D, S, lambda n0, ns: wo[l, n0 // TNW][:, :, :ns], D, epi2, NW=TNW)
    kb.end()


def moe_phase(kb, C, c, ins, l, h2T, x1T, g2, x2T):
    D, S, NG, EPG, NE, EH = c['D'], c['S'], c['NG'], c['EPG'], c['NE'], c['EH']
    NR = NG + NE
    wTd = kb.dram([NE, S], F32)
    kb.begin()
    brt = kb.sb('brt', [128, NR], F32)
    kb.load('sp', brt, [(brt[:, :], ins['b_rt'][l:l + 1, :].to_broadcast([128, NR]))])
    WT = kb.sb('WT', [NE, S], F32)
    lg = kb.rot('lg', [128, NR], F32, 2)
    s1 = kb.rot('s1', [128, 8], F32, 2)
    mx8 = kb.rot('mx8', [128, 8], F32, 2)
    pen = kb.rot('pen', [128, NG], F32, 2)
    lm = kb.rot('lm', [128, NE], F32, 2)
    sel = kb.rot('sel', [128, NE], F32, 2)
    ex = kb.rot('exr', [128, NE], F32, 2)
    tps = kb.rot('tps', [128, 512], F32, 2, psum=True)
    junk = kb.rot('junk', [128, NG], F32, 2)

    def epi(n0, ns, t0, ts, ps):
        L_ = lg.next()
        kb.op('dve', lambda e: e.tensor_tensor(out=L_[:, :], in0=ps[:, :NR], in1=brt[:, :], op=ALU.add), reads=(ps, brt), writes=(L_,))
        s = s1.next()
        kb.op('dve', lambda e: e.tensor_reduce(out=s[:, 0:1], in_=L_[:, 0:NG], axis=AX.X, op=ALU.max), reads=(L_,), writes=(s,))
        kb.op('dve', lambda e: e.tensor_scalar(out=s[:, 1:2], in0=s[:, 0:1], scalar1=-1.0, scalar2=None, op0=ALU.mult), reads=(s,), writes=(s,))
        jk = junk.next()
        kb.op('act', lambda e: e.activation(out=jk[:, :], in_=L_[:, 0:NG], func=AF.Exp, bias=s[:, 1:2], accum_out=s[:, 2:3]), reads=(L_, s), writes=(jk, s))
        p = pen.next()
        kb.op('dve', lambda e: e.tensor_scalar(out=p[:, :], in0=L_[:, 0:NG], scalar1=s[:, 0:1], scalar2=None, op0=ALU.is_ge), reads=(L_, s), writes=(p,))
        kb.op('dve', lambda e: e.tensor_scalar(out=p[:, :], in0=p[:, :], scalar1=-1.0, scalar2=1e30, op0=ALU.add, op1=ALU.mult), reads=(p,), writes=(p,))
        m = lm.next()
        kb.op('dve', lambda e: e.tensor_tensor(out=m[:, :].rearrange("p (g k) -> p g k", g=NG), in0=L_[:, NG:NR].rearrange("p (g k) -> p g k", g=NG),
                                               in1=p[:, :].unsqueeze(2).to_broadcast([128, NG, EPG]), op=ALU.add), reads=(L_, p), writes=(m,))
        x8 = mx8.next()
        kb.op('dve', lambda e: e.max(out=x8[:, :], in_=m[:, :]), reads=(m,), writes=(x8,))
        sl = sel.next()
        kb.op('dve', lambda e: e.tensor_scalar(out=sl[:, :], in0=m[:, :], scalar1=x8[:, 1:2], scalar2=None, op0=ALU.is_ge), reads=(m, x8), writes=(sl,))
        kb.op('dve', lambda e: e.tensor_scalar(out=s[:, 4:5], in0=x8[:, 0:1], scalar1=-1.0, scalar2=None, op0=ALU.mult), reads=(x8, s), writes=(s,))
        e_ = ex.next()
        kb.op('act', lambda e: e.activation(out=e_[:, :], in_=m[:, :], func=AF.Exp, bias=s[:, 4:5]), reads=(m, s), writes=(e_,))
        kb.op('act', lambda e: e.activation(out=s[:, 5:6], in_=x8[:, 1:2], func=AF.Exp, bias=s[:, 4:5]), reads=(x8, s), writes=(s,))
        kb.op('dve', lambda e: e.scalar_tensor_tensor(out=s[:, 5:6], in0=s[:, 5:6], scalar=1.0, in1=s[:, 2:3], op0=ALU.add, op1=ALU.mult), reads=(s,), writes=(s,))
        kb.op('dve', lambda e: e.reciprocal(out=s[:, 3:4], in_=s[:, 5:6]), reads=(s,), writes=(s,))
        kb.op('dve', lambda e: e.scalar_tensor_tensor(out=e_[:, :], in0=e_[:, :], scalar=s[:, 3:4], in1=sl[:, :], op0=ALU.mult, op1=ALU.mult),
              reads=(e_, s, sl), writes=(e_,))
        tp = tps.next()
        kb.op('pe', lambda e: e.matmul(tp[:NE, :128], lhsT=e_[:, :], rhs=C['ident_f32'][:, :], start=True, stop=True), reads=(e_, C['ident_f32']), writes=(tp,))
        kb.op('act', lambda e: e.activation(out=WT[:, t0:t0 + 128], in_=tp[:NE, :128], func=AF.Identity), reads=(tp,), writes=(WT,))
    wr = ins['w_rt'][l]
    linear(kb, h2T, D, S, lambda n0, ns: wr[:, n0:n0 + ns], NR, epi, tm=True)
    kb.store('act', WT, [(wTd[:, :], WT[:, :])])
    kb.end()
    NWE = c['NWE']
    sgT = kb.dram([NE * EH, S], BF16)
    hidT = kb.dram([NE * EH, S], BF16)
    wg = ins['ewg_t']
    wu = ins['ewu_t']
    kb.begin()
    linear(kb, h2T, D, S, lambda n0, ns: wg[l, n0 // EH, (n0 % EH) // NWE][:, :, :ns], NE * EH, store_epi(kb, sgT, BF16, func=AF.Silu), NW=NWE)
    kb.end()
    kb.begin()
    sgs = kb.rot('sg', [128, EB], BF16, 2)
    wbs = kb.rot('wb', [128, EB], F32, 2)
    hos = kb.rot('ho', [128, EB], BF16, 2)
    tus = kb.rot('tu', [128, 512], F32, 2)

    class UpEpi:
        def start(self, n0, ns, e0, es):
            ei = n0 // EH
            self.wb = wbs.next()
            kb.load('sp', self.wb, [(self.wb[:, :es], wTd[ei:ei + 1, e0:e0 + es].to_broadcast([128, es]))])
            self.sg = sgs.next()
            kb.load('sp', self.sg, [(self.sg[:ns, :es], sgT[n0:n0 + ns, e0:e0 + es])])
            self.ho = hos.next()
            self.e0 = e0

        def tile(self, n0, ns, t0, ts, ps):
            wb, sg, ho, o = self.wb, self.sg, self.ho, t0 - self.e0
            tmpb = tus.next()
            kb.op('dve', lambda e: e.tensor_tensor(out=tmpb[:ns, :ts], in0=ps[:ns, :ts], in1=wb[:ns, o:o + ts], op=ALU.mult), reads=(ps, wb), writes=(tmpb,))
            kb.op('dve', lambda e: e.tensor_tensor(out=ho[:ns, o:o + ts], in0=tmpb[:ns, :ts], in1=sg[:ns, o:o + ts], op=ALU.mult), reads=(tmpb, sg), writes=(ho,))

        def finish(self, n0, ns, e0, es):
            kb.store('act', self.ho, [(hidT[n0:n0 + ns, e0:e0 + es], self.ho[:ns, :es])])
    linear(kb, h2T, D, S, lambda n0, ns: wu[l, n0 // EH, (n0 % EH) // NWE][:, :, :ns], NE * EH, UpEpi(), NW=NWE)
    kb.end()
    KG = EPG * EH
    parts = [kb.dram([D, S], F32) for _ in range(NG)]
    wd = ins['ewd_t']
    for g in range(NG):
        kb.begin()
        linear(kb, hidT[g * KG:(g + 1) * KG, :], KG, S, lambda n0, ns, g=g: wd[l, g, n0 // TNW][:, :, :ns], D, store_epi(kb, parts[g], F32), NW=TNW)
        kb.end()
    kb.begin(); ew_combine(kb, parts, x2T, D, S, F32, gate=g2, base=x1T); kb.end()


def in_specs(c):
    D, S, L = c['D'], c['S'], c['L']
    DC = D // 128
    NB = S // c['MBLK']
    CC = c['CONVC'] // 128
    sp = [
        ('xT', [D, S], F32), ('cT', [D, 1], F32), ('pos', [1, S], I32),
        ('ada_w_t', [L, 6 * D // TNW, 128, DC, TNW], F32), ('ada_b_pk', [L, 128, 6 * DC], F32),
        ('nm_pk', [L, 128, DC], F32), ('nf_pk', [L, 128, DC], F32), ('fin_pk', [128, DC], F32),
        ('w_in_t', [L, c['WG'], 128, DC, TNW], F32), ('w_krsw', [L, D, c['ROPE']], F32),
        ('conv_w_pk', [L, 128, CC, 4], F32), ('conv_b_pk', [L, 128, CC], F32),
        ('dt_bias', [L, c['SH']], F32), ('a_log', [L, c['SH']], F32), ('d_skip', [L, c['SH']], F32), ('ssm_norm', [L, c['SDI']], F32),
        ('qn_pk', [L, 128, c['QL'] // 128], F32), ('kvn_pk', [L, 128, c['KVL'] // 128], F32),
        ('wq_perm', [L, c['QL'], c['LH'] * (128 + 2 * c['ROPE'])], F32), ('wkv_perm', [L, c['KVL'], c['LH'] * 256], F32),
        ('sinks', [L, c['WH']], F32), ('w_branch', [L, 4, c['BW'], D], F32), ('w_out_t', [L, D // TNW, 128, DC, TNW], F32),
        ('w_rt', [L, D, c['NG'] + c['NE']], F32), ('b_rt', [L, c['NG'] + c['NE']], F32),
        ('ewg_t', [L, c['NE'], c['EH'] // c['NWE'], 128, DC, c['NWE']], F32), ('ewu_t', [L, c['NE'], c['EH'] // c['NWE'], 128, DC, c['NWE']], F32),
        ('ewd_t', [L, c['NG'], D // TNW, 128, c['EPG'] * c['EH'] // 128, TNW], F32),
        ('masks', [128, 4, 256], F32), ('ident', [128, 128], F32), ('elig', [S, NB], F32), ('esel', [NB, NB * 128], F32),
        ('ropec', [128, 2], F32),
    ]
    return sp


def build(cfg):
    c = derive(cfg)
    D, S, L = c['D'], c['S'], c['L']
    DC = D // 128
    NB = S // c['MBLK']
    nc = bass.Bass("TRN2", target_bir_lowering=False)
    ins = {n: nc.dram_tensor(n, sh, dt, kind="ExternalInput").ap() for n, sh, dt in in_specs(c)}
    outT = nc.dram_tensor("outT", [D, S], F32, kind="ExternalOutput").ap()
    kb = KB(nc)
    kb.begin()
    C = {}
    C['ones_f32'] = kb.sb('ones', [128, 128], F32)
    kb.op('dve', lambda e: e.memset(C['ones_f32'][:, :], 1.0), writes=(C['ones_f32'],))
    C['ones_bf'] = kb.sb('onesb', [128, 128], BF16)
    kb.op('dve', lambda e: e.memset(C['ones_bf'][:, :], 1.0), writes=(C['ones_bf'],))
    C['ident_f32'] = kb.sb('ident', [128, 128], F32)
    kb.load('sp', C['ident_f32'], [(C['ident_f32'][:, :], ins['ident'])])
    C['masks'] = kb.sb('masks', [128, 4, 256], F32)
    kb.load('sp', C['masks'], [(C['masks'][:, :, :], ins['masks'])])
    C['esel'] = kb.sb('esel', [NB, NB, 128], BF16)
    kb.load('pool', C['esel'], [(C['esel'][:, :, :], ins['esel'].rearrange("k (n m) -> k n m", m=128))])
    posf = kb.dram([1, S], F32)
    kb.begin()
    pi = kb.sb('posi', [1, S], I32)
    pf = kb.sb('posf', [1, S], F32)
    kb.load('sp', pi, [(pi[:, :], ins['pos'])])
    kb.op('dve', lambda e: e.tensor_copy(out=pf[:, :], in_=pi[:, :]), reads=(pi,), writes=(pf,))
    kb.store('act', pf, [(posf[:, :], pf[:, :])])
    kb.end()
    mods = []
    for l in range(L):
        m = kb.sb('mod%d' % l, [128, 6 * DC], F32)
        ab = kb.sb('adab%d' % l, [128, 6 * DC], F32)
        kb.load('sp', ab, [(ab[:, :], ins['ada_b_pk'][l])])
        kb.begin()

        def epi(n0, ns, t0, ts, ps, m=m, ab=ab):
            j = n0 // 128
            kb.op('dve', lambda e: e.tensor_tensor(out=m[:, j:j + 1], in0=ps[:, 0:1], in1=ab[:, j:j + 1], op=ALU.add), reads=(ps, ab), writes=(m,))
        aw = ins['ada_w_t']
        linear(kb, ins['cT'], D, 1, lambda n0, ns, l=l: aw[l, n0 // TNW][:, :, :ns], 6 * D, epi, xq='pool', NW=TNW)
        kb.end()
        nm = kb.sb('nm%d' % l, [128, DC], F32)
        nf = kb.sb('nf%d' % l, [128, DC], F32)
        kb.load('sp', nm, [(nm[:, :], ins['nm_pk'][l])])
        kb.load('sp', nf, [(nf[:, :], ins['nf_pk'][l])])
        gam1 = kb.sb('gam1_%d' % l, [128, DC], F32)
        gam2 = kb.sb('gam2_%d' % l, [128, DC], F32)
        kb.op('dve', lambda e: e.scalar_tensor_tensor(out=gam1[:, :], in0=m[:, DC:2 * DC], scalar=1.0, in1=nm[:, :], op0=ALU.add, op1=ALU.mult),
              reads=(m, nm), writes=(gam1,))
        kb.op('dve', lambda e: e.scalar_tensor_tensor(out=gam2[:, :], in0=m[:, 4 * DC:5 * DC], scalar=1.0, in1=nf[:, :], op0=ALU.add, op1=ALU.mult),
              reads=(m, nf), writes=(gam2,))
        sh1 = kb.sb('sh1_%d' % l, [128, DC], F32)
        g1 = kb.sb('g1_%d' % l, [128, DC], F32)
        sh2 = kb.sb('sh2_%d' % l, [128, DC], F32)
        g2 = kb.sb('g2_%d' % l, [128, DC], F32)
        for dst, k in ((sh1, 0), (g1, 2), (sh2, 3), (g2, 5)):
            kb.op('dve', lambda e: e.tensor_copy(out=dst[:, :], in_=m[:, k * DC:(k + 1) * DC]), reads=(m,), writes=(dst,))
        mods.append(dict(gam1=gam1, sh1=sh1, g1=g1, gam2=gam2, sh2=sh2, g2=g2))
    fin = kb.sb('fin', [128, DC], F32)
    kb.load('sp', fin, [(fin[:, :], ins['fin_pk'])])
    xT = ins['xT']
    for l in range(L):
        md = mods[l]
        hT = kb.dram([D, S], BF16)
        kb.begin(); rmsnorm_fm(kb, C, xT, hT, D, S, md['gam1'], md['sh1']); kb.end()
        o = win_phase(kb, C, c, ins, l, hT)
        ocT = kb.dram([4 * c['BW'], S], BF16)
        moba_phase(kb, C, c, ins, o['mqT'], o['mkT'], o['mv'], ocT, posf)
        ssm_phase(kb, C, c, ins, l, o['z'], o['xbcT'], o['dtr'], ocT)
        mla_phase(kb, C, c, ins, l, o['qlT'], o['kvlT'], o['krT'], o['krswT'], ocT, posf)
        swa_phase(kb, C, c, ins, l, o['wqT'], o['wkT'], o['wv'], ocT, posf)
        x1T = kb.dram([D, S], F32)
        merge_phase(kb, C, c, ins, l, ocT, o['gatesT'], xT, md['g1'], x1T)
        h2T = kb.dram([D, S], BF16)
        kb.begin(); rmsnorm_fm(kb, C, x1T, h2T, D, S, md['gam2'], md['sh2']); kb.end()
        x2T = kb.dram([D, S], F32)
        moe_phase(kb, C, c, ins, l, h2T, x1T, md['g2'], x2T)
        xT = x2T
    kb.begin(); rmsnorm_fm(kb, C, xT, outT, D, S, fin, None, dst_dt=F32); kb.end()
    kb.end()
    return nc


def host_consts(c):
    S = c['S']
    NB = S // c['MBLK']
    p = np.arange(128)[:, None]
    q = np.arange(256)[None, :]
    masks = np.zeros((128, 4, 256), np.float32)
    masks[:, 0] = (p <= q)
    masks[:, 1] = (128 + p <= q)
    masks[:, 2, :128] = (p > q[:, :128])
    masks[:, 3] = np.where(p <= q, 0.0, NEG)
    ident = np.eye(128, dtype=np.float32)
    qb = (np.arange(S) // c['MBLK'])[:, None]
    elig = (np.arange(NB)[None, :] < qb).astype(np.float32)
    esel = np.zeros((NB, NB, 128), np.float32)
    for n in range(NB):
        esel[n, n, :] = 1.0
    half = c['ROPE'] // 2
    inv = (np.float32(10000.0) ** (-np.arange(half, dtype=np.float32) / np.float32(half))).astype(np.float32)
    pp = np.arange(128) % c['ROPE']
    ropec = np.stack([inv[pp % half], np.where(pp < half, -1.0, 1.0)], 1).astype(np.float32)
    return dict(masks=masks, ident=ident, elig=elig, esel=esel.reshape(NB, NB * 128), ropec=ropec)


def pk(v):
    v = np.asarray(v)
    return np.ascontiguousarray(np.swapaxes(v.reshape(v.shape[:-1] + (-1, 128)), -1, -2))


def tile_w(W, NW):
    W = np.asarray(W)
    K, N = W.shape[-2:]
    G = -(-N // NW)
    if G * NW != N:
        W = np.concatenate([W, np.zeros(W.shape[:-1] + (G * NW - N,), W.dtype)], -1)
    W = W.reshape(W.shape[:-2] + (K // 128, 128, G, NW))
    nd = W.ndim
    perm = tuple(range(nd - 4)) + (nd - 2, nd - 3, nd - 4, nd - 1)
    return np.ascontiguousarray(W.transpose(perm))


def prep_shared(inp, c):
    L = c['L']
    off = c['IN_OFF']
    half = c['ROPE'] // 2
    LH, RP = c['LH'], c['ROPE']
    w_in = np.asarray(inp['w_in'])
    kr = w_in[:, :, off[6]:off[7]]
    d = {}
    d['ada_w_t'] = tile_w(inp['ada_w'], TNW)
    d['ada_b_pk'] = pk(inp['ada_b'])
    d['nm_pk'] = pk(inp['norm_mix'])
    d['nf_pk'] = pk(inp['norm_ffn'])
    d['fin_pk'] = pk(inp['final_norm'])
    d['w_in_t'] = np.concatenate([tile_w(w_in[:, :, c0:c0 + n], TNW) for (c0, n, go) in c['WSEG'].values()], 1)
    d['w_krsw'] = np.ascontiguousarray(np.concatenate([kr[:, :, half:], kr[:, :, :half]], -1))
    CC = c['CONVC'] // 128
    cw = np.asarray(inp['conv_w'])
    d['conv_w_pk'] = np.ascontiguousarray(cw.reshape(L, 4, CC, 128).transpose(0, 3, 2, 1))
    d['conv_b_pk'] = pk(inp['conv_b'])
    for k in ('dt_bias', 'a_log', 'd_skip', 'ssm_norm'):
        d[k] = np.asarray(inp[k])
    d['qn_pk'] = pk(inp['mla_q_norm'])
    d['kvn_pk'] = pk(inp['mla_kv_norm'])
    wq = np.asarray(inp['mla_wq_b']).reshape(L, c['QL'], LH, c['NOPE'] + RP)
    nope = wq[..., :c['NOPE']].reshape(L, c['QL'], -1)
    rope = wq[..., c['NOPE']:]
    rsw = np.concatenate([rope[..., half:], rope[..., :half]], -1)
    d['wq_perm'] = np.ascontiguousarray(np.concatenate([nope, rope.reshape(L, c['QL'], -1), rsw.reshape(L, c['QL'], -1)], -1))
    wkv = np.asarray(inp['mla_wkv_b']).reshape(L, c['KVL'], LH, c['NOPE'] + c['LV'])
    d['wkv_perm'] = np.ascontiguousarray(np.concatenate([wkv[..., :c['NOPE']].reshape(L, c['KVL'], -1),
                                                         wkv[..., c['NOPE']:].reshape(L, c['KVL'], -1)], -1))
    d['sinks'] = np.asarray(inp['swa_sinks'])
    d['w_branch'] = np.asarray(inp['w_branch'])
    d['w_out_t'] = tile_w(inp['w_out'], TNW)
    d['w_rt'] = np.ascontiguousarray(np.concatenate([inp['router_group_w'], inp['router_w']], -1))
    d['b_rt'] = np.ascontiguousarray(np.concatenate([inp['router_group_b'], inp['router_b']], -1))
    d['ewg_t'] = tile_w(inp['exp_w_gate'], c['NWE'])
    d['ewu_t'] = tile_w(inp['exp_w_up'], c['NWE'])
    ed = np.asarray(inp['exp_w_down'])
    d['ewd_t'] = tile_w(ed.reshape(L, c['NG'], c['EPG'] * c['EH'], c['D']), TNW)
    d.update(host_consts(c))
    return d


_NC_CACHE = {}


def run(inp, cfg):
    c = derive(cfg)
    key = tuple(sorted((k, v) for k, v in cfg.items()))
    if key not in _NC_CACHE:
        _NC_CACHE[key] = build(cfg)
    nc = _NC_CACHE[key]
    shared = prep_shared(inp, c)
    x = np.asarray(inp['x'])
    B = x.shape[0]
    maps = []
    for b in range(B):
        m = dict(shared)
        m['xT'] = np.ascontiguousarray(x[b].T)
        m['cT'] = np.ascontiguousarray(np.asarray(inp['c'])[b][:, None])
        m['pos'] = np.ascontiguousarray(np.asarray(inp['positions'])[b][None, :].astype(np.int32))
        maps.append(m)
    res = run_bass_kernel_spmd(nc, maps, core_ids=list(range(B)))
    out = np.stack([np.ascontiguousarray(res.results[b]['outT'].T) for b in range(B)], 0)
    return out.astype(np.float32)


def kernel(**inputs):
    return run(inputs, FULL)
```

```python
import math
from contextlib import ExitStack
import numpy as np
import concourse.bass as bass
import concourse.mybir as mybir
from concourse.bass_utils import run_bass_kernel_spmd

F32 = mybir.dt.float32
BF16 = mybir.dt.bfloat16
I32 = mybir.dt.int32
AF = mybir.ActivationFunctionType
ALU = mybir.AluOpType
AX = mybir.AxisListType

FULL = dict(D=4096, S=4096, L=2, MH=8, MBLK=256, MTOPK=3, SDI=1024, SHD=64, SG=2, SN=128, SCONV=4,
            LH=8, QL=768, KVL=512, NOPE=128, ROPE=64, LV=128, WH=16, WKV=2, WD=64, WW=128, BW=1024,
            NG=4, EPG=8, EH=512, B=2)
NEG = -30000.0
TNW = 256


def derive(c):
    c = dict(c)
    c['SH'] = c['SDI'] // c['SHD']
    c['CONVC'] = c['SDI'] + 2 * c['SG'] * c['SN']
    c['NE'] = c['NG'] * c['EPG']
    sizes = [3 * c['MH'] * 128, c['SDI'], c['CONVC'], c['SH'], c['QL'], c['KVL'], c['ROPE'],
             c['WH'] * c['WD'], c['WKV'] * c['WD'], c['WKV'] * c['WD'], 4 * c['D']]
    offs = np.concatenate([[0], np.cumsum(sizes)]).tolist()
    c['IN_OFF'] = offs
    c['N_IN'] = offs[-1]
    c['NALIBI'] = c['MH'] + c['WH']
    M = c['MH'] * 128
    segs = [('mqT', offs[0], M), ('mkT', offs[0] + M, M), ('mv', offs[0] + 2 * M, M)]
    for nm, i in (('z', 1), ('xbcT', 2), ('dtr', 3), ('qlT', 4), ('kvlT', 5), ('krT', 6), ('wqT', 7), ('wkT', 8), ('wv', 9), ('gatesT', 10)):
        segs.append((nm, offs[i], sizes[i]))
    go = 0
    tab = {}
    for nm, c0, n in segs:
        tab[nm] = (c0, n, go)
        go += -(-n // TNW)
    c['WSEG'] = tab
    c['WG'] = go
    c['NWE'] = min(TNW, c['EH'])
    return c


class Buf:
    __slots__ = ('t', 'w', 'r', 'ds')

    def __init__(self, t):
        self.t = t
        self.w = None
        self.r = {}
        self.ds = None

    def __getitem__(self, k):
        return self.t[k]


class Rot:
    def __init__(self, bufs):
        self.bufs = bufs
        self.i = 0

    def next(self):
        b = self.bufs[self.i % len(self.bufs)]
        self.i += 1
        return b


class KB:
    def __init__(self, nc):
        self.nc = nc
        self.eng = {'pe': nc.tensor, 'act': nc.scalar, 'dve': nc.vector, 'pool': nc.gpsimd, 'sp': nc.sync}
        self.sems = {}
        self.cnt = {}
        for k in self.eng:
            self.sems[k] = nc.alloc_semaphore('e_' + k)
            self.cnt[k] = 0
        self.seen = {k: {} for k in self.eng}
        self.free_ds = []
        self.nds = 0
        self.scopes = []
        self.uid = 0
        self.ndram = 0

    def begin(self):
        self.scopes.append((ExitStack(), []))

    def end(self):
        self.barrier()
        st, bufs = self.scopes.pop()
        for b in bufs:
            if b.ds is not None:
                self.free_ds.append(b.ds)
                b.ds = None
        st.close()

    def dram(self, shape, dt):
        self.ndram += 1
        return self.nc.dram_tensor(f'scr{self.ndram}', list(shape), dt).ap()

    def sb(self, name, shape, dt):
        self.uid += 1
        st, bufs = self.scopes[-1]
        t = st.enter_context(self.nc.sbuf_tensor(f'{name}_{self.uid}', list(shape), dt))
        b = Buf(t)
        bufs.append(b)
        return b

    def ps(self, name, shape, dt=F32):
        self.uid += 1
        st, bufs = self.scopes[-1]
        t = st.enter_context(self.nc.psum_tensor(f'{name}_{self.uid}', list(shape), dt))
        b = Buf(t)
        bufs.append(b)
        return b

    def rot(self, name, shape, dt, n, psum=False):
        return Rot([(self.ps if psum else self.sb)(f'{name}{i}', shape, dt) for i in range(n)])

    def _dsem(self, b):
        if b.ds is None:
            if self.free_ds:
                b.ds = self.free_ds.pop()
            else:
                k = f'd{self.nds}'
                self.nds += 1
                self.sems[k] = self.nc.alloc_semaphore(k)
                self.cnt[k] = 0
                b.ds = k
        return b.ds

    def _need(self, e, dep, raw=True):
        if dep is None:
            return
        k, v = dep
        if k == e and (e == 'pe' or not raw):
            return
        if self.seen[e].get(k, 0) >= v:
            return
        self.eng[e].wait_ge(self.sems[k], v)
        self.seen[e][k] = v

    def _deps(self, e, reads, writes):
        for b in reads:
            self._need(e, b.w, raw=True)
        for b in writes:
            self._need(e, b.w, raw=False)
            for k, v in b.r.items():
                self._need(e, (k, v), raw=False)

    def op(self, e, ins_fn, reads=(), writes=()):
        self._deps(e, reads, writes)
        ins = ins_fn(self.eng[e])
        self.cnt[e] += 1
        ins.then_inc(self.sems[e], 1)
        v = self.cnt[e]
        for b in reads:
            if b.r.get(e, 0) < v:
                b.r[e] = v
        for b in writes:
            b.w = (e, v)
            b.r = {}
        return ins

    def load(self, q, b, pairs, **kw):
        self._deps(q, (), (b,))
        ds = self._dsem(b)
        for o, i in pairs:
            self.eng[q].dma_start(out=o, in_=i, **{'allow_slow_non_contiguous': True, **kw}).then_inc(self.sems[ds], 16)
            self.cnt[ds] += 16
        b.w = (ds, self.cnt[ds])
        b.r = {}

    def store(self, q, b, pairs, **kw):
        self._deps(q, (b,), ())
        ds = self._dsem(b)
        for o, i in pairs:
            self.eng[q].dma_start(out=o, in_=i, **{'allow_slow_non_contiguous': True, **kw}).then_inc(self.sems[ds], 16)
            self.cnt[ds] += 16
        b.r[ds] = self.cnt[ds]

    def barrier(self):
        for e in self.eng:
            for k, v in self.cnt.items():
                if k != e and v > 0:
                    self._need(e, (k, v), raw=False)


class FnEpi:
    def __init__(self, fn):
        self.fn = fn

    def start(self, n0, ns, e0, es):
        pass

    def tile(self, n0, ns, t0, ts, ps):
        self.fn(n0, ns, t0, ts, ps)

    def finish(self, n0, ns, e0, es):
        pass


EB = 1024
TR_CAP = None


def linear(kb, XT, K, T, Wfn, N, epi, tm=False, NW=None, wq='pool', xq='sp'):
    if not hasattr(epi, 'tile'):
        epi = FnEpi(epi)
    KC = K // 128
    assert K % 128 == 0
    if NW is None:
        NW = 256 if KC >= 32 else 512
    TR = min(T, max(512, (65536 // KC) // 512 * 512))
    if TR_CAP:
        TR = min(TR, TR_CAP)
    wts = kb.rot('lw', [128, KC, NW], BF16, 2)
    xres = kb.sb('lx', [128, KC, TR], BF16)
    pss = kb.rot('lp', [128, 512], F32, 4, psum=True)
    XTv = XT.rearrange("(kc p) t -> p kc t", p=128)
    for tb0 in range(0, T, TR):
        tbs = min(TR, T - tb0)
        kb.load(xq, xres, [(xres[:, :, :tbs], XTv[:, :, tb0:tb0 + tbs])])
        for n0g in range(0, N, NW):
            nsg = min(NW, N - n0g)
            wt = wts.next()
            W = Wfn(n0g, nsg)
            if len(W.shape) == 2:
                W = W.rearrange("(kc p) n -> p kc n", p=128)
            kb.load(wq, wt, [(wt[:, :, :nsg], W)])
            if not tm:
                for c0 in range(0, nsg, 128):
                    cs = min(128, nsg - c0)
                    for e0 in range(tb0, tb0 + tbs, EB):
                        es = min(EB, tb0 + tbs - e0)
                        epi.start(n0g + c0, cs, e0, es)
                        for t0 in range(e0, e0 + es, 512):
                            ts = min(512, e0 + es - t0)
                            ps = pss.next()
                            for kc in range(KC):
                                kb.op('pe', lambda e, kc=kc: e.matmul(ps[:cs, :ts], lhsT=wt[:, kc, c0:c0 + cs],
                                                                      rhs=xres[:, kc, t0 - tb0:t0 - tb0 + ts], start=(kc == 0),
                                                                      stop=(kc == KC - 1)),
                                      reads=(wt, xres), writes=(ps,))
                            epi.tile(n0g + c0, cs, t0, ts, ps)
                        epi.finish(n0g + c0, cs, e0, es)
            else:
                for s0 in range(0, tbs, 128):
                    ss = min(128, tbs - s0)
                    ps = pss.next()
                    for kc in range(KC):
                        kb.op('pe', lambda e, kc=kc: e.matmul(ps[:ss, :nsg], lhsT=xres[:, kc, s0:s0 + ss],
                                                              rhs=wt[:, kc, :nsg], start=(kc == 0),
                                                              stop=(kc == KC - 1)),
                              reads=(wt, xres), writes=(ps,))
                    epi.tile(n0g, nsg, tb0 + s0, ss, ps)


class store_epi:
    def __init__(self, kb, dst, dt, scale=None, func=None, q='act', tm=False, coloff=0):
        self.kb, self.dst, self.dt, self.scale, self.func, self.q, self.tm, self.coloff = kb, dst, dt, scale, func, q, tm, coloff
        self.obs = kb.rot('eo', [128, 512 if tm else EB], dt, 3 if tm else 2)

    def start(self, n0, ns, e0, es):
        self.ob = self.obs.next()
        self.e0 = e0

    def tile(self, n0, ns, t0, ts, ps):
        kb = self.kb
        sc = 1.0 if self.scale is None else self.scale
        fn = self.func or AF.Identity
        if self.tm:
            ob = self.obs.next()
            kb.op('act', lambda e: e.activation(out=ob[:ts, :ns], in_=ps[:ts, :ns], func=fn, scale=sc), reads=(ps,), writes=(ob,))
            kb.store(self.q, ob, [(self.dst[t0:t0 + ts, self.coloff + n0:self.coloff + n0 + ns], ob[:ts, :ns])])
        else:
            ob = self.ob
            o = t0 - self.e0
            kb.op('act', lambda e: e.activation(out=ob[:ns, o:o + ts], in_=ps[:ns, :ts], func=fn, scale=sc), reads=(ps,), writes=(ob,))

    def finish(self, n0, ns, e0, es):
        if not self.tm:
            self.kb.store(self.q, self.ob, [(self.dst[self.coloff + n0:self.coloff + n0 + ns, e0:e0 + es], self.ob[:ns, :es])])


def rmsnorm_fm(kb, C, src, dst, F, T, gam, bet, eps=1e-6, dst_dt=BF16, TT=256):
    FC = F // 128
    xs = kb.rot('nx', [128, FC, TT], F32, 2)
    sq = kb.rot('nq', [128, TT], F32, 2)
    pr = kb.rot('np', [128, 512], F32, 2, psum=True)
    rs = kb.rot('nr', [128, TT], F32, 2)
    ob = kb.rot('no', [128, FC, TT], dst_dt, 2)
    ones = C['ones_f32']
    sv = src.rearrange("(fc p) t -> p fc t", p=128)
    dv = dst.rearrange("(fc p) t -> p fc t", p=128)
    for t0 in range(0, T, TT):
        ts = min(TT, T - t0)
        x = xs.next()
        kb.load('sp', x, [(x[:, :, :ts], sv[:, :, t0:t0 + ts])])
        p = pr.next()
        for fc in range(FC):
            s = sq.next()
            kb.op('act', lambda e: e.activation(out=s[:, :ts], in_=x[:, fc, :ts], func=AF.Square), reads=(x,), writes=(s,))
            kb.op('pe', lambda e: e.matmul(p[:, :ts], lhsT=ones[:, :], rhs=s[:, :ts], start=(fc == 0), stop=(fc == FC - 1)),
                  reads=(s, ones), writes=(p,))
        r = rs.next()
        kb.op('dve', lambda e: e.tensor_scalar(out=r[:, :ts], in0=p[:, :ts], scalar1=1.0 / F, scalar2=eps,
                                               op0=ALU.mult, op1=ALU.add), reads=(p,), writes=(r,))
        kb.op('act', lambda e: e.activation(out=r[:, :ts], in_=r[:, :ts], func=AF.Ln), reads=(r,), writes=(r,))
        kb.op('act', lambda e: e.activation(out=r[:, :ts], in_=r[:, :ts], func=AF.Exp, scale=-0.5), reads=(r,), writes=(r,))
        o = ob.next()
        for fc in range(FC):
            kb.op('dve', lambda e: e.tensor_tensor(out=x[:, fc, :ts], in0=x[:, fc, :ts], in1=r[:, :ts], op=ALU.mult),
                  reads=(x, r), writes=(x,))
            if bet is not None:
                kb.op('act', lambda e: e.activation(out=o[:, fc, :ts], in_=x[:, fc, :ts], func=AF.Identity,
                                                    scale=gam[:, fc:fc + 1], bias=bet[:, fc:fc + 1]),
                      reads=(x, gam, bet), writes=(o,))
            else:
                kb.op('act', lambda e: e.activation(out=o[:, fc, :ts], in_=x[:, fc, :ts], func=AF.Identity,
                                                    scale=gam[:, fc:fc + 1]),
                      reads=(x, gam), writes=(o,))
        kb.store('act', o, [(dv[:, :, t0:t0 + ts], o[:, :, :ts])])


def ew_combine(kb, srcs, dst, F, T, dst_dt, gate=None, base=None, TT=512):
    n = len(srcs) + (1 if base is not None else 0)
    tl = kb.rot('ec', [128, n, TT], F32, 2)
    ob = kb.rot('eo', [128, TT], dst_dt, 2)
    for fc in range(F // 128):
        r0 = fc * 128
        for t0 in range(0, T, TT):
            ts = min(TT, T - t0)
            x = tl.next()
            pairs = [(x[:, i, :ts], s[r0:r0 + 128, t0:t0 + ts]) for i, s in enumerate(srcs)]
            if base is not None:
                pairs.append((x[:, n - 1, :ts], base[r0:r0 + 128, t0:t0 + ts]))
            kb.load('sp', x, pairs)
            for i in range(1, len(srcs)):
                kb.op('dve', lambda e: e.tensor_tensor(out=x[:, 0, :ts], in0=x[:, 0, :ts], in1=x[:, i, :ts], op=ALU.add),
                      reads=(x,), writes=(x,))
            o = ob.next()
            if base is not None:
                kb.op('dve', lambda e: e.scalar_tensor_tensor(out=o[:, :ts], in0=x[:, 0, :ts], scalar=gate[:, fc:fc + 1],
                                                              in1=x[:, n - 1, :ts], op0=ALU.mult, op1=ALU.add),
                      reads=(x, gate), writes=(o,))
            else:
                kb.op('act', lambda e: e.activation(out=o[:, :ts], in_=x[:, 0, :ts], func=AF.Identity), reads=(x,), writes=(o,))
            kb.store('act', o, [(dst[r0:r0 + 128, t0:t0 + ts], o[:, :ts])])


def attn_head(kb, C, c, parts, v_ap, dv, out_ap, scale, slope, mode, A, selT=None, sinkcol=None):
    S = c['S']
    NKT = S // 128
    QT = 128 if mode == 'swa' else 256
    qs, ks = [], []
    for i, (q_ap, k_ap, dk) in enumerate(parts):
        qb = A['q%d' % i].next()
        kb.load('sp', qb, [(qb[:dk, :], q_ap)])
        kbuf = A['k%d' % i].next()
        kb.load('sp', kbuf, [(kbuf[:dk, :], k_ap)])
        qs.append(qb)
        ks.append(kbuf)
    vb = A['v'].next()
    kb.load('sp', vb, [(vb[:, :, :dv], v_ap.rearrange("(t p) d -> p t d", p=128))])
    masks = C['masks']
    pairs = []
    for qi in range(S // QT):
        if mode == 'swa':
            kts = ([(qi - 1, 2)] if qi >= 1 else []) + [(qi, 0)]
        else:
            kts = []
            for kt in range(2 * qi + 2):
                if kt // 2 == qi:
                    kts.append((kt, kt % 2))
                else:
                    kts.append((kt, 'sel' if mode == 'moba' else None))
        for j, (kt, mk) in enumerate(kts):
            pairs.append(dict(qi=qi, q0=qi * QT, kt=kt, mk=mk, first=(j == 0), last=(j == len(kts) - 1)))

    def stage_a(p):
        q0, kt = p['q0'], p['kt']
        s_ps = A['s_ps'].next()
        for i, (q_ap, k_ap, dk) in enumerate(parts):
            kb.op('pe', lambda e: e.matmul(s_ps[:, :QT], lhsT=ks[i][:dk, kt * 128:(kt + 1) * 128],
                                           rhs=qs[i][:dk, q0:q0 + QT], start=(i == 0), stop=(i == len(parts) - 1)),
                  reads=(ks[i], qs[i]), writes=(s_ps,))
        p['s_ps'] = s_ps
        if p['mk'] == 'sel':
            m_ps = A['m_ps'].next()
            kb.op('pe', lambda e: e.matmul(m_ps[:, :QT], lhsT=C['esel'][:, kt // 2, :], rhs=selT[:, q0:q0 + QT], start=True, stop=True),
                  reads=(C['esel'], selT), writes=(m_ps,))
            p['m_ps'] = m_ps

    cur = {}

    def stage_b(p):
        q0, kt, mk, s_ps = p['q0'], p['kt'], p['mk'], p['s_ps']
        if p['first']:
            cur['o'] = A['o_ps'].next()
            cur['d'] = A['d_ps'].next()
        o_ps, d_ps = cur['o'], cur['d']
        if slope is not None:
            ds = A['ds'].next()
            kb.op('dve', lambda e: e.tensor_scalar(out=ds[:, :QT], in0=A['posq'][:, q0:q0 + QT], scalar1=A['posk'][:, kt:kt + 1],
                                                   scalar2=None, op0=ALU.subtract),
                  reads=(A['posq'], A['posk']), writes=(ds,))
            kb.op('dve', lambda e: e.scalar_tensor_tensor(out=ds[:, :QT], in0=ds[:, :QT], scalar=-1.0, in1=ds[:, :QT],
                                                          op0=ALU.mult, op1=ALU.max), reads=(ds,), writes=(ds,))
            kb.op('dve', lambda e: e.scalar_tensor_tensor(out=ds[:, :QT], in0=ds[:, :QT], scalar=-slope / scale, in1=s_ps[:, :QT],
                                                          op0=ALU.mult, op1=ALU.add), reads=(ds, s_ps), writes=(ds,))
            src = ds
        else:
            src = s_ps
        pb = A['pb'].next()
        if mk is None:
            kb.op('act', lambda e: e.activation(out=pb[:, :QT], in_=src[:, :QT], func=AF.Exp, scale=scale), reads=(src,), writes=(pb,))
        else:
            ex = A['ex'].next()
            kb.op('act', lambda e: e.activation(out=ex[:, :QT], in_=src[:, :QT], func=AF.Exp, scale=scale), reads=(src,), writes=(ex,))
            if mk == 'sel':
                m_ps = p['m_ps']
                kb.op('dve', lambda e: e.tensor_tensor(out=pb[:, :QT], in0=ex[:, :QT], in1=m_ps[:, :QT], op=ALU.mult),
                      reads=(ex, m_ps), writes=(pb,))
            else:
                kb.op('dve', lambda e: e.tensor_tensor(out=pb[:, :QT], in0=ex[:, :QT], in1=masks[:, mk, :QT], op=ALU.mult),
                      reads=(ex, masks), writes=(pb,))
        kb.op('pe', lambda e: e.matmul(o_ps[:dv, :QT], lhsT=vb[:, kt, :dv], rhs=pb[:, :QT], start=p['first'], stop=p['last']),
              reads=(vb, pb), writes=(o_ps,))
        kb.op('pe', lambda e: e.matmul(d_ps[:, :QT], lhsT=C['ones_bf'][:, :], rhs=pb[:, :QT], start=p['first'], stop=p['last']),
              reads=(C['ones_bf'], pb), writes=(d_ps,))
        if p['last']:
            rc = A['rc'].next()
            if sinkcol is not None:
                kb.op('dve', lambda e: e.tensor_scalar(out=rc[:, :QT], in0=d_ps[:, :QT], scalar1=sinkcol, scalar2=None, op0=ALU.add),
                      reads=(d_ps, A['esink']), writes=(rc,))
                kb.op('dve', lambda e: e.reciprocal(out=rc[:, :QT], in_=rc[:, :QT]), reads=(rc,), writes=(rc,))
            else:
                kb.op('dve', lambda e: e.reciprocal(out=rc[:, :QT], in_=d_ps[:, :QT]), reads=(d_ps,), writes=(rc,))
            ob = A['ob'].next()
            kb.op('dve', lambda e: e.tensor_tensor(out=ob[:dv, :QT], in0=o_ps[:dv, :QT], in1=rc[:dv, :QT], op=ALU.mult),
                  reads=(o_ps, rc), writes=(ob,))
            kb.store('act', ob, [(out_ap[:, q0:q0 + QT], ob[:dv, :QT])])

    LA = len(A['s_ps'].bufs) - 1
    for i in range(min(LA, len(pairs))):
        stage_a(pairs[i])
    for i, p in enumerate(pairs):
        if i + LA < len(pairs):
            stage_a(pairs[i + LA])
        stage_b(p)


def attn_bufs(kb, c, nparts, dks, need_pos, posf, deep=False):
    S = c['S']
    A = {}
    for i in range(nparts):
        A['q%d' % i] = kb.rot('aq%d' % i, [128, S], BF16, 2)
        A['k%d' % i] = kb.rot('ak%d' % i, [128, S], BF16, 2)
    A['v'] = kb.rot('av', [128, S // 128, 128], BF16, 2)
    nsm, nod = (3, 1) if deep else (2, 2)
    A['s_ps'] = kb.rot('as', [128, 512], F32, nsm, psum=True)
    A['m_ps'] = kb.rot('am', [128, 512], F32, nsm, psum=True)
    A['o_ps'] = kb.rot('ao', [128, 512], F32, nod, psum=True)
    A['d_ps'] = kb.rot('ad', [128, 512], F32, nod, psum=True)
    A['ex'] = kb.rot('aex', [128, 256], F32, 4)
    A['ds'] = kb.rot('ads', [128, 256], F32, 4)
    A['pb'] = kb.rot('apb', [128, 256], BF16, 4)
    A['rc'] = kb.rot('arc', [128, 256], F32, 2)
    A['ob'] = kb.rot('aob', [128, 256], BF16, 2)
    if need_pos:
        A['posq'] = kb.sb('posq', [128, S], F32)
        kb.load('sp', A['posq'], [(A['posq'][:, :], posf[0:1, :].to_broadcast([128, S]))])
        A['posk'] = kb.sb('posk', [128, S // 128], F32)
        kb.load('sp', A['posk'], [(A['posk'][:, :], posf[0, :].rearrange("(t p) -> p t", p=128))], allow_slow_non_contiguous=True)
    return A


def alibi_slopes(c):
    i = np.arange(1, c['NALIBI'] + 1, dtype=np.float32)
    s = np.exp2(np.float32(-8.0) * i / np.float32(c['NALIBI'])).astype(np.float32)
    return [float(v) for v in s[:c['WH']]], [float(v) for v in s[c['WH']:]]


def moba_phase(kb, C, c, ins, mqT, mkT, mv, ocT, posf):
    S, MH, NB = c['S'], c['MH'], c['S'] // c['MBLK']
    swa_sl, moba_sl = alibi_slopes(c)
    kb.begin()
    A = attn_bufs(kb, c, 1, [128], True, posf, deep=True)
    NT = S // 128
    el = kb.sb('elig', [128, NT, NB], F32)
    kb.load('sp', el, [(el[:, :, :], ins['elig'].rearrange("(t p) n -> p t n", p=128))])
    eln = kb.sb('eln', [128, NT, NB], F32)
    kb.op('dve', lambda e: e.tensor_scalar(out=eln[:, :, :], in0=el[:, :, :], scalar1=-1.0, scalar2=1e30, op0=ALU.add, op1=ALU.mult),
          reads=(el,), writes=(eln,))
    W8 = max(NB, 8)
    gms = kb.rot('gm', [128, W8], F32, 2)
    for b in gms.bufs:
        kb.op('dve', lambda e: e.memset(b[:, :], -1e30), writes=(b,))
    mx = kb.rot('mx', [128, 8], F32, 2)
    sl = kb.rot('sl', [128, NB], F32, 2)
    km = kb.sb('km', [128, NB], F32)
    kmb = kb.sb('kmb', [128, NB], BF16)
    selT = kb.rot('selT', [NB, S], BF16, 2)
    kfull = kb.rot('kfull', [128, S], BF16, 2)
    qfull = kb.rot('qfull', [128, S], BF16, 2)
    for h in range(MH):
        kf = kfull.next()
        qf = qfull.next()
        kb.load('sp', kf, [(kf[:, :], mkT[h * 128:(h + 1) * 128, :])])
        kb.load('sp', qf, [(qf[:, :], mqT[h * 128:(h + 1) * 128, :])])
        kb.op('dve', lambda e: e.tensor_reduce(out=km[:, :], in_=kf[:, :].rearrange("p (n k) -> p n k", n=NB), axis=AX.X, op=ALU.add),
              reads=(kf,), writes=(km,))
        kb.op('act', lambda e: e.activation(out=kmb[:, :], in_=km[:, :], func=AF.Identity, scale=1.0 / c['MBLK']), reads=(km,), writes=(kmb,))
        st = selT.next()
        for qt in range(NT):
            g_ps = A['s_ps'].next()
            kb.op('pe', lambda e: e.matmul(g_ps[:, :NB], lhsT=qf[:, qt * 128:(qt + 1) * 128], rhs=kmb[:, :], start=True, stop=True),
                  reads=(qf, kmb), writes=(g_ps,))
            gm = gms.next()
            kb.op('dve', lambda e: e.tensor_tensor(out=gm[:, :NB], in0=g_ps[:, :NB], in1=el[:, qt, :], op=ALU.mult), reads=(g_ps, el), writes=(gm,))
            kb.op('dve', lambda e: e.tensor_tensor(out=gm[:, :NB], in0=gm[:, :NB], in1=eln[:, qt, :], op=ALU.add), reads=(gm, eln), writes=(gm,))
            m = mx.next()
            kb.op('dve', lambda e: e.max(out=m[:, :], in_=gm[:, :]), reads=(gm,), writes=(m,))
            s = sl.next()
            kk = min(c['MTOPK'], NB) - 1
            kb.op('dve', lambda e: e.scalar_tensor_tensor(out=s[:, :], in0=gm[:, :NB], scalar=m[:, kk:kk + 1], in1=el[:, qt, :],
                                                          op0=ALU.is_ge, op1=ALU.mult), reads=(gm, m, el), writes=(s,))
            t_ps = A['m_ps'].next()
            kb.op('pe', lambda e: e.matmul(t_ps[:NB, :128], lhsT=s[:, :], rhs=C['ident_f32'][:, :], start=True, stop=True),
                  reads=(s, C['ident_f32']), writes=(t_ps,))
            kb.op('act', lambda e: e.activation(out=st[:, qt * 128:(qt + 1) * 128], in_=t_ps[:NB, :128], func=AF.Identity),
                  reads=(t_ps,), writes=(st,))
        attn_head(kb, C, c, [(mqT[h * 128:(h + 1) * 128, :], mkT[h * 128:(h + 1) * 128, :], 128)], mv[:, h * 128:(h + 1) * 128], 128,
                  ocT[h * 128:(h + 1) * 128, :], 128 ** -0.5, moba_sl[h], 'moba', A, selT=st)
    kb.end()


def swa_phase(kb, C, c, ins, l, wqT, wkT, wv, ocT, posf):
    S, WH, WKV, WD = c['S'], c['WH'], c['WKV'], c['WD']
    swa_sl, moba_sl = alibi_slopes(c)
    kb.begin()
    A = attn_bufs(kb, c, 1, [WD], True, posf)
    A['esink'] = kb.sb('esink', [128, WH], F32)
    kb.load('sp', A['esink'], [(A['esink'][:, :], ins['sinks'][l:l + 1, :].to_broadcast([128, WH]))])
    kb.op('act', lambda e: e.activation(out=A['esink'][:, :], in_=A['esink'][:, :], func=AF.Exp), reads=(A['esink'],), writes=(A['esink'],))
    R = WH // WKV
    base = 3 * c['BW']
    for h in range(WH):
        g = h // R
        attn_head(kb, C, c, [(wqT[h * WD:(h + 1) * WD, :], wkT[g * WD:(g + 1) * WD, :], WD)], wv[:, g * WD:(g + 1) * WD], WD,
                  ocT[base + h * WD:base + (h + 1) * WD, :], WD ** -0.5, swa_sl[h], 'swa', A, sinkcol=A['esink'][:, h:h + 1])
    kb.end()


def mla_phase(kb, C, c, ins, l, qlT, kvlT, krT, krswT, ocT, posf):
    S, LH, QL, KVL, RP = c['S'], c['LH'], c['QL'], c['KVL'], c['ROPE']
    qnT = kb.dram([QL, S], BF16)
    kvnT = kb.dram([KVL, S], BF16)
    kb.begin()
    gq = kb.sb('gq', [128, QL // 128], F32)
    gk = kb.sb('gk', [128, KVL // 128], F32)
    kb.load('sp', gq, [(gq[:, :], ins['qn_pk'][l])])
    kb.load('sp', gk, [(gk[:, :], ins['kvn_pk'][l])])
    kb.begin(); rmsnorm_fm(kb, C, qlT, qnT, QL, S, gq, None); kb.end()
    kb.begin(); rmsnorm_fm(kb, C, kvlT, kvnT, KVL, S, gk, None); kb.end()
    kb.end()
    qnopeT = kb.dram([LH * 128, S], BF16)
    qr_raw = kb.dram([LH * RP, S], F32)
    qr_sw = kb.dram([LH * RP, S], F32)
    knopeT = kb.dram([LH * 128, S], BF16)
    vtm = kb.dram([S, LH * 128], BF16)
    wq = ins['wq_perm'][l]
    wkv = ins['wkv_perm'][l]
    n1 = LH * 128
    n2 = LH * RP
    kb.begin(); linear(kb, qnT, QL, S, lambda n0, ns: wq[:, n0:n0 + ns], n1, store_epi(kb, qnopeT, BF16)); kb.end()
    kb.begin(); linear(kb, qnT, QL, S, lambda n0, ns: wq[:, n1 + n0:n1 + n0 + ns], n2, store_epi(kb, qr_raw, F32)); kb.end()
    kb.begin(); linear(kb, qnT, QL, S, lambda n0, ns: wq[:, n1 + n2 + n0:n1 + n2 + n0 + ns], n2, store_epi(kb, qr_sw, F32)); kb.end()
    kb.begin(); linear(kb, kvnT, KVL, S, lambda n0, ns: wkv[:, n0:n0 + ns], n1, store_epi(kb, knopeT, BF16)); kb.end()
    kb.begin(); linear(kb, kvnT, KVL, S, lambda n0, ns: wkv[:, n1 + n0:n1 + n0 + ns], n1, store_epi(kb, vtm, BF16, tm=True), tm=True); kb.end()
    qrT = kb.dram([LH * RP, S], BF16)
    krotT = kb.dram([RP, S], BF16)
    kb.begin()
    pq = kb.sb('pq', [128, S], F32)
    kb.load('sp', pq, [(pq[:, :], posf[0:1, :].to_broadcast([128, S]))])
    rc = kb.sb('ropec', [128, 2], F32)
    kb.load('sp', rc, [(rc[:, :], ins['ropec'])])
    cs = kb.sb('cos', [128, S], F32)
    sn = kb.sb('sin', [128, S], F32)
    kb.op('dve', lambda e: e.tensor_scalar(out=pq[:, :], in0=pq[:, :], scalar1=rc[:, 0:1], scalar2=None, op0=ALU.mult), reads=(pq, rc), writes=(pq,))
    ki = kb.sb('ki', [128, S], I32)
    kf = kb.sb('kf', [128, S], F32)
    yy = kb.sb('yy', [128, S], F32)
    for tb, sh in ((sn, 0.0), (cs, 0.5 * math.pi)):
        kb.op('dve', lambda e: e.tensor_scalar(out=yy[:, :], in0=pq[:, :], scalar1=1.0 / (2 * math.pi), scalar2=sh / (2 * math.pi) + 0.5,
                                               op0=ALU.mult, op1=ALU.add), reads=(pq,), writes=(yy,))
        kb.op('dve', lambda e: e.tensor_copy(out=ki[:, :], in_=yy[:, :]), reads=(yy,), writes=(ki,))
        kb.op('dve', lambda e: e.tensor_copy(out=kf[:, :], in_=ki[:, :]), reads=(ki,), writes=(kf,))
        kb.op('dve', lambda e: e.tensor_tensor(out=yy[:, :], in0=kf[:, :], in1=yy[:, :], op=ALU.is_gt), reads=(kf, yy), writes=(yy,))
        kb.op('dve', lambda e: e.tensor_tensor(out=kf[:, :], in0=kf[:, :], in1=yy[:, :], op=ALU.subtract), reads=(kf, yy), writes=(kf,))
        kb.op('dve', lambda e: e.tensor_scalar(out=tb[:, :], in0=pq[:, :], scalar1=sh, scalar2=None, op0=ALU.add), reads=(pq,), writes=(tb,))
        kb.op('dve', lambda e: e.scalar_tensor_tensor(out=tb[:, :], in0=kf[:, :], scalar=-2 * math.pi, in1=tb[:, :], op0=ALU.mult, op1=ALU.add),
              reads=(kf, tb), writes=(tb,))
        kb.op('act', lambda e: e.activation(out=tb[:, :], in_=tb[:, :], func=AF.Sin), reads=(tb,), writes=(tb,))
    kb.op('dve', lambda e: e.tensor_scalar(out=sn[:, :], in0=sn[:, :], scalar1=rc[:, 1:2], scalar2=None, op0=ALU.mult), reads=(sn, rc), writes=(sn,))
    ra = kb.rot('ra', [128, 2, 512], F32, 2)
    ro = kb.rot('ro', [128, 512], BF16, 2)
    jobs = [(qr_raw[r0:r0 + 128, :], qr_sw[r0:r0 + 128, :], qrT[r0:r0 + 128, :], 128) for r0 in range(0, LH * RP, 128)]
    jobs.append((krT, krswT, krotT, RP))
    for a_ap, b_ap, o_ap, p in jobs:
        for t0 in range(0, S, 512):
            x = ra.next()
            kb.load('sp', x, [(x[:p, 0, :], a_ap[:, t0:t0 + 512]), (x[:p, 1, :], b_ap[:, t0:t0 + 512])])
            kb.op('dve', lambda e: e.tensor_tensor(out=x[:p, 0, :], in0=x[:p, 0, :], in1=cs[:p, t0:t0 + 512], op=ALU.mult), reads=(x, cs), writes=(x,))
            kb.op('dve', lambda e: e.tensor_tensor(out=x[:p, 1, :], in0=x[:p, 1, :], in1=sn[:p, t0:t0 + 512], op=ALU.mult), reads=(x, sn), writes=(x,))
            o = ro.next()
            kb.op('dve', lambda e: e.tensor_tensor(out=o[:p, :], in0=x[:p, 0, :], in1=x[:p, 1, :], op=ALU.add), reads=(x,), writes=(o,))
            kb.store('act', o, [(o_ap[:, t0:t0 + 512], o[:p, :])])
    kb.end()
    kb.begin()
    A = attn_bufs(kb, c, 2, [128, RP], False, posf, deep=True)
    base = 2 * c['BW']
    for h in range(LH):
        attn_head(kb, C, c, [(qnopeT[h * 128:(h + 1) * 128, :], knopeT[h * 128:(h + 1) * 128, :], 128),
                             (qrT[h * RP:(h + 1) * RP, :], krotT, RP)], vtm[:, h * 128:(h + 1) * 128], 128,
                  ocT[base + h * 128:base + (h + 1) * 128, :], (c['NOPE'] + RP) ** -0.5, None, 'causal', A)
    kb.end()


def ssm_phase(kb, C, c, ins, l, zt, xbcT, dtr, ocT):
    S, SDI, SH, SG, SN, CC = c['S'], c['SDI'], c['SH'], c['SG'], c['SN'], c['CONVC'] // 128
    assert SN == 128 and c['SHD'] == 64
    NXC = SDI // 128
    HPG = SH // SG
    xcT = kb.dram([c['CONVC'], S], F32)
    kb.begin()
    cw = kb.sb('cw', [128, CC, 4], F32)
    cb = kb.sb('cb', [128, CC], F32)
    kb.load('sp', cw, [(cw[:, :, :], ins['conv_w_pk'][l])])
    kb.load('sp', cb, [(cb[:, :], ins['conv_b_pk'][l])])
    TT = 512
    ci = kb.rot('ci', [128, TT + 3], F32, 2)
    ca = kb.rot('ca', [128, TT], F32, 2)
    for cc in range(CC):
        for t0 in range(0, S, TT):
            u = ci.next()
            if t0 == 0:
                kb.op('dve', lambda e: e.memset(u[:, 0:3], 0.0), writes=(u,))
                kb.load('sp', u, [(u[:, 3:], xbcT[cc * 128:(cc + 1) * 128, 0:TT])])
            else:
                kb.load('sp', u, [(u[:, :], xbcT[cc * 128:(cc + 1) * 128, t0 - 3:t0 + TT])])
            a = ca.next()
            kb.op('dve', lambda e: e.tensor_scalar(out=a[:, :], in0=u[:, 0:TT], scalar1=cw[:, cc, 0:1], scalar2=None, op0=ALU.mult),
                  reads=(u, cw), writes=(a,))
            for k in range(1, 4):
                kb.op('dve', lambda e: e.scalar_tensor_tensor(out=a[:, :], in0=u[:, k:k + TT], scalar=cw[:, cc, k:k + 1], in1=a[:, :],
                                                              op0=ALU.mult, op1=ALU.add), reads=(u, cw, a), writes=(a,))
            kb.op('act', lambda e: e.activation(out=a[:, :], in_=a[:, :], func=AF.Silu, bias=cb[:, cc:cc + 1]), reads=(a, cb), writes=(a,))
            kb.store('act', a, [(xcT[cc * 128:(cc + 1) * 128, t0:t0 + TT], a[:, :])])
    kb.end()
    kb.begin()
    tri = C['masks']
    mneg = C['masks']
    ident = C['ident_f32']

    def bc(name, src, n):
        b = kb.sb(name, [128, n], F32)
        kb.load('sp', b, [(b[:, :], src.to_broadcast([128, n]))])
        return b
    dtb = bc('dtb', ins['dt_bias'][l:l + 1, :], SH)
    Abc = bc('Abc', ins['a_log'][l:l + 1, :], SH)
    kb.op('act', lambda e: e.activation(out=Abc[:, :], in_=Abc[:, :], func=AF.Exp), reads=(Abc,), writes=(Abc,))
    kb.op('dve', lambda e: e.tensor_scalar(out=Abc[:, :], in0=Abc[:, :], scalar1=-1.0, scalar2=None, op0=ALU.mult), reads=(Abc,), writes=(Abc,))
    Dbc = bc('Dbc', ins['d_skip'][l:l + 1, :], SH)
    nwb = bc('nwb', ins['ssm_norm'][l:l + 1, :], SDI)
    state = kb.sb('state', [128, SDI], F32)
    kb.op('dve', lambda e: e.memset(state[:, :], 0.0), writes=(state,))
    xcs = kb.rot('xc', [128, CC, 128], F32, 2)
    zs = kb.rot('z', [128, SDI], F32, 2)
    dts = kb.rot('dtr', [128, SH], F32, 2)
    pA = kb.rot('pA', [128, 512], F32, 3, psum=True)
    yps = [kb.ps('yps%d' % i, [128, 512], F32) for i in range((SDI + 511) // 512)]
    sps = [kb.ps('sps%d' % i, [128, 512], F32) for i in range((SDI + 511) // 512)]
    sm = {n: kb.sb(n, [128, SH], F32) for n in ('dt', 'a', 'acs', 'dec', 'cdec', 'tmp')}
    xs = kb.sb('xs', [128, SDI], F32)
    xdt = kb.sb('xdt', [128, SDI], F32)
    xdt_bf = kb.sb('xdtb', [128, SDI], BF16)
    xdec_bf = kb.sb('xdec', [128, SDI], BF16)
    st_bf = kb.sb('stb', [128, SDI], BF16)
    btm = kb.sb('btm', [128, SG, 128], BF16)
    BT = kb.sb('BT', [128, SG, 128], BF16)
    CT = kb.sb('CT', [128, SG, 128], BF16)
    gt = kb.sb('gt', [128, SG, 128], F32)
    arep = kb.rot('arep', [128, 128], F32, 2)
    dif = kb.rot('dif', [128, 128], F32, 2)
    edec = kb.rot('edec', [128, 128], F32, 2)
    MT = kb.rot('MT', [128, 128], BF16, 2)
    Cd = kb.rot('Cd', [128, 128], BF16, 2)
    ysb = kb.sb('ysb', [128, SDI], F32)
    gsb = kb.sb('gsb', [128, SDI], F32)
    ssq = kb.sb('ssq', [128, SG], F32)
    otb = kb.rot('otb', [128, NXC, 128], BF16, 2)
    v3 = lambda b: b[:, :].rearrange("p (h d) -> p h d", d=64)
    b3 = lambda b: b[:, :].unsqueeze(2).to_broadcast([128, SH, 64])
    GW = HPG * 64
    for ch in range(S // 128):
        t0 = ch * 128
        xc = xcs.next()
        kb.load('sp', xc, [(xc[:, :, :], xcT[:, t0:t0 + 128].rearrange("(cc p) t -> p cc t", p=128))])
        z = zs.next()
        kb.load('sp', z, [(z[:, :], zt[t0:t0 + 128, :])])
        dr = dts.next()
        kb.load('sp', dr, [(dr[:, :], dtr[t0:t0 + 128, :])])
        dt, a, acs, dec, cdec, tmp = (sm[n] for n in ('dt', 'a', 'acs', 'dec', 'cdec', 'tmp'))
        kb.op('dve', lambda e: e.tensor_tensor(out=dt[:, :], in0=dr[:, :], in1=dtb[:, :], op=ALU.add), reads=(dr, dtb), writes=(dt,))
        kb.op('act', lambda e: e.activation(out=dt[:, :], in_=dt[:, :], func=AF.Exp), reads=(dt,), writes=(dt,))
        kb.op('dve', lambda e: e.tensor_scalar(out=dt[:, :], in0=dt[:, :], scalar1=1.0, scalar2=None, op0=ALU.add), reads=(dt,), writes=(dt,))
        kb.op('act', lambda e: e.activation(out=dt[:, :], in_=dt[:, :], func=AF.Ln), reads=(dt,), writes=(dt,))
        kb.op('dve', lambda e: e.tensor_tensor(out=a[:, :], in0=dt[:, :], in1=Abc[:, :], op=ALU.mult), reads=(dt, Abc), writes=(a,))
        p1 = pA.next()
        kb.op('pe', lambda e: e.matmul(p1[:, 0:SH], lhsT=tri[:, 0, 0:128], rhs=a[:, :], start=True, stop=True), reads=(tri, a), writes=(p1,))
        kb.op('pe', lambda e: e.matmul(p1[:, 256:256 + SH], lhsT=C['ones_f32'][:, :], rhs=a[:, :], start=True, stop=True),
              reads=(C['ones_f32'], a), writes=(p1,))
        kb.op('act', lambda e: e.activation(out=acs[:, :], in_=p1[:, 0:SH], func=AF.Identity), reads=(p1,), writes=(acs,))
        kb.op('dve', lambda e: e.tensor_tensor(out=tmp[:, :], in0=p1[:, 256:256 + SH], in1=acs[:, :], op=ALU.subtract), reads=(p1, acs), writes=(tmp,))
        kb.op('act', lambda e: e.activation(out=dec[:, :], in_=tmp[:, :], func=AF.Exp), reads=(tmp,), writes=(dec,))
        kb.op('act', lambda e: e.activation(out=cdec[:, :], in_=p1[:, 256:256 + SH], func=AF.Exp), reads=(p1,), writes=(cdec,))
        for j0 in range(0, NXC, 4):
            pt = pA.next()
            nj = min(4, NXC - j0)
            for j in range(j0, j0 + nj):
                kb.op('pe', lambda e: e.matmul(pt[:, (j - j0) * 128:(j - j0 + 1) * 128], lhsT=xc[:, j, :], rhs=ident[:, :], start=True, stop=True),
                      reads=(xc, ident), writes=(pt,))
            kb.op('act', lambda e: e.activation(out=xs[:, j0 * 128:(j0 + nj) * 128], in_=pt[:, :nj * 128], func=AF.Identity), reads=(pt,), writes=(xs,))
        pt = pA.next()
        for g in range(SG):
            kb.op('pe', lambda e: e.matmul(pt[:, g * 128:(g + 1) * 128], lhsT=xc[:, NXC + g, :], rhs=ident[:, :], start=True, stop=True),
                  reads=(xc, ident), writes=(pt,))
        kb.op('act', lambda e: e.activation(out=btm[:, :, :].rearrange("p g n -> p (g n)"), in_=pt[:, :SG * 128], func=AF.Identity), reads=(pt,), writes=(btm,))
        kb.op('dve', lambda e: e.tensor_tensor(out=v3(xdt), in0=v3(xs), in1=b3(dt), op=ALU.mult), reads=(xs, dt), writes=(xdt,))
        kb.op('act', lambda e: e.activation(out=xdt_bf[:, :], in_=xdt[:, :], func=AF.Identity), reads=(xdt,), writes=(xdt_bf,))
        kb.op('dve', lambda e: e.tensor_tensor(out=v3(xdec_bf), in0=v3(xdt), in1=b3(dec), op=ALU.mult), reads=(xdt, dec), writes=(xdec_bf,))
        kb.op('act', lambda e: e.activation(out=BT[:, :, :], in_=xc[:, NXC:NXC + SG, :], func=AF.Identity), reads=(xc,), writes=(BT,))
        kb.op('act', lambda e: e.activation(out=CT[:, :, :], in_=xc[:, NXC + SG:NXC + 2 * SG, :], func=AF.Identity), reads=(xc,), writes=(CT,))
        pg = pA.next()
        for g in range(SG):
            kb.op('pe', lambda e: e.matmul(pg[:, g * 128:(g + 1) * 128], lhsT=BT[:, g, :], rhs=CT[:, g, :], start=True, stop=True),
                  reads=(BT, CT), writes=(pg,))
        kb.op('act', lambda e: e.activation(out=gt[:, :, :].rearrange("p g n -> p (g n)"), in_=pg[:, :SG * 128], func=AF.Identity), reads=(pg,), writes=(gt,))
        kb.op('act', lambda e: e.activation(out=st_bf[:, :], in_=state[:, :], func=AF.Identity), reads=(state,), writes=(st_bf,))
        pbs = {}

        def ssm_a(h):
            ar = arep.next()
            kb.op('dve', lambda e: e.tensor_copy(out=ar[:, :], in_=a[:, h:h + 1].to_broadcast([128, 128])), reads=(a,), writes=(ar,))
            pbh = pA.next()
            kb.op('pe', lambda e: e.matmul(pbh[:, 0:128], lhsT=ar[:, :], rhs=tri[:, 0, 0:128], start=True, stop=True), reads=(ar, tri), writes=(pbh,))
            pbs[h] = pbh
        ssm_a(0)
        for h in range(SH):
            g = h // HPG
            if h + 1 < SH:
                ssm_a(h + 1)
            pb = pbs.pop(h)
            df = dif.next()
            kb.op('dve', lambda e: e.scalar_tensor_tensor(out=df[:, :], in0=pb[:, 0:128], scalar=acs[:, h:h + 1], in1=mneg[:, 3, 0:128],
                                                          op0=ALU.subtract, op1=ALU.add), reads=(pb, acs, mneg), writes=(df,))
            kb.op('act', lambda e: e.activation(out=df[:, :], in_=df[:, :], func=AF.Exp), reads=(df,), writes=(df,))
            mt = MT.next()
            kb.op('dve', lambda e: e.tensor_tensor(out=mt[:, :], in0=df[:, :], in1=gt[:, g, :], op=ALU.mult), reads=(df, gt), writes=(mt,))
            ed = edec.next()
            kb.op('act', lambda e: e.activation(out=ed[:, :], in_=pb[:, 0:128], func=AF.Exp), reads=(pb,), writes=(ed,))
            cd = Cd.next()
            kb.op('dve', lambda e: e.tensor_tensor(out=cd[:, :], in0=xc[:, NXC + SG + g, :], in1=ed[:, :], op=ALU.mult), reads=(xc, ed), writes=(cd,))
            yp = yps[(h * 64) // 512]
            yc = (h * 64) % 512
            kb.op('pe', lambda e: e.matmul(yp[:, yc:yc + 64], lhsT=mt[:, :], rhs=xdt_bf[:, h * 64:(h + 1) * 64], start=True, stop=False),
                  reads=(mt, xdt_bf), writes=(yp,))
            kb.op('pe', lambda e: e.matmul(yp[:, yc:yc + 64], lhsT=cd[:, :], rhs=st_bf[:, h * 64:(h + 1) * 64], start=False, stop=True),
                  reads=(cd, st_bf), writes=(yp,))
        for g in range(SG):
            sp_ = sps[(g * GW) // 512]
            sc = (g * GW) % 512
            kb.op('pe', lambda e: e.matmul(sp_[:, sc:sc + GW], lhsT=btm[:, g, :], rhs=xdec_bf[:, g * GW:(g + 1) * GW], start=True, stop=True),
                  reads=(btm, xdec_bf), writes=(sp_,))
        kb.op('dve', lambda e: e.tensor_tensor(out=v3(state), in0=v3(state), in1=b3(cdec), op=ALU.mult), reads=(state, cdec), writes=(state,))
        for i, sp_ in enumerate(sps):
            w = min(512, SDI - i * 512)
            kb.op('dve', lambda e: e.tensor_tensor(out=state[:, i * 512:i * 512 + w], in0=state[:, i * 512:i * 512 + w], in1=sp_[:, :w], op=ALU.add),
                  reads=(state, sp_), writes=(state,))
        kb.op('dve', lambda e: e.tensor_tensor(out=v3(ysb), in0=v3(xs), in1=b3(Dbc), op=ALU.mult), reads=(xs, Dbc), writes=(ysb,))
        for i, yp in enumerate(yps):
            w = min(512, SDI - i * 512)
            kb.op('dve', lambda e: e.tensor_tensor(out=ysb[:, i * 512:i * 512 + w], in0=ysb[:, i * 512:i * 512 + w], in1=yp[:, :w], op=ALU.add),
                  reads=(ysb, yp), writes=(ysb,))
        kb.op('act', lambda e: e.activation(out=z[:, :], in_=z[:, :], func=AF.Silu), reads=(z,), writes=(z,))
        kb.op('dve', lambda e: e.tensor_tensor(out=gsb[:, :], in0=ysb[:, :], in1=z[:, :], op=ALU.mult), reads=(ysb, z), writes=(gsb,))
        kb.op('dve', lambda e: e.tensor_tensor(out=ysb[:, :], in0=gsb[:, :], in1=gsb[:, :], op=ALU.mult), reads=(gsb,), writes=(ysb,))
        kb.op('dve', lambda e: e.tensor_reduce(out=ssq[:, :], in_=ysb[:, :].rearrange("p (g k) -> p g k", g=SG), axis=AX.X, op=ALU.add),
              reads=(ysb,), writes=(ssq,))
        kb.op('dve', lambda e: e.tensor_scalar(out=ssq[:, :], in0=ssq[:, :], scalar1=float(SG) / SDI, scalar2=1e-6, op0=ALU.mult, op1=ALU.add),
              reads=(ssq,), writes=(ssq,))
        kb.op('act', lambda e: e.activation(out=ssq[:, :], in_=ssq[:, :], func=AF.Ln), reads=(ssq,), writes=(ssq,))
        kb.op('act', lambda e: e.activation(out=ssq[:, :], in_=ssq[:, :], func=AF.Exp, scale=-0.5), reads=(ssq,), writes=(ssq,))
        kb.op('dve', lambda e: e.tensor_tensor(out=gsb[:, :].rearrange("p (g k) -> p g k", g=SG), in0=gsb[:, :].rearrange("p (g k) -> p g k", g=SG),
                                               in1=ssq[:, :].unsqueeze(2).to_broadcast([128, SG, SDI // SG]), op=ALU.mult), reads=(gsb, ssq), writes=(gsb,))
        kb.op('dve', lambda e: e.tensor_tensor(out=gsb[:, :], in0=gsb[:, :], in1=nwb[:, :], op=ALU.mult), reads=(gsb, nwb), writes=(gsb,))
        ot = otb.next()
        for j0 in range(0, NXC, 4):
            pt = pA.next()
            nj = min(4, NXC - j0)
            for j in range(j0, j0 + nj):
                kb.op('pe', lambda e: e.matmul(pt[:, (j - j0) * 128:(j - j0 + 1) * 128], lhsT=gsb[:, j * 128:(j + 1) * 128], rhs=ident[:, :], start=True, stop=True),
                      reads=(gsb, ident), writes=(pt,))
            kb.op('act', lambda e: e.activation(out=ot[:, j0:j0 + nj, :].rearrange("p j t -> p (j t)"), in_=pt[:, :nj * 128], func=AF.Identity),
                  reads=(pt,), writes=(ot,))
        kb.store('act', ot, [(ocT[c['BW']:c['BW'] + SDI, t0:t0 + 128].rearrange("(j p) t -> p j t", p=128), ot[:, :, :])])
    kb.end()


def win_phase(kb, C, c, ins, l, hT):
    D, S = c['D'], c['S']
    wt = ins['w_in_t']
    o = {}

    def seg(name, dt, tm=False, func=None):
        c0, n_, go = c['WSEG'][name]
        dst = kb.dram([S, n_] if tm else [n_, S], dt)
        kb.begin()
        linear(kb, hT, D, S, lambda n0, ns: wt[l, go + n0 // TNW][:, :, :ns], n_, store_epi(kb, dst, dt, func=func, tm=tm), tm=tm, NW=TNW)
        kb.end()
        o[name] = dst
    seg('mqT', BF16)
    seg('mkT', BF16)
    seg('mv', BF16, tm=True)
    seg('z', F32, tm=True)
    seg('xbcT', F32)
    seg('dtr', F32, tm=True)
    seg('qlT', F32)
    seg('kvlT', F32)
    seg('krT', F32)
    dst = kb.dram([c['ROPE'], S], F32)
    wk = ins['w_krsw'][l]
    kb.begin(); linear(kb, hT, D, S, lambda n0, ns: wk[:, n0:n0 + ns], c['ROPE'], store_epi(kb, dst, F32)); kb.end()
    o['krswT'] = dst
    seg('wqT', BF16)
    seg('wkT', BF16)
    seg('wv', BF16, tm=True)
    seg('gatesT', F32, func=AF.Sigmoid)
    return o


def merge_phase(kb, C, c, ins, l, ocT, gatesT, xT, g1, x1T):
    D, S, BW = c['D'], c['S'], c['BW']
    parts = [kb.dram([D, S], F32) for _ in range(4)]
    for r in range(4):
        kb.begin()
        gts = kb.rot('gt', [128, EB], F32, 2)
        obs = kb.rot('mo', [128, EB], F32, 2)

        class MergeEpi:
            def start(self, n0, ns, e0, es, r=r):
                self.g = gts.next()
                kb.load('sp', self.g, [(self.g[:ns, :es], gatesT[r * D + n0:r * D + n0 + ns, e0:e0 + es])])
                self.ob = obs.next()
                self.e0 = e0

            def tile(self, n0, ns, t0, ts, ps):
                g, ob, o = self.g, self.ob, t0 - self.e0
                kb.op('dve', lambda e: e.tensor_tensor(out=ob[:ns, o:o + ts], in0=ps[:ns, :ts], in1=g[:ns, o:o + ts], op=ALU.mult),
                      reads=(ps, g), writes=(ob,))

            def finish(self, n0, ns, e0, es, r=r):
                kb.store('act', self.ob, [(parts[r][n0:n0 + ns, e0:e0 + es], self.ob[:ns, :es])])
        epi = MergeEpi()
        wb = ins['w_branch'][l, r]
        linear(kb, ocT[r * BW:(r + 1) * BW, :], BW, S, lambda n0, ns: wb[:, n0:n0 + ns], D, epi)
        kb.end()
    mT = kb.dram([D, S], BF16)
    kb.begin(); ew_combine(kb, parts, mT, D, S, BF16); kb.end()
    kb.begin()
    xts = kb.rot('xr', [128, EB], F32, 2)
    obs = kb.rot('xo', [128, EB], F32, 2)

    class OutEpi:
        def start(self, n0, ns, e0, es):
            self.x = xts.next()
            kb.load('sp', self.x, [(self.x[:ns, :es], xT[n0:n0 + ns, e0:e0 + es])])
            self.ob = obs.next()
            self.e0 = e0

        def tile(self, n0, ns, t0, ts, ps):
            x, ob, o, fc = self.x, self.ob, t0 - self.e0, n0 // 128
            kb.op('dve', lambda e: e.scalar_tensor_tensor(out=ob[:ns, o:o + ts], in0=ps[:ns, :ts], scalar=g1[:ns, fc:fc + 1], in1=x[:ns, o:o + ts],
                                                          op0=ALU.mult, op1=ALU.add), reads=(ps, g1, x), writes=(ob,))

        def finish(self, n0, ns, e0, es):
            kb.store('act', self.ob, [(x1T[n0:n0 + ns, e0:e0 + es], self.ob[:ns, :es])])
    epi2 = OutEpi()
    wo = ins['w_out_t']
    linear(kb, mT, D, S, lambda n0, ns: wo[l, n0 // TNW][:, :, :ns], D, epi2, NW=TNW)
    kb.end()


def moe_phase(kb, C, c, ins, l, h2T, x1T, g2, x2T):
    D, S, NG, EPG, NE, EH = c['D'], c['S'], c['NG'], c['EPG'], c['NE'], c['EH']
    NR = NG + NE
    wTd = kb.dram([NE, S], F32)
    kb.begin()
    brt = kb.sb('brt', [128, NR], F32)
    kb.load('sp', brt, [(brt[:, :], ins['b_rt'][l:l + 1, :].to_broadcast([128, NR]))])
    WT = kb.sb('WT', [NE, S], F32)
    lg = kb.rot('lg', [128, NR], F32, 2)
    s1 = kb.rot('s1', [128, 8], F32, 2)
    mx8 = kb.rot('mx8', [128, 8], F32, 2)
    pen = kb.rot('pen', [128, NG], F32, 2)
    lm = kb.rot('lm', [128, NE], F32, 2)
    sel = kb.rot('sel', [128, NE], F32, 2)
    ex = kb.rot('exr', [128, NE], F32, 2)
    tps = kb.rot('tps', [128, 512], F32, 2, psum=True)
    junk = kb.rot('junk', [128, NG], F32, 2)

    def epi(n0, ns, t0, ts, ps):
        L_ = lg.next()
        kb.op('dve', lambda e: e.tensor_tensor(out=L_[:, :], in0=ps[:, :NR], in1=brt[:, :], op=ALU.add), reads=(ps, brt), writes=(L_,))
        s = s1.next()
        kb.op('dve', lambda e: e.tensor_reduce(out=s[:, 0:1], in_=L_[:, 0:NG], axis=AX.X, op=ALU.max), reads=(L_,), writes=(s,))
        kb.op('dve', lambda e: e.tensor_scalar(out=s[:, 1:2], in0=s[:, 0:1], scalar1=-1.0, scalar2=None, op0=ALU.mult), reads=(s,), writes=(s,))
        jk = junk.next()
        kb.op('act', lambda e: e.activation(out=jk[:, :], in_=L_[:, 0:NG], func=AF.Exp, bias=s[:, 1:2], accum_out=s[:, 2:3]), reads=(L_, s), writes=(jk, s))
        p = pen.next()
        kb.op('dve', lambda e: e.tensor_scalar(out=p[:, :], in0=L_[:, 0:NG], scalar1=s[:, 0:1], scalar2=None, op0=ALU.is_ge), reads=(L_, s), writes=(p,))
        kb.op('dve', lambda e: e.tensor_scalar(out=p[:, :], in0=p[:, :], scalar1=-1.0, scalar2=1e30, op0=ALU.add, op1=ALU.mult), reads=(p,), writes=(p,))
        m = lm.next()
        kb.op('dve', lambda e: e.tensor_tensor(out=m[:, :].rearrange("p (g k) -> p g k", g=NG), in0=L_[:, NG:NR].rearrange("p (g k) -> p g k", g=NG),
                                               in1=p[:, :].unsqueeze(2).to_broadcast([128, NG, EPG]), op=ALU.add), reads=(L_, p), writes=(m,))
        x8 = mx8.next()
        kb.op('dve', lambda e: e.max(out=x8[:, :], in_=m[:, :]), reads=(m,), writes=(x8,))
        sl = sel.next()
        kb.op('dve', lambda e: e.tensor_scalar(out=sl[:, :], in0=m[:, :], scalar1=x8[:, 1:2], scalar2=None, op0=ALU.is_ge), reads=(m, x8), writes=(sl,))
        kb.op('dve', lambda e: e.tensor_scalar(out=s[:, 4:5], in0=x8[:, 0:1], scalar1=-1.0, scalar2=None, op0=ALU.mult), reads=(x8, s), writes=(s,))
        e_ = ex.next()
        kb.op('act', lambda e: e.activation(out=e_[:, :], in_=m[:, :], func=AF.Exp, bias=s[:, 4:5]), reads=(m, s), writes=(e_,))
        kb.op('act', lambda e: e.activation(out=s[:, 5:6], in_=x8[:, 1:2], func=AF.Exp, bias=s[:, 4:5]), reads=(x8, s), writes=(s,))
        kb.op('dve', lambda e: e.scalar_tensor_tensor(out=s[:, 5:6], in0=s[:, 5:6], scalar=1.0, in1=s[:, 2:3], op0=ALU.add, op1=ALU.mult), reads=(s,), writes=(s,))
        kb.op('dve', lambda e: e.reciprocal(out=s[:, 3:4], in_=s[:, 5:6]), reads=(s,), writes=(s,))
        kb.op('dve', lambda e: e.scalar_tensor_tensor(out=e_[:, :], in0=e_[:, :], scalar=s[:, 3:4], in1=sl[:, :], op0=ALU.mult, op1=ALU.mult),
              reads=(e_, s, sl), writes=(e_,))
        tp = tps.next()
        kb.op('pe', lambda e: e.matmul(tp[:NE, :128], lhsT=e_[:, :], rhs=C['ident_f32'][:, :], start=True, stop=True), reads=(e_, C['ident_f32']), writes=(tp,))
        kb.op('act', lambda e: e.activation(out=WT[:, t0:t0 + 128], in_=tp[:NE, :128], func=AF.Identity), reads=(tp,), writes=(WT,))
    wr = ins['w_rt'][l]
    linear(kb, h2T, D, S, lambda n0, ns: wr[:, n0:n0 + ns], NR, epi, tm=True)
    kb.store('act', WT, [(wTd[:, :], WT[:, :])])
    kb.end()
    NWE = c['NWE']
    sgT = kb.dram([NE * EH, S], BF16)
    hidT = kb.dram([NE * EH, S], BF16)
    wg = ins['ewg_t']
    wu = ins['ewu_t']
    kb.begin()
    linear(kb, h2T, D, S, lambda n0, ns: wg[l, n0 // EH, (n0 % EH) // NWE][:, :, :ns], NE * EH, store_epi(kb, sgT, BF16, func=AF.Silu), NW=NWE)
    kb.end()
    kb.begin()
    sgs = kb.rot('sg', [128, EB], BF16, 2)
    wbs = kb.rot('wb', [128, EB], F32, 2)
    hos = kb.rot('ho', [128, EB], BF16, 2)
    tus = kb.rot('tu', [128, 512], F32, 2)

    class UpEpi:
        def start(self, n0, ns, e0, es):
            ei = n0 // EH
            self.wb = wbs.next()
            kb.load('sp', self.wb, [(self.wb[:, :es], wTd[ei:ei + 1, e0:e0 + es].to_broadcast([128, es]))])
            self.sg = sgs.next()
            kb.load('sp', self.sg, [(self.sg[:ns, :es], sgT[n0:n0 + ns, e0:e0 + es])])
            self.ho = hos.next()
            self.e0 = e0

        def tile(self, n0, ns, t0, ts, ps):
            wb, sg, ho, o = self.wb, self.sg, self.ho, t0 - self.e0
            tmpb = tus.next()
            kb.op('dve', lambda e: e.tensor_tensor(out=tmpb[:ns, :ts], in0=ps[:ns, :ts], in1=wb[:ns, o:o + ts], op=ALU.mult), reads=(ps, wb), writes=(tmpb,))
            kb.op('dve', lambda e: e.tensor_tensor(out=ho[:ns, o:o + ts], in0=tmpb[:ns, :ts], in1=sg[:ns, o:o + ts], op=ALU.mult), reads=(tmpb, sg), writes=(ho,))

        def finish(self, n0, ns, e0, es):
            kb.store('act', self.ho, [(hidT[n0:n0 + ns, e0:e0 + es], self.ho[:ns, :es])])
    linear(kb, h2T, D, S, lambda n0, ns: wu[l, n0 // EH, (n0 % EH) // NWE][:, :, :ns], NE * EH, UpEpi(), NW=NWE)
    kb.end()
    KG = EPG * EH
    parts = [kb.dram([D, S], F32) for _ in range(NG)]
    wd = ins['ewd_t']
    for g in range(NG):
        kb.begin()
        linear(kb, hidT[g * KG:(g + 1) * KG, :], KG, S, lambda n0, ns, g=g: wd[l, g, n0 // TNW][:, :, :ns], D, store_epi(kb, parts[g], F32), NW=TNW)
        kb.end()
    kb.begin(); ew_combine(kb, parts, x2T, D, S, F32, gate=g2, base=x1T); kb.end()


def in_specs(c):
    D, S, L = c['D'], c['S'], c['L']
    DC = D // 128
    NB = S // c['MBLK']
    CC = c['CONVC'] // 128
    sp = [
        ('xT', [D, S], F32), ('cT', [D, 1], F32), ('pos', [1, S], I32),
        ('ada_w_t', [L, 6 * D // TNW, 128, DC, TNW], F32), ('ada_b_pk', [L, 128, 6 * DC], F32),
        ('nm_pk', [L, 128, DC], F32), ('nf_pk', [L, 128, DC], F32), ('fin_pk', [128, DC], F32),
        ('w_in_t', [L, c['WG'], 128, DC, TNW], F32), ('w_krsw', [L, D, c['ROPE']], F32),
        ('conv_w_pk', [L, 128, CC, 4], F32), ('conv_b_pk', [L, 128, CC], F32),
        ('dt_bias', [L, c['SH']], F32), ('a_log', [L, c['SH']], F32), ('d_skip', [L, c['SH']], F32), ('ssm_norm', [L, c['SDI']], F32),
        ('qn_pk', [L, 128, c['QL'] // 128], F32), ('kvn_pk', [L, 128, c['KVL'] // 128], F32),
        ('wq_perm', [L, c['QL'], c['LH'] * (128 + 2 * c['ROPE'])], F32), ('wkv_perm', [L, c['KVL'], c['LH'] * 256], F32),
        ('sinks', [L, c['WH']], F32), ('w_branch', [L, 4, c['BW'], D], F32), ('w_out_t', [L, D // TNW, 128, DC, TNW], F32),
        ('w_rt', [L, D, c['NG'] + c['NE']], F32), ('b_rt', [L, c['NG'] + c['NE']], F32),
        ('ewg_t', [L, c['NE'], c['EH'] // c['NWE'], 128, DC, c['NWE']], F32), ('ewu_t', [L, c['NE'], c['EH'] // c['NWE'], 128, DC, c['NWE']], F32),
        ('ewd_t', [L, c['NG'], D // TNW, 128, c['EPG'] * c['EH'] // 128, TNW], F32),
        ('masks', [128, 4, 256], F32), ('ident', [128, 128], F32), ('elig', [S, NB], F32), ('esel', [NB, NB * 128], F32),
        ('ropec', [128, 2], F32),
    ]
    return sp


def build(cfg):
    c = derive(cfg)
    D, S, L = c['D'], c['S'], c['L']
    DC = D // 128
    NB = S // c['MBLK']
    nc = bass.Bass("TRN2", target_bir_lowering=False)
    ins = {n: nc.dram_tensor(n, sh, dt, kind="ExternalInput").ap() for n, sh, dt in in_specs(c)}
    outT = nc.dram_tensor("outT", [D, S], F32, kind="ExternalOutput").ap()
    kb = KB(nc)
    kb.begin()
    C = {}
    C['ones_f32'] = kb.sb('ones', [128, 128], F32)
    kb.op('dve', lambda e: e.memset(C['ones_f32'][:, :], 1.0), writes=(C['ones_f32'],))
    C['ones_bf'] = kb.sb('onesb', [128, 128], BF16)
    kb.op('dve', lambda e: e.memset(C['ones_bf'][:, :], 1.0), writes=(C['ones_bf'],))
    C['ident_f32'] = kb.sb('ident', [128, 128], F32)
    kb.load('sp', C['ident_f32'], [(C['ident_f32'][:, :], ins['ident'])])
    C['masks'] = kb.sb('masks', [128, 4, 256], F32)
    kb.load('sp', C['masks'], [(C['masks'][:, :, :], ins['masks'])])
    C['esel'] = kb.sb('esel', [NB, NB, 128], BF16)
    kb.load('pool', C['esel'], [(C['esel'][:, :, :], ins['esel'].rearrange("k (n m) -> k n m", m=128))])
    posf = kb.dram([1, S], F32)
    kb.begin()
    pi = kb.sb('posi', [1, S], I32)
    pf = kb.sb('posf', [1, S], F32)
    kb.load('sp', pi, [(pi[:, :], ins['pos'])])
    kb.op('dve', lambda e: e.tensor_copy(out=pf[:, :], in_=pi[:, :]), reads=(pi,), writes=(pf,))
    kb.store('act', pf, [(posf[:, :], pf[:, :])])
    kb.end()
    mods = []
    for l in range(L):
        m = kb.sb('mod%d' % l, [128, 6 * DC], F32)
        ab = kb.sb('adab%d' % l, [128, 6 * DC], F32)
        kb.load('sp', ab, [(ab[:, :], ins['ada_b_pk'][l])])
        kb.begin()

        def epi(n0, ns, t0, ts, ps, m=m, ab=ab):
            j = n0 // 128
            kb.op('dve', lambda e: e.tensor_tensor(out=m[:, j:j + 1], in0=ps[:, 0:1], in1=ab[:, j:j + 1], op=ALU.add), reads=(ps, ab), writes=(m,))
        aw = ins['ada_w_t']
        linear(kb, ins['cT'], D, 1, lambda n0, ns, l=l: aw[l, n0 // TNW][:, :, :ns], 6 * D, epi, xq='pool', NW=TNW)
        kb.end()
        nm = kb.sb('nm%d' % l, [128, DC], F32)
        nf = kb.sb('nf%d' % l, [128, DC], F32)
        kb.load('sp', nm, [(nm[:, :], ins['nm_pk'][l])])
        kb.load('sp', nf, [(nf[:, :], ins['nf_pk'][l])])
        gam1 = kb.sb('gam1_%d' % l, [128, DC], F32)
        gam2 = kb.sb('gam2_%d' % l, [128, DC], F32)
        kb.op('dve', lambda e: e.scalar_tensor_tensor(out=gam1[:, :], in0=m[:, DC:2 * DC], scalar=1.0, in1=nm[:, :], op0=ALU.add, op1=ALU.mult),
              reads=(m, nm), writes=(gam1,))
        kb.op('dve', lambda e: e.scalar_tensor_tensor(out=gam2[:, :], in0=m[:, 4 * DC:5 * DC], scalar=1.0, in1=nf[:, :], op0=ALU.add, op1=ALU.mult),
              reads=(m, nf), writes=(gam2,))
        sh1 = kb.sb('sh1_%d' % l, [128, DC], F32)
        g1 = kb.sb('g1_%d' % l, [128, DC], F32)
        sh2 = kb.sb('sh2_%d' % l, [128, DC], F32)
        g2 = kb.sb('g2_%d' % l, [128, DC], F32)
        for dst, k in ((sh1, 0), (g1, 2), (sh2, 3), (g2, 5)):
            kb.op('dve', lambda e: e.tensor_copy(out=dst[:, :], in_=m[:, k * DC:(k + 1) * DC]), reads=(m,), writes=(dst,))
        mods.append(dict(gam1=gam1, sh1=sh1, g1=g1, gam2=gam2, sh2=sh2, g2=g2))
    fin = kb.sb('fin', [128, DC], F32)
    kb.load('sp', fin, [(fin[:, :], ins['fin_pk'])])
    xT = ins['xT']
    for l in range(L):
        md = mods[l]
        hT = kb.dram([D, S], BF16)
        kb.begin(); rmsnorm_fm(kb, C, xT, hT, D, S, md['gam1'], md['sh1']); kb.end()
        o = win_phase(kb, C, c, ins, l, hT)
        ocT = kb.dram([4 * c['BW'], S], BF16)
        moba_phase(kb, C, c, ins, o['mqT'], o['mkT'], o['mv'], ocT, posf)
        ssm_phase(kb, C, c, ins, l, o['z'], o['xbcT'], o['dtr'], ocT)
        mla_phase(kb, C, c, ins, l, o['qlT'], o['kvlT'], o['krT'], o['krswT'], ocT, posf)
        swa_phase(kb, C, c, ins, l, o['wqT'], o['wkT'], o['wv'], ocT, posf)
        x1T = kb.dram([D, S], F32)
        merge_phase(kb, C, c, ins, l, ocT, o['gatesT'], xT, md['g1'], x1T)
        h2T = kb.dram([D, S], BF16)
        kb.begin(); rmsnorm_fm(kb, C, x1T, h2T, D, S, md['gam2'], md['sh2']); kb.end()
        x2T = kb.dram([D, S], F32)
        moe_phase(kb, C, c, ins, l, h2T, x1T, md['g2'], x2T)
        xT = x2T
    kb.begin(); rmsnorm_fm(kb, C, xT, outT, D, S, fin, None, dst_dt=F32); kb.end()
    kb.end()
    return nc


def host_consts(c):
    S = c['S']
    NB = S // c['MBLK']
    p = np.arange(128)[:, None]
    q = np.arange(256)[None, :]
    masks = np.zeros((128, 4, 256), np.float32)
    masks[:, 0] = (p <= q)
    masks[:, 1] = (128 + p <= q)
    masks[:, 2, :128] = (p > q[:, :128])
    masks[:, 3] = np.where(p <= q, 0.0, NEG)
    ident = np.eye(128, dtype=np.float32)
    qb = (np.arange(S) // c['MBLK'])[:, None]
    elig = (np.arange(NB)[None, :] < qb).astype(np.float32)
    esel = np.zeros((NB, NB, 128), np.float32)
    for n in range(NB):
        esel[n, n, :] = 1.0
    half = c['ROPE'] // 2
    inv = (np.float32(10000.0) ** (-np.arange(half, dtype=np.float32) / np.float32(half))).astype(np.float32)
    pp = np.arange(128) % c['ROPE']
    ropec = np.stack([inv[pp % half], np.where(pp < half, -1.0, 1.0)], 1).astype(np.float32)
    return dict(masks=masks, ident=ident, elig=elig, esel=esel.reshape(NB, NB * 128), ropec=ropec)


def pk(v):
    v = np.asarray(v)
    return np.ascontiguousarray(np.swapaxes(v.reshape(v.shape[:-1] + (-1, 128)), -1, -2))


def tile_w(W, NW):
    W = np.asarray(W)
    K, N = W.shape[-2:]
    G = -(-N // NW)
    if G * NW != N:
        W = np.concatenate([W, np.zeros(W.shape[:-1] + (G * NW - N,), W.dtype)], -1)
    W = W.reshape(W.shape[:-2] + (K // 128, 128, G, NW))
    nd = W.ndim
    perm = tuple(range(nd - 4)) + (nd - 2, nd - 3, nd - 4, nd - 1)
    return np.ascontiguousarray(W.transpose(perm))


def prep_shared(inp, c):
    L = c['L']
    off = c['IN_OFF']
    half = c['ROPE'] // 2
    LH, RP = c['LH'], c['ROPE']
    w_in = np.asarray(inp['w_in'])
    kr = w_in[:, :, off[6]:off[7]]
    d = {}
    d['ada_w_t'] = tile_w(inp['ada_w'], TNW)
    d['ada_b_pk'] = pk(inp['ada_b'])
    d['nm_pk'] = pk(inp['norm_mix'])
    d['nf_pk'] = pk(inp['norm_ffn'])
    d['fin_pk'] = pk(inp['final_norm'])
    d['w_in_t'] = np.concatenate([tile_w(w_in[:, :, c0:c0 + n], TNW) for (c0, n, go) in c['WSEG'].values()], 1)
    d['w_krsw'] = np.ascontiguousarray(np.concatenate([kr[:, :, half:], kr[:, :, :half]], -1))
    CC = c['CONVC'] // 128
    cw = np.asarray(inp['conv_w'])
    d['conv_w_pk'] = np.ascontiguousarray(cw.reshape(L, 4, CC, 128).transpose(0, 3, 2, 1))
    d['conv_b_pk'] = pk(inp['conv_b'])
    for k in ('dt_bias', 'a_log', 'd_skip', 'ssm_norm'):
        d[k] = np.asarray(inp[k])
    d['qn_pk'] = pk(inp['mla_q_norm'])
    d['kvn_pk'] = pk(inp['mla_kv_norm'])
    wq = np.asarray(inp['mla_wq_b']).reshape(L, c['QL'], LH, c['NOPE'] + RP)
    nope = wq[..., :c['NOPE']].reshape(L, c['QL'], -1)
    rope = wq[..., c['NOPE']:]
    rsw = np.concatenate([rope[..., half:], rope[..., :half]], -1)
    d['wq_perm'] = np.ascontiguousarray(np.concatenate([nope, rope.reshape(L, c['QL'], -1), rsw.reshape(L, c['QL'], -1)], -1))
    wkv = np.asarray(inp['mla_wkv_b']).reshape(L, c['KVL'], LH, c['NOPE'] + c['LV'])
    d['wkv_perm'] = np.ascontiguousarray(np.concatenate([wkv[..., :c['NOPE']].reshape(L, c['KVL'], -1),
                                                         wkv[..., c['NOPE']:].reshape(L, c['KVL'], -1)], -1))
    d['sinks'] = np.asarray(inp['swa_sinks'])
    d['w_branch'] = np.asarray(inp['w_branch'])
    d['w_out_t'] = tile_w(inp['w_out'], TNW)
    d['w_rt'] = np.ascontiguousarray(np.concatenate([inp['router_group_w'], inp['router_w']], -1))
    d['b_rt'] = np.ascontiguousarray(np.concatenate([inp['router_group_b'], inp['router_b']], -1))
    d['ewg_t'] = tile_w(inp['exp_w_gate'], c['NWE'])
    d['ewu_t'] = tile_w(inp['exp_w_up'], c['NWE'])
    ed = np.asarray(inp['exp_w_down'])
    d['ewd_t'] = tile_w(ed.reshape(L, c['NG'], c['EPG'] * c['EH'], c['D']), TNW)
    d.update(host_consts(c))
    return d


_NC_CACHE = {}


def run(inp, cfg):
    c = derive(cfg)
    key = tuple(sorted((k, v) for k, v in cfg.items()))
    if key not in _NC_CACHE:
        _NC_CACHE[key] = build(cfg)
    nc = _NC_CACHE[key]
    shared = prep_shared(inp, c)
    x = np.asarray(inp['x'])
    B = x.shape[0]
    maps = []
    for b in range(B):
        m = dict(shared)
        m['xT'] = np.ascontiguousarray(x[b].T)
        m['cT'] = np.ascontiguousarray(np.asarray(inp['c'])[b][:, None])
        m['pos'] = np.ascontiguousarray(np.asarray(inp['positions'])[b][None, :].astype(np.int32))
        maps.append(m)
    res = run_bass_kernel_spmd(nc, maps, core_ids=list(range(B)))
    out = np.stack([np.ascontiguousarray(res.results[b]['outT'].T) for b in range(B)], 0)
    return out.astype(np.float32)


def kernel(**inputs):
    return run(inputs, FULL)
```
